# Optimizing a Trainium2 kernel written in Bass

```python
import math
import jax
import jax.numpy as jnp
from jax import lax
import numpy as np


D_MODEL = 1024
BATCH = 2
SEQ = 8192
DEPTH = 2

N_BRANCH = 4
SC_WIDTH = D_MODEL // 4
SC_GROUPS = 4
SC_K = 3
LRU_WIDTH = D_MODEL // 4
LRU_HEADS = 4
LRU_HEAD_DIM = LRU_WIDTH // LRU_HEADS
LRU_CONV_K = 4
LRU_C = 8.0
SG_WIDTH = D_MODEL // 4
SG_GROUPS = 4
SG_GROUP_DIM = SG_WIDTH // SG_GROUPS
SG_CHUNK = 128
RET_WIDTH = D_MODEL // 4
RET_HEADS = 4
RET_HEAD_DIM = RET_WIDTH // RET_HEADS
RET_CHUNK = 128
ROPE_BASE = 10000.0
N_GROUPS = 4
EXPERTS_PER_GROUP = 8
N_EXPERTS = N_GROUPS * EXPERTS_PER_GROUP
TOP_K = 2
D_EXPERT = D_MODEL // 4
LN_EPS = 1e-5
DEEPNORM_ALPHA = (2.0 * DEPTH) ** 0.25
DEEPNORM_BETA = (8.0 * DEPTH) ** -0.25

kernel_name = 'hybrid_gated_parallel_mixers_hier_moe_deepnorm'


def _standardize(x):
    x32 = x.astype(jnp.float32)
    mu = jnp.mean(x32, axis=-1, keepdims=True)
    var = jnp.mean(jnp.square(x32 - mu), axis=-1, keepdims=True)
    return (x32 - mu) * lax.rsqrt(var + LN_EPS)


def _layer_norm(x, g, b):
    return (_standardize(x) * g.astype(jnp.float32) + b.astype(jnp.float32)).astype(x.dtype)


def _causal_depthwise_conv(x, w, b):
    k = w.shape[0]
    c = x.shape[-1]
    y = lax.conv_general_dilated(x, w[:, None, :].astype(x.dtype), window_strides=(1,),
                                 padding=[(k - 1, 0)], dimension_numbers=('NWC', 'WIO', 'NWC'),
                                 feature_group_count=c)
    return y + b.astype(x.dtype)


def _rope_tables(positions):
    half = RET_HEAD_DIM // 2
    inv_freq = ROPE_BASE ** (-jnp.arange(half, dtype=jnp.float32) / half)
    ang = positions.astype(jnp.float32)[..., None] * inv_freq
    return jnp.cos(ang)[:, :, None, :], jnp.sin(ang)[:, :, None, :]


def _apply_rope(t, cos, sin):
    t1, t2 = jnp.split(t.astype(jnp.float32), 2, axis=-1)
    return jnp.concatenate([t1 * cos - t2 * sin, t2 * cos + t1 * sin], axis=-1)


def _short_conv_mixer(b_gate, c_gate, xa, conv_w, conv_b):
    return b_gate * _causal_depthwise_conv(c_gate * xa, conv_w, conv_b)


def _rg_lru_mixer(xb, conv_w, conv_b, w_r, b_r, w_i, b_i, lam):
    bsz, seq, width = xb.shape
    xc = _causal_depthwise_conv(xb, conv_w, conv_b)
    xh = xc.reshape(bsz, seq, LRU_HEADS, LRU_HEAD_DIM)
    r = jax.nn.sigmoid(jnp.einsum('bshd,hde->bshe', xh, w_r).reshape(bsz, seq, width) + b_r).astype(jnp.float32)
    i = jax.nn.sigmoid(jnp.einsum('bshd,hde->bshe', xh, w_i).reshape(bsz, seq, width) + b_i).astype(jnp.float32)
    log_a = -LRU_C * r * jax.nn.softplus(-lam.astype(jnp.float32))
    a = jnp.exp(log_a)
    u = jnp.sqrt(-jnp.expm1(2.0 * log_a)) * (i * xc.astype(jnp.float32))

    def combine(left, right):
        a_l, h_l = left
        a_r, h_r = right
        return a_l * a_r, a_r * h_l + h_r

    _, h = lax.associative_scan(combine, (a, u), axis=1)
    return h.astype(xb.dtype)


def _chunked_spatial_gating(zu, zv, norm_g, w_s, b_s):
    bsz, seq, width = zu.shape
    n_chunks = seq // SG_CHUNK
    u = jax.nn.gelu(zu)
    v = jax.nn.gelu(zv).reshape(bsz, seq, SG_GROUPS, SG_GROUP_DIM)
    v = _standardize(v) * norm_g.astype(jnp.float32).reshape(SG_GROUPS, SG_GROUP_DIM)
    v = v.reshape(bsz, n_chunks, SG_CHUNK, SG_GROUPS, SG_GROUP_DIM)
    causal = jnp.tril(jnp.ones((SG_CHUNK, SG_CHUNK), jnp.float32))
    sv = jnp.einsum('gts,bcsgd->bctgd', w_s.astype(jnp.float32) * causal, v) \
        + b_s.astype(jnp.float32).T[:, :, None]
    return u * sv.reshape(bsz, seq, width).astype(zu.dtype)


def _retention(zq, zk, zv, zg, norm_g, cos, sin):
    bsz, seq, width = zq.shape
    n_chunks = seq // RET_CHUNK
    shp = (bsz, seq, RET_HEADS, RET_HEAD_DIM)
    cshp = (bsz, n_chunks, RET_CHUNK, RET_HEADS, RET_HEAD_DIM)
    q = _apply_rope(zq.reshape(shp), cos, sin).reshape(cshp)
    k = (_apply_rope(zk.reshape(shp), cos, sin) * RET_HEAD_DIM ** -0.5).reshape(cshp)
    v = zv.astype(jnp.float32).reshape(cshp)
    log_gamma = jnp.log1p(-jnp.exp2(-5.0 - jnp.arange(RET_HEADS, dtype=jnp.float32)))
    pos = jnp.arange(RET_CHUNK, dtype=jnp.float32)
    diff = pos[:, None] - pos[None, :]
    decay = jnp.where(diff >= 0, jnp.exp(jnp.maximum(diff, 0.0) * log_gamma[:, None, None]), 0.0)
    scores = jnp.einsum('bcnhd,bcmhd->bchnm', q, k) * decay
    inner = jnp.einsum('bchnm,bcmhe->bcnhe', scores, v)
    k_decay = jnp.exp((RET_CHUNK - 1.0 - pos)[:, None] * log_gamma)
    kv = jnp.einsum('bcmhd,bcmhe,mh->bchde', k, v, k_decay)
    chunk_decay = jnp.exp(RET_CHUNK * log_gamma)[None, :, None, None]

    def step(state, kv_c):
        return chunk_decay * state + kv_c, state

    init = jnp.zeros((bsz, RET_HEADS, RET_HEAD_DIM, RET_HEAD_DIM), jnp.float32)
    _, prev = lax.scan(step, init, jnp.moveaxis(kv, 1, 0))
    prev = jnp.moveaxis(prev, 0, 1)
    q_decay = jnp.exp((pos + 1.0)[:, None] * log_gamma)
    cross = jnp.einsum('bcnhd,bchde,nh->bcnhe', q, prev, q_decay)
    o = (inner + cross).reshape(shp)
    o = (_standardize(o) * norm_g.astype(jnp.float32).reshape(RET_HEADS, RET_HEAD_DIM)).reshape(bsz, seq, width)
    return (jax.nn.silu(zg.astype(jnp.float32)) * o).astype(zq.dtype)


def _hier_moe(h, wg, bg, we, be, w_gate, w_up, w_down):
    bsz, seq, d = h.shape
    ht = h.reshape(-1, d)
    g_prob = jax.nn.softmax((ht @ wg + bg).astype(jnp.float32), axis=-1)
    g_top_p, g_idx = lax.top_k(g_prob, 1)
    e_logits = (ht @ we + be).astype(jnp.float32).reshape(-1, N_GROUPS, EXPERTS_PER_GROUP)
    sel = jnp.take_along_axis(e_logits, g_idx[:, :, None], axis=1)[:, 0]
    e_prob = jax.nn.softmax(sel, axis=-1)
    top_p, top_i = lax.top_k(e_prob, TOP_K)
    top_p = top_p / jnp.sum(top_p, axis=-1, keepdims=True)
    weights = g_top_p * top_p
    expert_id = g_idx * EXPERTS_PER_GROUP + top_i
    combine = jnp.sum(jax.nn.one_hot(expert_id, N_EXPERTS, dtype=jnp.float32) * weights[..., None], axis=1)
    out = jnp.zeros(ht.shape, jnp.float32)
    for e in range(N_EXPERTS):
        hid = jax.nn.silu(ht @ w_gate[e]) * (ht @ w_up[e])
        out = out + combine[:, e:e + 1] * (hid @ w_down[e]).astype(jnp.float32)
    return out.reshape(bsz, seq, d).astype(h.dtype)


def setup_inputs(seed: int = 0) -> dict:
    key = jax.random.key(seed)
    k = jax.random.split(key, 32)
    L, D = DEPTH, D_MODEL
    f32 = jnp.float32

    def nrm(i, shape, scale):
        return jax.random.normal(k[i], shape, f32) * scale

    n_in = 3 * SC_WIDTH + LRU_WIDTH + 2 * SG_WIDTH + 4 * RET_WIDTH + N_BRANCH * D
    u = jax.random.uniform(k[10], (L, LRU_WIDTH), f32, 0.9, 0.999)
    a0 = u ** (1.0 / LRU_C)
    return {
        'x': nrm(0, (BATCH, SEQ, D), 1.0),
        'positions': jnp.broadcast_to(jnp.arange(SEQ, dtype=jnp.int32)[None, :], (BATCH, SEQ)),
        'w_in': nrm(1, (L, D, n_in), D ** -0.5),
        'sc_conv_w': nrm(2, (L, SC_K, SC_WIDTH), SC_K ** -0.5),
        'sc_conv_b': nrm(3, (L, SC_WIDTH), 0.01),
        'lru_conv_w': nrm(4, (L, LRU_CONV_K, LRU_WIDTH), LRU_CONV_K ** -0.5),
        'lru_conv_b': nrm(5, (L, LRU_WIDTH), 0.01),
        'lru_w_r': nrm(6, (L, LRU_HEADS, LRU_HEAD_DIM, LRU_HEAD_DIM), LRU_HEAD_DIM ** -0.5),
        'lru_b_r': nrm(7, (L, LRU_WIDTH), 0.01),
        'lru_w_i': nrm(8, (L, LRU_HEADS, LRU_HEAD_DIM, LRU_HEAD_DIM), LRU_HEAD_DIM ** -0.5),
        'lru_b_i': nrm(9, (L, LRU_WIDTH), 0.01),
        'lru_lambda': jnp.log(a0) - jnp.log1p(-a0),
        'sg_norm_g': 1.0 + nrm(11, (L, SG_WIDTH), 0.02),
        'sg_w_s': nrm(12, (L, SG_GROUPS, SG_CHUNK, SG_CHUNK), SG_CHUNK ** -0.5),
        'sg_b_s': 1.0 + nrm(13, (L, SG_GROUPS, SG_CHUNK), 0.02),
        'ret_norm_g': 1.0 + nrm(14, (L, RET_WIDTH), 0.02),
        'branch_proj': nrm(15, (L, N_BRANCH, SC_WIDTH, D), SC_WIDTH ** -0.5 * DEEPNORM_BETA),
        'w_out': nrm(16, (L, D, D), D ** -0.5 * DEEPNORM_BETA),
        'ln_mix_g': 1.0 + nrm(17, (L, D), 0.02),
        'ln_mix_b': nrm(18, (L, D), 0.02),
        'router_group_w': nrm(19, (L, D, N_GROUPS), D ** -0.5),
        'router_group_b': nrm(20, (L, N_GROUPS), 0.01),
        'router_expert_w': nrm(21, (L, D, N_EXPERTS), D ** -0.5),
        'router_expert_b': nrm(22, (L, N_EXPERTS), 0.01),
        'exp_w_gate': nrm(23, (L, N_EXPERTS, D, D_EXPERT), D ** -0.5),
        'exp_w_up': nrm(24, (L, N_EXPERTS, D, D_EXPERT), D ** -0.5),
        'exp_w_down': nrm(25, (L, N_EXPERTS, D_EXPERT, D), D_EXPERT ** -0.5 * DEEPNORM_BETA),
        'ln_ffn_g': 1.0 + nrm(26, (L, D), 0.02),
        'ln_ffn_b': nrm(27, (L, D), 0.02),
    }


def reference(x, positions, w_in, sc_conv_w, sc_conv_b, lru_conv_w, lru_conv_b, lru_w_r, lru_b_r,
              lru_w_i, lru_b_i, lru_lambda, sg_norm_g, sg_w_s, sg_b_s, ret_norm_g, branch_proj, w_out,
              ln_mix_g, ln_mix_b, router_group_w, router_group_b, router_expert_w, router_expert_b,
              exp_w_gate, exp_w_up, exp_w_down, ln_ffn_g, ln_ffn_b):
    dt = x.dtype
    cos, sin = _rope_tables(positions)
    widths = [SC_WIDTH] * 3 + [LRU_WIDTH] + [SG_WIDTH] * 2 + [RET_WIDTH] * 4 + [D_MODEL] * N_BRANCH
    split_at = [int(s) for s in np.cumsum(widths)[:-1]]
    h = x
    for l in range(DEPTH):
        z = h @ w_in[l]
        (sc_b, sc_c, sc_x, lru_x, sg_u, sg_v, r_q, r_k, r_v, r_g, *gate_logits) = jnp.split(z, split_at, axis=-1)
        branches = (
            _short_conv_mixer(sc_b, sc_c, sc_x, sc_conv_w[l], sc_conv_b[l]),
            _rg_lru_mixer(lru_x, lru_conv_w[l], lru_conv_b[l], lru_w_r[l], lru_b_r[l],
                          lru_w_i[l], lru_b_i[l], lru_lambda[l]),
            _chunked_spatial_gating(sg_u, sg_v, sg_norm_g[l], sg_w_s[l], sg_b_s[l]),
            _retention(r_q, r_k, r_v, r_g, ret_norm_g[l], cos, sin),
        )
        merged = jnp.zeros(h.shape, dt)
        for b_idx in range(N_BRANCH):
            merged = merged + (jax.nn.sigmoid(gate_logits[b_idx]) * (branches[b_idx] @ branch_proj[l, b_idx])).astype(dt)
        mix = (merged @ w_out[l]).astype(dt)
        h = _layer_norm(DEEPNORM_ALPHA * h + mix, ln_mix_g[l], ln_mix_b[l])
        ffn = _hier_moe(h, router_group_w[l], router_group_b[l], router_expert_w[l], router_expert_b[l],
                        exp_w_gate[l], exp_w_up[l], exp_w_down[l])
        h = _layer_norm(DEEPNORM_ALPHA * h + ffn, ln_ffn_g[l], ln_ffn_b[l])
    return h
```

```python
import numpy as np
from contextlib import ExitStack
import ml_dtypes
import concourse.bass as bass
import concourse.mybir as mybir
from concourse.bass_utils import run_bass_kernel_spmd

F32 = mybir.dt.float32
BF16 = mybir.dt.bfloat16
I32 = mybir.dt.int32
AF = mybir.ActivationFunctionType
ALU = mybir.AluOpType
AX = mybir.AxisListType

NCORES = 8
D = 1024
TOK = 2048
NCH = TOK // 128
NT = TOK // 512
GAMMA = [1.0 - 2.0 ** (-5.0 - h) for h in range(4)]


class Buf:
    __slots__ = ("w", "r", "name")

    def __init__(self, name=""):
        self.w = None
        self.r = {}
        self.name = name


class Chan:
    __slots__ = ("key", "cnt")

    def __init__(self, key):
        self.key = key
        self.cnt = 0


class Prog:
    ENG = ("pe", "act", "dve", "pool", "sp")

    def __init__(self, nc, stack):
        self.nc = nc
        self.stack = stack
        self.q = {e: [] for e in self.ENG}
        self.sems = {}
        for e in self.ENG:
            self.sems[e] = stack.enter_context(nc.semaphore("s_" + e))
        self.cnt = {e: 0 for e in self.ENG}
        self.seen = {e: {} for e in self.ENG}
        self.nchan = 0

    def chan(self):
        key = "c%d" % self.nchan
        self.nchan += 1
        self.sems[key] = self.stack.enter_context(self.nc.semaphore("s_" + key))
        return Chan(key)

    def _deps(self, eng, reads, writes, extra=()):
        need = {}

        def add(sp):
            if sp is None:
                return
            k, v = sp
            if need.get(k, 0) < v:
                need[k] = v

        for b in reads:
            add(b.w)
        for b in writes:
            add(b.w)
            for k, v in b.r.items():
                add((k, v))
        for sp in extra:
            add(sp)
        waits = []
        for k, v in need.items():
            if k == eng and eng == "pe":
                continue
            if self.seen[eng].get(k, 0) < v:
                self.seen[eng][k] = v
                waits.append((k, v))
        return waits

    def op(self, eng, fn, reads=(), writes=()):
        waits = self._deps(eng, reads, writes)
        self.cnt[eng] += 1
        v = self.cnt[eng]
        self.q[eng].append((waits, fn, eng, 1))
        for b in reads:
            if b.r.get(eng, 0) < v:
                b.r[eng] = v
        for b in writes:
            b.w = (eng, v)
            b.r = {}

    def dma(self, qeng, chan, out, in_, reads=(), writes=(), **kw):
        prev = (chan.key, chan.cnt) if chan.cnt else None
        waits = self._deps(qeng, reads, writes, extra=(prev,) if prev else ())
        chan.cnt += 16
        v = chan.cnt

        def fn(e, out=out, in_=in_, kw=kw):
            return e.dma_start(out=out, in_=in_, **kw)

        self.q[qeng].append((waits, fn, chan.key, 16))
        for b in reads:
            b.r[chan.key] = v
        for b in writes:
            b.w = (chan.key, v)
            b.r = {}

    def dma_group(self, qeng, chan, pairs, reads=(), writes=(), **kw):
        prev = (chan.key, chan.cnt) if chan.cnt else None
        waits = self._deps(qeng, reads, writes, extra=(prev,) if prev else ())
        for i, (out, in_) in enumerate(pairs):
            chan.cnt += 16

            def fn(e, out=out, in_=in_, kw=kw):
                return e.dma_start(out=out, in_=in_, **kw)

            self.q[qeng].append((waits if i == 0 else [], fn, chan.key, 16))
        v = chan.cnt
        for b in reads:
            b.r[chan.key] = v
        for b in writes:
            b.w = (chan.key, v)
            b.r = {}

    def barrier(self, chans=()):
        for e in self.ENG:
            waits = []
            for k in self.ENG:
                v = self.cnt[k]
                if k != e and v and self.seen[e].get(k, 0) < v:
                    self.seen[e][k] = v
                    waits.append((k, v))
            for ch in chans:
                if ch.cnt and self.seen[e].get(ch.key, 0) < ch.cnt:
                    self.seen[e][ch.key] = ch.cnt
                    waits.append((ch.key, ch.cnt))
            self.q[e].append((waits, None, None, 0))

    def wait_all(self, eng, bufs):
        waits = self._deps(eng, bufs, bufs)
        self.q[eng].append((waits, None, None, 0))

    def emit(self, block):
        sems = self.sems

        def run(e, items):
            for waits, fn, key, inc in items:
                for k, v in waits:
                    e.wait_ge(sems[k], v)
                if fn is not None:
                    fn(e).then_inc(sems[key], inc)

        @block.tensor
        def _(e):
            run(e, self.q["pe"])

        @block.scalar
        def _(e):
            run(e, self.q["act"])

        @block.vector
        def _(e):
            run(e, self.q["dve"])

        @block.gpsimd
        def _(e):
            run(e, self.q["pool"])

        @block.sync
        def _(e):
            run(e, self.q["sp"])

        self.q = {e: [] for e in self.ENG}


class Ctx:
    def __init__(self, nc, st):
        self.nc, self.st = nc, st
        self.P = Prog(nc, st)
        self.banks = [st.enter_context(nc.psum_tensor("bank%d" % i, [128, 512], F32)) for i in range(8)]
        self.bbank = [Buf("bank%d" % i) for i in range(8)]
        self.rr = 0

    def sb(self, name, shape, dt):
        return self.st.enter_context(self.nc.sbuf_tensor("sb_" + name, shape, dt))

    def din(self, name, shape, dt):
        return self.nc.dram_tensor(name, list(shape), dt, kind="ExternalInput").ap()

    def dout(self, name, shape, dt):
        return self.nc.dram_tensor(name, list(shape), dt, kind="ExternalOutput").ap()

    def bank(self, lo=0, hi=8):
        n = hi - lo
        i = lo + (self.rr % n)
        self.rr += 1
        return self.banks[i], self.bbank[i]


def consts_np():
    c = {}
    c["ident_f"] = np.eye(128, dtype=np.float32)
    c["ident_b"] = np.eye(128, dtype=np.float32).astype(ml_dtypes.bfloat16)
    rot = np.zeros((128, 128), np.float32)
    for h in range(2):
        for d in range(32):
            rot[h * 64 + d + 32, h * 64 + d] = -1.0
            rot[h * 64 + d, h * 64 + d + 32] = 1.0
    c["rot_b"] = rot.astype(ml_dtypes.bfloat16)
    half = 32
    invf = (10000.0 ** (-np.arange(half, dtype=np.float32) / half)).astype(np.float32)
    c["invf"] = np.tile(invf, 4).reshape(128, 1).astype(np.float32)
    lg = np.log1p(-np.exp2(-5.0 - np.arange(4, dtype=np.float64)))
    pos = np.arange(128, dtype=np.float64)
    kdec = np.exp((127.0 - pos)[:, None] * lg[None, :])
    c["kdec"] = np.repeat(kdec, 64, axis=1).astype(np.float32)
    cd = np.exp(128.0 * lg)
    c["cd"] = np.stack([np.repeat(cd[0:2], 64), np.repeat(cd[2:4], 64)], 1).astype(np.float32)
    return c


def build_p1(debug=False):
    nc = bass.Bass("TRN2", target_bir_lowering=False)
    with ExitStack() as st:
        C = Ctx(nc, st)
        P = C.P
        h_d = C.din("h", [TOK, D], F32)
        halo_d = C.din("halo", [4, D], F32)
        pos_d = C.din("pos", [1, TOK], I32)
        w_d = C.din("w_p1", [D, 1024], F32)
        cw_d = C.din("lru_conv_w", [4, 256], F32)
        cb_d = C.din("lru_conv_b", [1, 256], F32)
        wr_d = C.din("lru_w_r", [4, 64, 64], F32)
        wi_d = C.din("lru_w_i", [4, 64, 64], F32)
        br_d = C.din("lru_b_r", [1, 256], F32)
        bi_d = C.din("lru_b_i", [1, 256], F32)
        lam_d = C.din("lru_lambda", [1, 256], F32)
        identf_d = C.din("ident_f", [128, 128], F32)
        identb_d = C.din("ident_b", [128, 128], BF16)
        rot_d = C.din("rot_b", [128, 128], BF16)
        invf_d = C.din("invf", [128, 1], F32)
        kdec_d = C.din("kdec", [128, 256], F32)
        cd_d = C.din("cd", [128, 2], F32)
        o_hloc = C.dout("o_hloc", [2, 128, TOK], F32)
        o_P = C.dout("o_P", [2, 128, TOK], F32)
        o_qT = C.dout("o_qT", [2, 128, TOK], BF16)
        o_kT = C.dout("o_kT", [2, 128, TOK], BF16)
        o_v = C.dout("o_v", [NCH, 128, 256], BF16)
        o_kv = C.dout("o_kv", [NCH, 128, 128], F32)
        o_end = C.dout("o_end", [128, 4 + 128], F32)
        hT = C.sb("hT", [128, 8, TOK], BF16)
        bhT = [Buf("hT%d" % c) for c in range(NCH)]
        haloT = C.sb("haloT", [128, 8, 128], BF16)
        bhaloT = Buf("haloT")
        w_sb = C.sb("w_sb", [128, 8, 1024], BF16)
        bw = Buf("w")
        identf = C.sb("identf", [128, 128], F32)
        identb = C.sb("identb", [128, 128], BF16)
        rot = C.sb("rot", [128, 128], BF16)
        invf = C.sb("invf", [128, 1], F32)
        kdec = C.sb("kdec", [128, 256], F32)
        cd = C.sb("cd", [128, 2], F32)
        bconst = Buf("const")
        hin = [C.sb("hin%d" % i, [128, D], F32) for i in range(2)]
        bhin = [Buf("hin%d" % i) for i in range(2)]
        chin = [P.chan() for _ in range(2)]
        halo_sb = hin[1]
        posb = C.sb("posb", [128, TOK], I32)
        posf = C.sb("posf", [128, TOK], F32)
        bpos = Buf("pos")
        cw = C.sb("cw", [128, 2, 4], F32)
        cb = C.sb("cb", [128, 2], F32)
        brs = C.sb("brs", [128, 2], F32)
        bis = C.sb("bis", [128, 2], F32)
        lam = C.sb("lam", [128, 2], F32)
        cexp = C.sb("cexp", [128, 2], F32)
        cexp2 = C.sb("cexp2", [128, 2], F32)
        bdf = C.sb("bdf", [128, 2, 2, 128], F32)
        bd = C.sb("bd", [128, 2, 2, 128], BF16)
        bpar = Buf("par")
        cst = P.chan()
        cst2 = P.chan()
        for dst, src in ((identf, identf_d), (identb, identb_d), (rot, rot_d), (invf, invf_d), (kdec, kdec_d), (cd, cd_d)):
            P.dma("sp", cst, dst[:], src, writes=[bconst])
        wst = [C.sb("wst%d" % i, [128, 1024], F32) for i in range(2)]
        bwst = [Buf("wst%d" % i) for i in range(2)]
        cwst = [P.chan() for _ in range(2)]
        bwk = [Buf("w%d" % k) for k in range(8)]
        for k in range(8):
            P.dma("act", cwst[k % 2], wst[k % 2][:], w_d[k * 128:(k + 1) * 128, :], writes=[bwst[k % 2]])
            P.op("pool", lambda e, k=k: e.tensor_copy(out=w_sb[:, k, :], in_=wst[k % 2][:]), reads=[bwst[k % 2]], writes=[bwk[k], bw])
        P.dma("sp", cst, posb[:], pos_d.partition_broadcast(128), writes=[bpos])
        P.op("dve", lambda e: e.tensor_copy(out=posf[:], in_=posb[:]), reads=[bpos], writes=[bpos])
        for c_ in range(2):
            P.dma("sp", cst, cw[:, c_, :], cw_d[:, c_ * 128:(c_ + 1) * 128].rearrange("j p -> p j"), writes=[bpar],
                  allow_slow_non_contiguous=True)
        for dst, src in ((cb, cb_d), (brs, br_d), (bis, bi_d), (lam, lam_d)):
            P.dma("sp", cst, dst[:], src.rearrange("o (c p) -> p (o c)", p=128), writes=[bpar], allow_slow_non_contiguous=True)
        P.op("pool", lambda e: e.memset(bdf[:], 0.0), writes=[bpar])
        for gi, src in enumerate((wr_d, wi_d)):
            for hh in range(4):
                ch, lo = hh // 2, (hh % 2) * 64
                P.dma("sp", cst, bdf[lo:lo + 64, gi, ch, lo:lo + 64], src[hh], writes=[bpar])
        P.op("dve", lambda e: e.tensor_copy(out=bd[:], in_=bdf[:]), reads=[bpar], writes=[bpar])
        P.op("act", lambda e: e.activation(out=cexp[:], in_=lam[:], func=AF.Exp, scale=-1.0), reads=[bpar], writes=[bpar])
        P.op("act", lambda e: e.activation(out=cexp[:], in_=cexp[:], func=AF.Ln, bias=1.0), reads=[bpar], writes=[bpar])
        P.op("dve", lambda e: e.tensor_scalar(out=cexp2[:], in0=cexp[:], scalar1=-16.0, scalar2=None, op0=ALU.mult), reads=[bpar], writes=[bpar])
        P.op("dve", lambda e: e.tensor_scalar(out=cexp[:], in0=cexp[:], scalar1=-8.0, scalar2=None, op0=ALU.mult), reads=[bpar], writes=[bpar])

        hb16 = [C.sb("hb16_%d" % i, [128, D], BF16) for i in range(2)]
        bhb16 = [Buf("hb16_%d" % i) for i in range(2)]
        tcount = [0]

        def transpose_rows(src_sb, bsrc, nrows, dstT, bdst, col0):
            s_ = tcount[0] % 2
            tcount[0] += 1
            P.op("pool", lambda e, s_=s_: e.tensor_copy(out=hb16[s_][:], in_=src_sb[:]), reads=[bsrc], writes=[bhb16[s_]])
            bk, bbk = C.bank()
            bv = bk[:].bitcast(BF16)
            for k in range(8):
                P.op("pe", lambda e, k=k, bv=bv, s_=s_: e.transpose(
                    out=bv[:, k * 128:k * 128 + nrows], in_=hb16[s_][0:nrows, k * 128:(k + 1) * 128],
                    identity=identb[0:nrows, 0:nrows]), reads=[bhb16[s_], bconst], writes=[bbk])
            for half in range(2):
                src = bv[:, half * 512:(half + 1) * 512].rearrange("p (k n) -> p k n", k=4)[:, :, 0:nrows]
                dst = dstT[:, half * 4:half * 4 + 4, col0:col0 + nrows]
                if half == 0:
                    P.op("act", lambda e, src=src, dst=dst: e.activation(out=dst, in_=src, func=AF.Copy), reads=[bbk], writes=[bdst])
                else:
                    P.op("dve", lambda e, src=src, dst=dst: e.tensor_copy(out=dst, in_=src), reads=[bbk], writes=[bdst])

        bhalo = Buf("halo")
        bhalo = bhin[1]
        P.op("pool", lambda e: e.memset(halo_sb[:], 0.0), writes=[bhalo])
        P.dma("sp", chin[1], halo_sb[0:4, :], halo_d, writes=[bhalo])
        transpose_rows(halo_sb, bhalo, 128, haloT, bhaloT, 0)
        for c in range(NCH):
            s = c % 2
            P.dma("sp" if c % 2 == 0 else "act", chin[s], hin[s][:], h_d[c * 128:(c + 1) * 128, :], writes=[bhin[s]])
            transpose_rows(hin[s], bhin[s], 128, hT, bhT[c], c * 128)

        xl = C.sb("xl", [128, 2, 3 + 512], F32)
        bxl = Buf("xl")
        xc = C.sb("xc", [128, 2, 512], F32)
        xcb = C.sb("xcb", [128, 2, 512], BF16)
        bxc = Buf("xc")
        rg = C.sb("rg", [128, 2, 512], F32)
        ig = C.sb("ig", [128, 2, 512], F32)
        av = C.sb("av", [128, 2, 512], F32)
        uv = C.sb("uv", [128, 2, 512], F32)
        blru = Buf("lruwork")
        zeros = C.sb("zeros", [128, 512], F32)
        bzero = Buf("zeros")
        P.op("pool", lambda e: e.memset(zeros[:], 0.0), writes=[bzero])
        hloc = C.sb("hloc", [128, 2, TOK], F32)
        Pc = C.sb("Pc", [128, 2, TOK], F32)
        bscan = Buf("scan")
        ang = C.sb("ang", [128, 512], F32)
        tmpa = C.sb("tmpa", [128, 512], F32)
        tmpi = C.sb("tmpi", [128, 512], I32)
        cosT = C.sb("cosT", [128, 512], F32)
        sinT = C.sb("sinT", [128, 512], F32)
        btrig = Buf("trig")
        qk_f = C.sb("qk_f", [128, 4, 512], F32)
        qk_b = C.sb("qk_b", [128, 4, 512], BF16)
        bqk = Buf("qk")
        qkr = C.sb("qkr", [128, 4, TOK], BF16)
        bqkr = [Buf("qkr%d" % t) for t in range(NT)]
        vtok = C.sb("vtok", [128, NCH, 256], BF16)
        bvt = [Buf("vt%d" % c) for c in range(NCH)]
        kdt = C.sb("kdt", [128, 256], BF16)
        bkdt = Buf("kdt")
        kvs = C.sb("kvs", [128, NCH, 128], F32)
        bkvs = [Buf("kvs%d" % c) for c in range(NCH)]
        Sst = C.sb("Sst", [128, 128], F32)
        bS = Buf("S")
        P.op("pool", lambda e: e.memset(Sst[:], 0.0), writes=[bS])
        endt = C.sb("endt", [128, 4 + 128], F32)
        bend = Buf("end")
        TWO_PI = float(2.0 * np.pi)

        def dump_debug():
            o_dbg = C.dout("o_dbg", [10, 128, 512], F32)
            cdb = P.chan()
            bdbg = Buf("dbg")
            for i_, (t_, b_) in enumerate(((xc[:, 0, :], bxc), (rg[:, 0, :], blru), (ig[:, 0, :], blru), (av[:, 0, :], blru),
                                          (uv[:, 0, :], blru), (zeros[:], bzero), (xl[:, 0, 0:512], bxl), (xl[:, 1, 0:512], bxl),
                                          (xc[:, 1, :], bxc), (xl[:, 0, 3:515], bxl))):
                P.dma("sp", cdb, o_dbg[i_], t_, reads=[b_], writes=[bdbg])
            return bdbg
        dbg_bufs = []
        def tile_body(T):
            t0 = T * 512
            bhT_t = bhT[T * 4:T * 4 + 4]
            if T == 0:
                bk, bbk = C.bank()
                for j in range(2):
                    for k in range(8):
                        P.op("pe", lambda e, j=j, k=k, bk=bk: e.matmul(
                            bk[:, j * 4:j * 4 + 4], lhsT=w_sb[:, k, j * 128:(j + 1) * 128], rhs=haloT[:, k, 0:4],
                            start=(k == 0), stop=(k == 7)), reads=[bw, bhaloT], writes=[bbk])
                P.op("act", lambda e, bk=bk: e.activation(
                    out=xl[:, :, 0:3], in_=bk[:, 0:8].rearrange("p (j n) -> p j n", j=2)[:, :, 1:4], func=AF.Copy),
                    reads=[bbk], writes=[bxl])
            else:
                P.op("pool", lambda e: e.tensor_copy(out=xl[:, :, 0:3], in_=xl[:, :, 512:515]), reads=[bxl], writes=[bxl])
            for j in range(2):
                bk, bbk = C.bank()
                for k in range(8):
                    P.op("pe", lambda e, j=j, k=k, bk=bk: e.matmul(
                        bk[:], lhsT=w_sb[:, k, j * 128:(j + 1) * 128], rhs=hT[:, k, t0:t0 + 512],
                        start=(k == 0), stop=(k == 7)), reads=[bw] + bhT_t, writes=[bbk])
                P.op("act", lambda e, j=j, bk=bk: e.activation(out=xl[:, j, 3:515], in_=bk[:], func=AF.Copy),
                     reads=[bbk], writes=[bxl])
            for j in range(2):
                P.op("dve", lambda e, j=j: e.tensor_scalar(
                    out=xc[:, j, :], in0=xl[:, j, 0:512], scalar1=cw[:, j, 0:1], scalar2=cb[:, j:j + 1],
                    op0=ALU.mult, op1=ALU.add), reads=[bxl, bpar], writes=[bxc])
                for jj in range(1, 4):
                    P.op("dve", lambda e, j=j, jj=jj: e.scalar_tensor_tensor(
                        out=xc[:, j, :], in0=xl[:, j, jj:jj + 512], scalar=cw[:, j, jj:jj + 1], in1=xc[:, j, :],
                        op0=ALU.mult, op1=ALU.add), reads=[bxl, bpar, bxc], writes=[bxc])
            P.op("pool", lambda e: e.tensor_copy(out=xcb[:], in_=xc[:]), reads=[bxc], writes=[bxc])
            for j in range(2):
                for gi, (dst, bias) in enumerate(((rg, brs), (ig, bis))):
                    bk, bbk = C.bank()
                    P.op("pe", lambda e, j=j, gi=gi, bk=bk: e.matmul(
                        bk[:], lhsT=bd[:, gi, j, :], rhs=xcb[:, j, :], start=True, stop=True),
                        reads=[bpar, bxc], writes=[bbk])
                    P.op("act", lambda e, j=j, dst=dst, bias=bias, bk=bk: e.activation(
                        out=dst[:, j, :], in_=bk[:], func=AF.Sigmoid, bias=bias[:, j:j + 1]),
                        reads=[bbk, bpar], writes=[blru])
            for j in range(2):
                P.op("act", lambda e, j=j: e.activation(out=av[:, j, :], in_=rg[:, j, :], func=AF.Exp, scale=cexp[:, j:j + 1]),
                     reads=[blru, bpar], writes=[blru])
                P.op("act", lambda e, j=j: e.activation(out=uv[:, j, :], in_=rg[:, j, :], func=AF.Exp, scale=cexp2[:, j:j + 1]),
                     reads=[blru, bpar], writes=[blru])
                P.op("dve", lambda e, j=j: e.tensor_scalar(out=uv[:, j, :], in0=uv[:, j, :], scalar1=-1.0, scalar2=1.0,
                                                           op0=ALU.mult, op1=ALU.add), reads=[blru], writes=[blru])
                P.op("dve", lambda e, j=j: e.tensor_scalar_max(out=uv[:, j, :], in0=uv[:, j, :], scalar1=0.0),
                     reads=[blru], writes=[blru])
                P.op("act", lambda e, j=j: e.activation(out=uv[:, j, :], in_=uv[:, j, :], func=AF.Sqrt),
                     reads=[blru], writes=[blru])
                P.op("dve", lambda e, j=j: e.tensor_tensor(out=ig[:, j, :], in0=ig[:, j, :], in1=xc[:, j, :], op=ALU.mult),
                     reads=[blru, bxc], writes=[blru])
                P.op("dve", lambda e, j=j: e.tensor_tensor(out=uv[:, j, :], in0=uv[:, j, :], in1=ig[:, j, :], op=ALU.mult),
                     reads=[blru], writes=[blru])
                ini_h = 0.0 if T == 0 else hloc[:, j, t0 - 1:t0]
                ini_p = 1.0 if T == 0 else Pc[:, j, t0 - 1:t0]
                P.op("dve", lambda e, j=j, ini_h=ini_h: e.tensor_tensor_scan(
                    out=hloc[:, j, t0:t0 + 512], data0=av[:, j, :], data1=uv[:, j, :], initial=ini_h,
                    op0=ALU.mult, op1=ALU.add), reads=[blru, bscan], writes=[bscan])
                P.op("dve", lambda e, j=j, ini_p=ini_p: e.tensor_tensor_scan(
                    out=Pc[:, j, t0:t0 + 512], data0=av[:, j, :], data1=zeros[:], initial=ini_p,
                    op0=ALU.mult, op1=ALU.add), reads=[blru, bscan, bzero], writes=[bscan])

            if debug and T == debug - 1:
                dbg_bufs.append(dump_debug())
            P.op("dve", lambda e: e.tensor_scalar(out=ang[:], in0=posf[:, t0:t0 + 512], scalar1=invf[:, 0:1], scalar2=None,
                                                  op0=ALU.mult), reads=[bpos, bconst], writes=[btrig])
            for dst, shift in ((sinT, 0.0), (cosT, float(np.pi / 2))):
                P.op("dve", lambda e, shift=shift: e.tensor_scalar(out=tmpa[:], in0=ang[:], scalar1=shift, scalar2=1.0 / TWO_PI,
                                                                   op0=ALU.add, op1=ALU.mult), reads=[btrig], writes=[btrig])
                P.op("dve", lambda e: e.tensor_copy(out=tmpi[:], in_=tmpa[:]), reads=[btrig], writes=[btrig])
                P.op("dve", lambda e: e.tensor_copy(out=tmpa[:], in_=tmpi[:]), reads=[btrig], writes=[btrig])
                P.op("dve", lambda e: e.tensor_scalar(out=tmpa[:], in0=tmpa[:], scalar1=-TWO_PI, scalar2=None, op0=ALU.mult),
                     reads=[btrig], writes=[btrig])
                P.op("dve", lambda e, shift=shift: e.scalar_tensor_tensor(out=tmpa[:], in0=ang[:], scalar=shift, in1=tmpa[:],
                                                                          op0=ALU.add, op1=ALU.add), reads=[btrig], writes=[btrig])
                P.op("dve", lambda e, dst=dst: e.tensor_scalar(out=dst[:], in0=tmpa[:], scalar1=float(np.pi), scalar2=-TWO_PI,
                                                               op0=ALU.is_gt, op1=ALU.mult), reads=[btrig], writes=[btrig])
                P.op("dve", lambda e, dst=dst: e.tensor_tensor(out=tmpa[:], in0=tmpa[:], in1=dst[:], op=ALU.add),
                     reads=[btrig], writes=[btrig])
                P.op("dve", lambda e, dst=dst: e.tensor_scalar(out=dst[:], in0=tmpa[:], scalar1=float(-np.pi), scalar2=TWO_PI,
                                                               op0=ALU.is_lt, op1=ALU.mult), reads=[btrig], writes=[btrig])
                P.op("dve", lambda e, dst=dst: e.tensor_tensor(out=tmpa[:], in0=tmpa[:], in1=dst[:], op=ALU.add),
                     reads=[btrig], writes=[btrig])
                P.op("act", lambda e, dst=dst: e.activation(out=dst[:], in_=tmpa[:], func=AF.Sin), reads=[btrig], writes=[btrig])

            for i in range(4):
                col = 256 + i * 128
                bk, bbk = C.bank()
                for k in range(8):
                    P.op("pe", lambda e, k=k, col=col, bk=bk: e.matmul(
                        bk[:], lhsT=w_sb[:, k, col:col + 128], rhs=hT[:, k, t0:t0 + 512],
                        start=(k == 0), stop=(k == 7)), reads=[bw] + bhT_t, writes=[bbk])
                sc = 1.0 if i < 2 else 0.125
                P.op("act", lambda e, i=i, sc=sc, bk=bk: e.activation(out=qk_f[:, i, :], in_=bk[:], func=AF.Copy, scale=sc),
                     reads=[bbk], writes=[bqk])
            P.op("pool", lambda e: e.tensor_copy(out=qk_b[:], in_=qk_f[:]), reads=[bqk], writes=[bqk])
            for i in range(4):
                bk, bbk = C.bank()
                P.op("pe", lambda e, i=i, bk=bk: e.matmul(bk[:], lhsT=rot[:], rhs=qk_b[:, i, :], start=True, stop=True),
                     reads=[bconst, bqk], writes=[bbk])
                P.op("dve", lambda e, i=i, bk=bk: e.tensor_tensor(out=tmpa[:], in0=bk[:], in1=sinT[:], op=ALU.mult),
                     reads=[bbk, btrig], writes=[btrig])
                P.op("dve", lambda e, i=i: e.tensor_tensor(out=qk_f[:, i, :], in0=qk_f[:, i, :], in1=cosT[:], op=ALU.mult),
                     reads=[bqk, btrig], writes=[bqk])
                P.op("dve", lambda e, i=i: e.tensor_tensor(out=qkr[:, i, t0:t0 + 512], in0=qk_f[:, i, :], in1=tmpa[:], op=ALU.add),
                     reads=[bqk, btrig], writes=[bqkr[T]])

            if debug and T == debug - 1:
                o_qpre = C.dout("o_qpre", [128, 512], BF16)
                o_qrop = C.dout("o_qrop", [128, 512], BF16)
                cq = P.chan()
                bq_ = Buf("qdbg")
                P.dma("sp", cq, o_qpre, qk_b[:, 0, :], reads=[bqk], writes=[bq_])
                P.dma("sp", cq, o_qrop, qkr[:, 0, t0:t0 + 512], reads=[bqkr[T]], writes=[bq_])
                dbg_bufs.append(bq_)
            def chunk_body(c):
                c0 = c * 128
                bk, bbk = C.bank()
                for k in range(8):
                    P.op("pe", lambda e, k=k, c0=c0, bk=bk: e.matmul(
                        bk[:, 0:256], lhsT=hT[:, k, c0:c0 + 128], rhs=w_sb[:, k, 768:1024],
                        start=(k == 0), stop=(k == 7)), reads=[bw, bhT[c]], writes=[bbk])
                P.op("act", lambda e, c=c, bk=bk: e.activation(out=vtok[:, c, :], in_=bk[:, 0:256], func=AF.Copy),
                     reads=[bbk], writes=[bvt[c]])
                bk2, bbk2 = C.bank()
                kview = bk2[:].bitcast(BF16)
                for pr in range(2):
                    P.op("pe", lambda e, pr=pr, c0=c0, kview=kview: e.transpose(
                        out=kview[:, pr * 128:(pr + 1) * 128], in_=qkr[:, 2 + pr, c0:c0 + 128], identity=identb[:]),
                        reads=[bqkr[T], bconst], writes=[bbk2])
                P.op("dve", lambda e, kview=kview: e.tensor_tensor(out=kdt[:], in0=kview[:, 0:256], in1=kdec[:], op=ALU.mult),
                     reads=[bbk2, bconst], writes=[bkdt])
                bk3, bbk3 = C.bank()
                for pr in range(2):
                    P.op("pe", lambda e, pr=pr, c=c, bk3=bk3: e.matmul(
                        bk3[:, pr * 128:(pr + 1) * 128], lhsT=kdt[:, pr * 128:(pr + 1) * 128],
                        rhs=vtok[:, c, pr * 128:(pr + 1) * 128], start=True, stop=True),
                        reads=[bkdt, bvt[c]], writes=[bbk3])
                for pr in range(2):
                    for hh in range(2):
                        lo = hh * 64
                        P.op("act", lambda e, pr=pr, lo=lo, c=c, bk3=bk3: e.activation(
                            out=kvs[lo:lo + 64, c, pr * 64:(pr + 1) * 64],
                            in_=bk3[lo:lo + 64, pr * 128 + lo:pr * 128 + lo + 64], func=AF.Copy),
                            reads=[bbk3], writes=[bkvs[c]])
                for pr in range(2):
                    P.op("dve", lambda e, pr=pr, c=c: e.scalar_tensor_tensor(
                        out=Sst[:, pr * 64:(pr + 1) * 64], in0=Sst[:, pr * 64:(pr + 1) * 64], scalar=cd[:, pr:pr + 1],
                        in1=kvs[:, c, pr * 64:(pr + 1) * 64], op0=ALU.mult, op1=ALU.add),
                        reads=[bS, bconst, bkvs[c]], writes=[bS])

            for cc in range(4):
                chunk_body(T * 4 + cc)

        for T_ in range(NT):
            tile_body(T_)

        for j in range(2):
            P.op("act", lambda e, j=j: e.activation(out=endt[:, j:j + 1], in_=Pc[:, j, TOK - 1:TOK], func=AF.Copy),
                 reads=[bscan], writes=[bend])
            P.op("act", lambda e, j=j: e.activation(out=endt[:, 2 + j:3 + j], in_=hloc[:, j, TOK - 1:TOK], func=AF.Copy),
                 reads=[bscan], writes=[bend])
        P.op("act", lambda e: e.activation(out=endt[:, 4:132], in_=Sst[:], func=AF.Copy), reads=[bS], writes=[bend])
        bo = Buf("outs")
        co = [P.chan() for _ in range(4)]
        P.dma("sp", co[0], o_hloc.rearrange("j p t -> p j t"), hloc[:], reads=[bscan], writes=[bo])
        P.dma("act", co[1], o_P.rearrange("j p t -> p j t"), Pc[:], reads=[bscan], writes=[bo])
        P.dma("sp", co[2], o_qT.rearrange("j p t -> p j t"), qkr[:, 0:2, :], reads=bqkr, writes=[bo])
        P.dma("act", co[3], o_kT.rearrange("j p t -> p j t"), qkr[:, 2:4, :], reads=bqkr, writes=[bo])
        P.dma("sp", co[0], o_v.rearrange("c p n -> p c n"), vtok[:], reads=bvt, writes=[bo])
        P.dma("act", co[1], o_kv.rearrange("c p n -> p c n"), kvs[:], reads=bkvs, writes=[bo])
        P.dma("sp", co[2], o_end, endt[:], reads=[bend], writes=[bo])
        if debug:
            tD = (debug - 1) * 512
            bk, bbk = C.bank()
            for k in range(8):
                P.op("pe", lambda e, k=k, bk=bk: e.matmul(bk[:], lhsT=w_sb[:, k, 0:128], rhs=hT[:, k, tD:tD + 512],
                                                          start=(k == 0), stop=(k == 7)), reads=[bw] + bhT, writes=[bbk])
            P.op("act", lambda e, bk=bk: e.activation(out=ang[:], in_=bk[:], func=AF.Copy), reads=[bbk], writes=[btrig])
            o_late = C.dout("o_late", [128, 512], F32)
            o_hT = C.dout("o_hT", [128, 8, 512], BF16)
            o_w = C.dout("o_w", [128, 8, 128], BF16)
            cl = P.chan()
            bl = Buf("late")
            P.dma("sp", cl, o_late, ang[:], reads=[btrig], writes=[bl])
            P.dma("sp", cl, o_hT, hT[:, :, tD:tD + 512], reads=bhT, writes=[bl])
            P.dma("sp", cl, o_w, w_sb[:, :, 0:128], reads=[bw], writes=[bl])
            dbg_bufs.append(bl)
        P.wait_all("sp", [bo] + dbg_bufs)
        with nc.Block() as block:
            P.emit(block)
    return nc


ALPHA = float((2.0 * 2) ** 0.25)
LN_EPS = 1e-5


def consts2_np():
    c = {}
    lg = np.log1p(-np.exp2(-5.0 - np.arange(4, dtype=np.float64)))
    m = np.arange(128)[:, None].astype(np.float64)
    n = np.arange(128)[None, :].astype(np.float64)
    dec = np.zeros((128, 4, 128), np.float64)
    for h in range(4):
        dec[:, h, :] = np.where(n >= m, np.exp(np.maximum(n - m, 0.0) * lg[h]), 0.0)
    c["decayT"] = dec.astype(np.float32)
    qd = np.zeros((128, 2, 128), np.float64)
    cdT = np.zeros((128, 2), np.float64)
    for pr in range(2):
        for hh in range(2):
            h = pr * 2 + hh
            qd[hh * 64:(hh + 1) * 64, pr, :] = np.exp((np.arange(128) + 1.0) * lg[h])[None, :]
            cdT[hh * 64:(hh + 1) * 64, pr] = np.exp(2048.0 * lg[h])
    c["qdecT"] = qd.astype(np.float32)
    c["cdT"] = cdT.astype(np.float32)
    c["causal"] = np.tril(np.ones((128, 128), np.float32))
    return c


def sel_np(core):
    s = np.zeros((128, 8), np.float32)
    for j in range(8):
        if j // 4 == core // 4 and j < core:
            s[:, j] = 1.0
    return s


def build_p2(stage="full", lvl=9):
    nc = bass.Bass("TRN2", target_bir_lowering=False)
    with ExitStack() as st:
        C = Ctx(nc, st)
        P = C.P
        h_d = C.din("h", [TOK, D], F32)
        halo_d = C.din("halo", [4, D], F32)
        hloc_d = C.din("hloc", [2, 128, TOK], F32)
        Pc_d = C.din("Pc", [2, 128, TOK], F32)
        qT_d = C.din("qT", [2, 128, TOK], BF16)
        kT_d = C.din("kT", [2, 128, TOK], BF16)
        v_d = C.din("v", [NCH, 128, 256], BF16)
        kv_d = C.din("kv", [NCH, 128, 128], F32)
        ends_d = C.din("ends_all", [8, 128, 132], F32)
        sel_d = C.din("sel", [128, 8], F32)
        win_d = C.din("w_in", [D, 6656], F32)
        scw_d = C.din("sc_conv_w", [3, 256], F32)
        scb_d = C.din("sc_conv_b", [1, 256], F32)
        sgn_d = C.din("sg_norm_g", [1, 256], F32)
        sgw_d = C.din("sg_w_s", [4, 128, 128], F32)
        sgb_d = C.din("sg_b_s", [4, 128], F32)
        rng_d = C.din("ret_norm_g", [1, 256], F32)
        bp_d = C.din("branch_proj", [4, 256, D], F32)
        wout_d = C.din("w_out", [D, D], F32)
        lmg_d = C.din("ln_mix_g", [1, D], F32)
        lmb_d = C.din("ln_mix_b", [1, D], F32)
        rgw_d = C.din("router_group_w", [D, 4], F32)
        rgb_d = C.din("router_group_b", [1, 4], F32)
        rew_d = C.din("router_expert_w", [D, 32], F32)
        reb_d = C.din("router_expert_b", [1, 32], F32)
        if stage == "full":
            ewg_d = C.din("exp_w_gate", [32, D, 256], F32)
            ewu_d = C.din("exp_w_up", [32, D, 256], F32)
            ewd_d = C.din("exp_w_down", [32, 256, D], F32)
            lfg_d = C.din("ln_ffn_g", [1, D], F32)
            lfb_d = C.din("ln_ffn_b", [1, D], F32)
        identf_d = C.din("ident_f", [128, 128], F32)
        identb_d = C.din("ident_b", [128, 128], BF16)
        decayT_d = C.din("decayT", [128, 4, 128], F32)
        qdecT_d = C.din("qdecT", [128, 2, 128], F32)
        cd_d = C.din("cd", [128, 2], F32)
        cdT_d = C.din("cdT", [128, 2], F32)
        causal_d = C.din("causal", [128, 128], F32)
        o_h = C.dout("o_h", [TOK, D], F32)
        o_dbg = C.dout("o_dbg", [TOK, 32], F32) if stage != "full" else None
        o_brT = C.dout("o_brT", [8, 128, TOK], BF16) if stage != "full" else None

        hres = C.sb("hres", [128, NCH, D], F32)
        bhres = [Buf("hres%d" % c) for c in range(NCH)]
        hT = C.sb("hT", [128, 8, TOK], BF16)
        bhT = [Buf("hT%d" % c) for c in range(NCH)]
        identf = C.sb("identf", [128, 128], F32)
        identb = C.sb("identb", [128, 128], BF16)
        comb = C.sb("comb", [128, NCH, 32], F32)
        bcomb = [Buf("comb%d" % c) for c in range(NCH)]
        bconst = Buf("const")
        cst = P.chan()
        P.dma("sp", cst, identf[:], identf_d, writes=[bconst])
        P.dma("sp", cst, identb[:], identb_d, writes=[bconst])
        allchans = [cst]

        def newchan():
            ch = P.chan()
            allchans.append(ch)
            return ch

        with ExitStack() as stA:
            def sbA(name, shape, dt):
                return stA.enter_context(nc.sbuf_tensor("sa_" + name, shape, dt))

            haloT = sbA("haloT", [128, 8, 4], BF16)
            bhaloT = Buf("haloT")
            hb16 = [sbA("hb16_%d" % i, [128, D], BF16) for i in range(2)]
            bhb16 = [Buf("hb16_%d" % i) for i in range(2)]
            tcount = [0]

            def transpose_rows(src_ap, bsrc, dstT, bdst, col0, ncols=128):
                s_ = tcount[0] % 2
                tcount[0] += 1
                P.op("pool", lambda e: e.tensor_copy(out=hb16[s_][:], in_=src_ap), reads=[bsrc], writes=[bhb16[s_]])
                bk, bbk = C.bank()
                bv = bk[:].bitcast(BF16)
                for k in range(8):
                    P.op("pe", lambda e, k=k: e.transpose(out=bv[:, k * 128:(k + 1) * 128], in_=hb16[s_][:, k * 128:(k + 1) * 128],
                                                          identity=identb[:]), reads=[bhb16[s_], bconst], writes=[bbk])
                for half in range(2):
                    src = bv[:, half * 512:(half + 1) * 512].rearrange("p (k n) -> p k n", k=4)[:, :, 0:ncols]
                    dst = dstT[:, half * 4:half * 4 + 4, col0:col0 + ncols]
                    if half == 0:
                        P.op("act", lambda e, src=src, dst=dst: e.activation(out=dst, in_=src, func=AF.Copy), reads=[bbk], writes=[bdst])
                    else:
                        P.op("dve", lambda e, src=src, dst=dst: e.tensor_copy(out=dst, in_=src), reads=[bbk], writes=[bdst])

            decayT = sbA("decayT", [128, 4, 128], F32)
            qdecT = sbA("qdecT", [128, 2, 128], F32)
            cd = sbA("cd", [128, 2], F32)
            cdT = sbA("cdT", [128, 2], F32)
            causal = sbA("causal", [128, 128], F32)
            sel = sbA("sel", [128, 8], F32)
            ends = sbA("ends", [128, 8, 132], F32)
            for dst, src in ((decayT, decayT_d), (qdecT, qdecT_d), (cd, cd_d), (cdT, cdT_d), (causal, causal_d), (sel, sel_d)):
                P.dma("sp", cst, dst[:], src, writes=[bconst])
            P.dma("sp", cst, ends[:], ends_d.rearrange("r p n -> p r n"), writes=[bconst])
            scw = sbA("scw", [128, 2, 3], F32)
            scb = sbA("scb", [128, 2], F32)
            bpar = Buf("par")
            for c_ in range(2):
                P.dma("sp", cst, scw[:, c_, :], scw_d[:, c_ * 128:(c_ + 1) * 128].rearrange("j p -> p j"), writes=[bpar],
                      allow_slow_non_contiguous=True)
            P.dma("sp", cst, scb[:], scb_d.rearrange("o (c p) -> p (o c)", p=128), writes=[bpar], allow_slow_non_contiguous=True)
            sgn = sbA("sgn", [128, 256], F32)
            rng_ = sbA("rng", [128, 256], F32)
            lmg = sbA("lmg", [128, D], F32)
            lmb = sbA("lmb", [128, D], F32)
            rbias = sbA("rbias", [128, 36], F32)
            P.dma("sp", cst, sgn[:], sgn_d.partition_broadcast(128), writes=[bpar])
            P.dma("sp", cst, rng_[:], rng_d.partition_broadcast(128), writes=[bpar])
            P.dma("sp", cst, lmg[:], lmg_d.partition_broadcast(128), writes=[bpar])
            P.dma("sp", cst, lmb[:], lmb_d.partition_broadcast(128), writes=[bpar])
            P.dma("sp", cst, rbias[:, 0:4], rgb_d.partition_broadcast(128), writes=[bpar])
            P.dma("sp", cst, rbias[:, 4:36], reb_d.partition_broadcast(128), writes=[bpar])
            wr = sbA("wr", [128, 8, 36], F32)
            P.dma("sp", cst, wr[:, :, 0:4], rgw_d.rearrange("(k p) n -> p k n", p=128), writes=[bpar])
            P.dma("sp", cst, wr[:, :, 4:36], rew_d.rearrange("(k p) n -> p k n", p=128), writes=[bpar])
            bsb = sbA("bsb", [128, 2, 128], F32)
            for g in range(4):
                lo = (g % 2) * 64
                P.dma("sp", cst, bsb[lo:lo + 64, g // 2, :], sgb_d[g:g + 1, :].partition_broadcast(64), writes=[bpar])
            WcT = sbA("WcT", [128, 4, 128], BF16)
            wtmp = sbA("wtmp", [128, 128], F32)
            wtmpb = sbA("wtmpb", [128, 128], BF16)
            bwtmp = Buf("wtmp")
            for g in range(4):
                P.dma("sp", cst, wtmp[:], sgw_d[g], writes=[bwtmp])
                P.op("dve", lambda e: e.tensor_tensor(out=wtmpb[:], in0=wtmp[:], in1=causal[:], op=ALU.mult),
                     reads=[bwtmp, bconst], writes=[bwtmp])
                bk, bbk = C.bank()
                bv = bk[:].bitcast(BF16)
                P.op("pe", lambda e, bv=bv: e.transpose(out=bv[:, 0:128], in_=wtmpb[:], identity=identb[:]),
                     reads=[bwtmp, bconst], writes=[bbk])
                P.op("act", lambda e, bv=bv, g=g: e.activation(out=WcT[:, g, :], in_=bv[:, 0:128], func=AF.Copy),
                     reads=[bbk], writes=[bpar])

            hin_l = sbA("hin_l", [128, 2], F32)
            Sst = sbA("Sst", [128, 128], F32)
            Sb = sbA("Sb", [128, 128], BF16)
            ta = sbA("ta", [128, 2], F32)
            tb = sbA("tb", [128, 2], F32)
            tS = sbA("tS", [128, 128], F32)
            oms = sbA("oms", [128, 8], F32)
            bS = Buf("S")
            bcmb = Buf("cmb")
            P.op("pool", lambda e: e.memset(hin_l[:], 0.0), writes=[bcmb])
            P.op("pool", lambda e: e.memset(Sst[:], 0.0), writes=[bS])
            P.op("dve", lambda e: e.tensor_scalar(out=oms[:], in0=sel[:], scalar1=-1.0, scalar2=1.0, op0=ALU.mult, op1=ALU.add),
                 reads=[bconst], writes=[bcmb])
            for j in range(8):
                P.op("dve", lambda e, j=j: e.tensor_scalar(out=ta[:], in0=ends[:, j, 0:2], scalar1=sel[:, j:j + 1], scalar2=oms[:, j:j + 1],
                                                           op0=ALU.mult, op1=ALU.add), reads=[bconst, bcmb], writes=[bcmb])
                P.op("dve", lambda e, j=j: e.tensor_scalar(out=tb[:], in0=ends[:, j, 2:4], scalar1=sel[:, j:j + 1], scalar2=None,
                                                           op0=ALU.mult), reads=[bconst, bcmb], writes=[bcmb])
                P.op("dve", lambda e: e.tensor_tensor(out=hin_l[:], in0=hin_l[:], in1=ta[:], op=ALU.mult), reads=[bcmb], writes=[bcmb])
                P.op("dve", lambda e: e.tensor_tensor(out=hin_l[:], in0=hin_l[:], in1=tb[:], op=ALU.add), reads=[bcmb], writes=[bcmb])
                P.op("dve", lambda e, j=j: e.tensor_scalar(out=ta[:], in0=cdT[:], scalar1=sel[:, j:j + 1], scalar2=oms[:, j:j + 1],
                                                           op0=ALU.mult, op1=ALU.add), reads=[bconst, bcmb], writes=[bcmb])
                P.op("dve", lambda e, j=j: e.tensor_scalar(out=tS[:], in0=ends[:, j, 4:132], scalar1=sel[:, j:j + 1], scalar2=None,
                                                           op0=ALU.mult), reads=[bconst, bcmb], writes=[bcmb])
                for pr in range(2):
                    P.op("dve", lambda e, pr=pr: e.scalar_tensor_tensor(
                        out=Sst[:, pr * 64:(pr + 1) * 64], in0=Sst[:, pr * 64:(pr + 1) * 64], scalar=ta[:, pr:pr + 1],
                        in1=tS[:, pr * 64:(pr + 1) * 64], op0=ALU.mult, op1=ALU.add), reads=[bS, bcmb], writes=[bS])
            P.op("act", lambda e: e.activation(out=Sb[:], in_=Sst[:], func=AF.Copy), reads=[bS], writes=[bS])

            chh = [newchan() for _ in range(2)]
            h1 = sbA("h1", [128, D], F32)
            bh1 = Buf("h1")
            halo_sb = h1
            bhalo = bh1
            P.op("pool", lambda e: e.memset(halo_sb[:], 0.0), writes=[bhalo])
            P.dma("sp", chh[0], halo_sb[0:4, :], halo_d, writes=[bhalo])
            transpose_rows(halo_sb[:], bhalo, haloT, bhaloT, 0, ncols=4)
            for c in range(NCH):
                P.dma("sp" if c % 2 == 0 else "act", chh[c % 2], hres[:, c, :], h_d[c * 128:(c + 1) * 128, :], writes=[bhres[c]])
                transpose_rows(hres[:, c, :], bhres[c], hT, bhT[c], c * 128)

            NSLOT = 2
            wslot = [sbA("wslot%d" % i, [128, 8, 640], BF16) for i in range(NSLOT)]
            bws = [Buf("wslot%d" % i) for i in range(NSLOT)]
            cws = [newchan() for _ in range(NSLOT)]
            wsn = [0]

            def wload(pairs_fn):
                i = wsn[0] % NSLOT
                wsn[0] += 1
                P.dma_group("pool", cws[i], pairs_fn(wslot[i]), writes=[bws[i]])
                return wslot[i], bws[i]

            def win_cols(lo, n):
                return win_d[:, lo:lo + n].rearrange("(k p) n -> p k n", p=128)

            hl_t = sbA("hl_t", [128, 2, 512], F32)
            pc_t = sbA("pc_t", [128, 2, 512], F32)
            q_t = sbA("q_t", [128, 2, 512], BF16)
            k_t = sbA("k_t", [128, 2, 512], BF16)
            v_t = sbA("v_t", [128, 4, 256], BF16)
            kv_t = sbA("kv_t", [128, 4, 128], F32)
            bstore = Buf("store")
            cstore = newchan()

            cxl = sbA("cxl", [128, 2, 514], F32)
            bcxl = Buf("cxl")
            tmpx = sbA("tmpx", [128, 512], F32)
            accA = sbA("accA", [128, 512], F32)
            bwa = Buf("workA")
            brT = sbA("brT", [128, 8, 512], BF16)
            bbr = [Buf("br%d" % i) for i in range(8)]
            uT = sbA("uT", [128, 2, 512], F32)
            buT = Buf("uT")
            vn = sbA("vn", [128, 256], F32)
            vnb = sbA("vnb", [128, 256], BF16)
            stat = sbA("stat", [128, 4, 6], F32)
            mv = sbA("mv", [128, 4, 2], F32)
            rstd = sbA("rstd", [128, 4], F32)
            bwc = Buf("workC")
            tmpc = sbA("tmpc", [128, 128], F32)
            sm = sbA("sm", [128, 4, 128], BF16)
            qd = sbA("qd", [128, 2, 128], BF16)
            sgt = sbA("sgt", [128, 256], F32)
            on = sbA("on", [128, 256], F32)
            oraw = on
            sraw_t = hb16[0][:].bitcast(F32).rearrange("p (h n) -> p h n", h=4)
            onb = sbA("onb", [128, 256], BF16)
            bwd = Buf("workD")
            sig = tmpx
            mtmp = accA
            macc = sbA("macc", [128, 512], F32)
            bwg = bwa
            mergedT = sbA("mergedT", [128, 8, 512], BF16)
            bmg = [Buf("mg%d" % j) for j in range(8)]
            lnstat = sbA("lnstat", [128, 2, 6], F32)
            lnmv = sbA("lnmv", [128, 2], F32)
            lnr = sbA("lnr", [128, 2], F32)
            h1T = sbA("h1T", [128, 8, 128], F32)
            bln = Buf("ln")
            bh1T = Buf("h1T")
            lg = sbA("lg", [128, 36], F32)
            elm = sbA("elm", [128, 32], F32)
            top8 = sbA("top8", [128, 8], F32)
            rs = sbA("rs", [128, 16], F32)
            c1 = sbA("c1", [128, 32], F32)
            brt = Buf("router")
            cdbg = newchan()
            bdbg = Buf("dbg")

            def tile_body(T):
                t0 = T * 512
                bhT_t = bhT[T * 4:T * 4 + 4]
                if lvl < 1:
                    return
                P.dma_group("sp", cstore, [
                    (hl_t[:], hloc_d[:, :, t0:t0 + 512].rearrange("j p t -> p j t")),
                    (pc_t[:], Pc_d[:, :, t0:t0 + 512].rearrange("j p t -> p j t")),
                    (q_t[:], qT_d[:, :, t0:t0 + 512].rearrange("j p t -> p j t")),
                    (k_t[:], kT_d[:, :, t0:t0 + 512].rearrange("j p t -> p j t")),
                    (v_t[:], v_d[T * 4:T * 4 + 4].rearrange("c p n -> p c n")),
                    (kv_t[:], kv_d[T * 4:T * 4 + 4].rearrange("c p n -> p c n")),
                ], writes=[bstore])
                G0, bG0 = wload(lambda s_: [(s_[:, :, 0:512], win_cols(0, 512))])
                G1, bG1 = wload(lambda s_: [(s_[:, :, 0:256], win_cols(512, 256)), (s_[:, :, 256:512], win_cols(1024, 256))])

                def zfeat(G, bG, col, bk, bbk):
                    for k in range(8):
                        P.op("pe", lambda e, k=k: e.matmul(bk[:], lhsT=G[:, k, col:col + 128], rhs=hT[:, k, t0:t0 + 512],
                                                           start=(k == 0), stop=(k == 7)), reads=[bG] + bhT_t, writes=[bbk])

                if T == 0:
                    bkh, bbkh = C.bank()
                    for jc in range(2):
                        for wi, (G, bG, col) in enumerate(((G0, bG0, 256 + jc * 128), (G1, bG1, jc * 128))):
                            for k in range(8):
                                P.op("pe", lambda e, k=k, G=G, col=col, o_=(jc * 2 + wi) * 4: e.matmul(
                                    bkh[:, o_:o_ + 4], lhsT=G[:, k, col:col + 128], rhs=haloT[:, k, 0:4],
                                    start=(k == 0), stop=(k == 7)), reads=[bG, bhaloT], writes=[bbkh])
                    for jc in range(2):
                        P.op("act", lambda e, jc=jc: e.activation(out=tmpx[:, 0:2], in_=bkh[:, (jc * 2 + 1) * 4 + 2:(jc * 2 + 1) * 4 + 4],
                                                                  func=AF.Copy), reads=[bbkh], writes=[bwa])
                        P.op("dve", lambda e, jc=jc: e.tensor_tensor(out=cxl[:, jc, 0:2], in0=bkh[:, (jc * 2) * 4 + 2:(jc * 2) * 4 + 4],
                                                                     in1=tmpx[:, 0:2], op=ALU.mult), reads=[bbkh, bwa], writes=[bcxl])
                else:
                    P.op("pool", lambda e: e.tensor_copy(out=cxl[:, :, 0:2], in_=cxl[:, :, 512:514]), reads=[bcxl], writes=[bcxl])
                for jc in range(2):
                    bkb, bbkb = C.bank()
                    bkc, bbkc = C.bank()
                    bkx, bbkx = C.bank()
                    zfeat(G0, bG0, jc * 128, bkb, bbkb)
                    zfeat(G0, bG0, 256 + jc * 128, bkc, bbkc)
                    zfeat(G1, bG1, jc * 128, bkx, bbkx)
                    P.op("act", lambda e, bkx=bkx: e.activation(out=tmpx[:], in_=bkx[:], func=AF.Copy), reads=[bbkx], writes=[bwa])
                    P.op("dve", lambda e, jc=jc, bkc=bkc: e.tensor_tensor(out=cxl[:, jc, 2:514], in0=bkc[:], in1=tmpx[:], op=ALU.mult),
                         reads=[bbkc, bwa], writes=[bcxl])
                    P.op("dve", lambda e, jc=jc: e.tensor_scalar(out=accA[:], in0=cxl[:, jc, 0:512], scalar1=scw[:, jc, 0:1],
                                                                 scalar2=scb[:, jc:jc + 1], op0=ALU.mult, op1=ALU.add),
                         reads=[bcxl, bpar], writes=[bwa])
                    for jj in range(1, 3):
                        P.op("dve", lambda e, jc=jc, jj=jj: e.scalar_tensor_tensor(
                            out=accA[:], in0=cxl[:, jc, jj:jj + 512], scalar=scw[:, jc, jj:jj + 1], in1=accA[:],
                            op0=ALU.mult, op1=ALU.add), reads=[bcxl, bpar, bwa], writes=[bwa])
                    P.op("dve", lambda e, jc=jc, bkb=bkb: e.tensor_tensor(out=brT[:, jc, :], in0=bkb[:], in1=accA[:], op=ALU.mult),
                         reads=[bbkb, bwa], writes=[bbr[jc]])
                for j in range(2):
                    P.op("dve", lambda e, j=j: e.scalar_tensor_tensor(out=brT[:, 2 + j, :], in0=pc_t[:, j, :], scalar=hin_l[:, j:j + 1],
                                                                      in1=hl_t[:, j, :], op0=ALU.mult, op1=ALU.add),
                         reads=[bstore, bcmb], writes=[bbr[2 + j]])
                for jc in range(2):
                    bku, bbku = C.bank()
                    zfeat(G1, bG1, 256 + jc * 128, bku, bbku)
                    P.op("act", lambda e, jc=jc, bku=bku: e.activation(out=uT[:, jc, :], in_=bku[:], func=AF.Gelu_apprx_tanh),
                         reads=[bbku], writes=[buT])

                G2, bG2 = wload(lambda s_: [(s_[:, :, 0:256], win_cols(1280, 256)), (s_[:, :, 256:512], win_cols(2304, 256))])

                def chunk_body(cc):
                    c = T * 4 + cc
                    c0 = c * 128
                    l0 = cc * 128
                    bkv, bbkv = C.bank()
                    for k in range(8):
                        P.op("pe", lambda e, k=k: e.matmul(bkv[:, 0:256], lhsT=hT[:, k, c0:c0 + 128], rhs=G2[:, k, 0:256],
                                                           start=(k == 0), stop=(k == 7)), reads=[bG2, bhT[c]], writes=[bbkv])
                    P.op("act", lambda e: e.activation(out=vn[:], in_=bkv[:, 0:256], func=AF.Gelu_apprx_tanh), reads=[bbkv], writes=[bwc])
                    for g in range(4):
                        P.op("dve", lambda e, g=g: e.bn_stats(out=stat[:, g, :], in_=vn[:, g * 64:(g + 1) * 64]), reads=[bwc], writes=[bwc])
                    for g in range(4):
                        P.op("dve", lambda e, g=g: e.bn_aggr(out=mv[:, g, :], in_=stat[:, g, :]), reads=[bwc], writes=[bwc])
                    P.op("dve", lambda e: e.tensor_scalar(out=rstd[:], in0=mv[:, :, 1], scalar1=LN_EPS, scalar2=None, op0=ALU.add),
                         reads=[bwc], writes=[bwc])
                    P.op("act", lambda e: e.activation(out=rstd[:], in_=rstd[:], func=AF.Sqrt), reads=[bwc], writes=[bwc])
                    P.op("dve", lambda e: e.reciprocal(out=rstd[:], in_=rstd[:]), reads=[bwc], writes=[bwc])
                    for g in range(4):
                        P.op("dve", lambda e, g=g: e.tensor_scalar(out=vn[:, g * 64:(g + 1) * 64], in0=vn[:, g * 64:(g + 1) * 64],
                                                                   scalar1=mv[:, g, 0:1], scalar2=rstd[:, g:g + 1],
                                                                   op0=ALU.subtract, op1=ALU.mult), reads=[bwc], writes=[bwc])
                    P.op("dve", lambda e: e.tensor_tensor(out=vnb[:], in0=vn[:], in1=sgn[:], op=ALU.mult), reads=[bwc, bpar], writes=[bwc])
                    if lvl < 1.2:
                        return
                    bkc_, bbkc_ = C.bank()
                    for g in range(4):
                        lo = (g % 2) * 64
                        P.op("pe", lambda e, g=g, lo=lo: e.matmul(bkc_[lo:lo + 64, (g // 2) * 128:(g // 2) * 128 + 128],
                                                                  lhsT=vnb[:, g * 64:(g + 1) * 64], rhs=WcT[:, g, :], start=True, stop=True),
                             reads=[bwc, bpar], writes=[bbkc_])
                    if lvl < 1.3:
                        return
                    P.op("act", lambda e: e.activation(out=on[:], in_=bkc_[:, 0:256], func=AF.Copy), reads=[bbkc_, bwd], writes=[bwd])
                    for pr in range(2):
                        P.op("dve", lambda e, pr=pr: e.tensor_tensor(out=tmpc[:], in0=on[:, pr * 128:(pr + 1) * 128], in1=bsb[:, pr, :],
                                                                     op=ALU.add), reads=[bwd, bpar], writes=[bwc])
                        P.op("dve", lambda e, pr=pr: e.tensor_tensor(out=brT[:, 4 + pr, l0:l0 + 128], in0=tmpc[:], in1=uT[:, pr, l0:l0 + 128],
                                                                     op=ALU.mult), reads=[bwc, buT], writes=[bbr[4 + pr]])
                    if lvl < 1.4:
                        return
                    bks0, bbks0 = C.bank()
                    bks1, bbks1 = C.bank()
                    for h in range(4):
                        pr, hh = h // 2, h % 2
                        lo = hh * 64
                        bk_, bbk_ = (bks0, bbks0) if hh == 0 else (bks1, bbks1)
                        P.op("pe", lambda e, pr=pr, lo=lo, bk_=bk_: e.matmul(bk_[:, pr * 128:(pr + 1) * 128], lhsT=k_t[lo:lo + 64, pr, l0:l0 + 128],
                                                                             rhs=q_t[lo:lo + 64, pr, l0:l0 + 128], start=True, stop=True),
                             reads=[bstore], writes=[bbk_])
                    P.op("act", lambda e: e.activation(out=sraw_t.rearrange("p (pr hh) n -> p pr hh n", hh=2)[:, :, 0, :],
                                                       in_=bks0[:, 0:256].rearrange("p (pr n) -> p pr n", pr=2), func=AF.Copy),
                         reads=[bbks0, bwd], writes=[bwd, bhb16[0]])
                    P.op("act", lambda e: e.activation(out=sraw_t.rearrange("p (pr hh) n -> p pr hh n", hh=2)[:, :, 1, :],
                                                       in_=bks1[:, 0:256].rearrange("p (pr n) -> p pr n", pr=2), func=AF.Copy),
                         reads=[bbks1, bwd], writes=[bwd, bhb16[0]])
                    P.op("dve", lambda e: e.tensor_tensor(out=sm[:], in0=sraw_t, in1=decayT[:], op=ALU.mult),
                         reads=[bwd, bconst], writes=[bwd])
                    P.op("dve", lambda e: e.tensor_tensor(out=qd[:], in0=q_t[:, :, l0:l0 + 128], in1=qdecT[:], op=ALU.mult),
                         reads=[bstore, bconst], writes=[bwd])
                    bko0, bbko0 = C.bank()
                    bko1, bbko1 = C.bank()
                    for h in range(4):
                        pr, hh = h // 2, h % 2
                        lo = hh * 64
                        bk_, bbk_ = (bko0, bbko0) if hh == 0 else (bko1, bbko1)
                        P.op("pe", lambda e, h=h, pr=pr, bk_=bk_: e.matmul(bk_[:, pr * 64:(pr + 1) * 64], lhsT=sm[:, h, :], rhs=v_t[:, cc, h * 64:(h + 1) * 64],
                                                                           start=True, stop=False), reads=[bwd, bstore], writes=[bbk_])
                        P.op("pe", lambda e, pr=pr, lo=lo, bk_=bk_: e.matmul(bk_[:, pr * 64:(pr + 1) * 64], lhsT=qd[lo:lo + 64, pr, :],
                                                                             rhs=Sb[lo:lo + 64, pr * 64:(pr + 1) * 64], start=False, stop=True),
                             reads=[bwd, bS], writes=[bbk_])
                    P.op("act", lambda e: e.activation(out=on[:].rearrange("p (pr hh n) -> p pr hh n", pr=2, hh=2)[:, :, 0, :],
                                                       in_=bko0[:, 0:128].rearrange("p (pr n) -> p pr n", pr=2), func=AF.Copy),
                         reads=[bbko0, bwd], writes=[bwd])
                    P.op("act", lambda e: e.activation(out=on[:].rearrange("p (pr hh n) -> p pr hh n", pr=2, hh=2)[:, :, 1, :],
                                                       in_=bko1[:, 0:128].rearrange("p (pr n) -> p pr n", pr=2), func=AF.Copy),
                         reads=[bbko1, bwd], writes=[bwd])
                    if lvl < 1.6:
                        return
                    bkg, bbkg = C.bank()
                    for k in range(8):
                        P.op("pe", lambda e, k=k: e.matmul(bkg[:, 0:256], lhsT=hT[:, k, c0:c0 + 128], rhs=G2[:, k, 256:512],
                                                           start=(k == 0), stop=(k == 7)), reads=[bG2, bhT[c]], writes=[bbkg])
                    P.op("act", lambda e: e.activation(out=sgt[:], in_=bkg[:, 0:256], func=AF.Silu), reads=[bbkg], writes=[bwd])
                    for h in range(4):
                        P.op("dve", lambda e, h=h: e.bn_stats(out=stat[:, h, :], in_=oraw[:, h * 64:(h + 1) * 64]), reads=[bwd, bwc], writes=[bwc])
                    for h in range(4):
                        P.op("dve", lambda e, h=h: e.bn_aggr(out=mv[:, h, :], in_=stat[:, h, :]), reads=[bwc], writes=[bwc])
                    P.op("dve", lambda e: e.tensor_scalar(out=rstd[:], in0=mv[:, :, 1], scalar1=LN_EPS, scalar2=None, op0=ALU.add),
                         reads=[bwc], writes=[bwc])
                    P.op("act", lambda e: e.activation(out=rstd[:], in_=rstd[:], func=AF.Sqrt), reads=[bwc], writes=[bwc])
                    P.op("dve", lambda e: e.reciprocal(out=rstd[:], in_=rstd[:]), reads=[bwc], writes=[bwc])
                    for h in range(4):
                        P.op("dve", lambda e, h=h: e.tensor_scalar(out=on[:, h * 64:(h + 1) * 64], in0=oraw[:, h * 64:(h + 1) * 64],
                                                                   scalar1=mv[:, h, 0:1], scalar2=rstd[:, h:h + 1],
                                                                   op0=ALU.subtract, op1=ALU.mult), reads=[bwc, bwd], writes=[bwd])
                    P.op("dve", lambda e: e.tensor_tensor(out=on[:], in0=on[:], in1=rng_[:], op=ALU.mult), reads=[bwd, bpar], writes=[bwd])
                    P.op("dve", lambda e: e.tensor_tensor(out=onb[:], in0=on[:], in1=sgt[:], op=ALU.mult), reads=[bwd], writes=[bwd])
                    if lvl < 1.8:
                        return
                    bkt, bbkt = C.bank()
                    btv = bkt[:].bitcast(BF16)
                    for pr in range(2):
                        P.op("pe", lambda e, pr=pr: e.transpose(out=btv[:, pr * 128:(pr + 1) * 128], in_=onb[:, pr * 128:(pr + 1) * 128],
                                                                identity=identb[:]), reads=[bwd, bconst], writes=[bbkt])
                    P.op("act", lambda e: e.activation(out=brT[:, 6:8, l0:l0 + 128], in_=btv[:, 0:256].rearrange("p (j n) -> p j n", j=2),
                                                       func=AF.Copy), reads=[bbkt], writes=[bbr[6], bbr[7]])
                    for pr in range(2):
                        P.op("dve", lambda e, pr=pr: e.scalar_tensor_tensor(
                            out=Sst[:, pr * 64:(pr + 1) * 64], in0=Sst[:, pr * 64:(pr + 1) * 64], scalar=cd[:, pr:pr + 1],
                            in1=kv_t[:, cc, pr * 64:(pr + 1) * 64], op0=ALU.mult, op1=ALU.add), reads=[bS, bconst, bstore], writes=[bS])
                    P.op("act", lambda e: e.activation(out=Sb[:], in_=Sst[:], func=AF.Copy), reads=[bS], writes=[bS])

                if lvl < 1.1:
                    return
                for cc in range(4):
                    chunk_body(cc)
                if lvl < 3:
                    return

                if stage != "full":
                    P.dma("sp", cdbg, o_brT[:, :, t0:t0 + 512].rearrange("j p t -> p j t"), brT[:], reads=bbr, writes=[bdbg])
                def gate_body(j):
                    def pairs(s_):
                        pr_ = [(s_[:, :, b * 128:(b + 1) * 128], win_cols(2560 + b * 1024 + j * 128, 128)) for b in range(4)]
                        pr_ += [(s_[:, 0:2, 512 + b * 32:512 + b * 32 + 32].rearrange("p k n -> p k n"), None) for b in range(0)]
                        return pr_
                    Gj, bGj = wload(pairs)
                    i_ = (wsn[0] - 1) % NSLOT
                    P.dma_group("pool", cws[i_], [(wslot[i_][:, b * 2:b * 2 + 2, 512:640],
                                                   bp_d[b, :, j * 128:(j + 1) * 128].rearrange("(k p) n -> p k n", p=128)) for b in range(4)],
                                writes=[bGj])
                    if lvl == 3.1:
                        return
                    def gate_b(b):
                        bkz, bbkz = C.bank()
                        for k in range(8):
                            P.op("pe", lambda e, k=k, b=b: e.matmul(bkz[:], lhsT=Gj[:, k, b * 128:(b + 1) * 128], rhs=hT[:, k, t0:t0 + 512],
                                                                    start=(k == 0), stop=(k == 7)), reads=[bGj] + bhT_t, writes=[bbkz])
                        bkp, bbkp = C.bank()
                        for kc in range(2):
                            P.op("pe", lambda e, kc=kc, b=b: e.matmul(bkp[:], lhsT=Gj[:, b * 2 + kc, 512:640], rhs=brT[:, b * 2 + kc, :],
                                                                      start=(kc == 0), stop=(kc == 1)),
                                 reads=[bGj, bbr[b * 2], bbr[b * 2 + 1]], writes=[bbkp])
                        if lvl == 3.2:
                            return
                        P.op("act", lambda e: e.activation(out=sig[:], in_=bkz[:], func=AF.Tanh, scale=0.5), reads=[bbkz], writes=[bwg])
                        if lvl == 3.3:
                            return
                        if b == 0:
                            P.op("dve", lambda e: e.scalar_tensor_tensor(out=macc[:], in0=sig[:], scalar=1.0, in1=bkp[:], op0=ALU.add, op1=ALU.mult),
                                 reads=[bbkp, bwg], writes=[bwg])
                        else:
                            P.op("dve", lambda e: e.scalar_tensor_tensor(out=mtmp[:], in0=sig[:], scalar=1.0, in1=bkp[:], op0=ALU.add, op1=ALU.mult),
                                 reads=[bbkp, bwg], writes=[bwg])
                            P.op("dve", lambda e: e.tensor_tensor(out=macc[:], in0=macc[:], in1=mtmp[:], op=ALU.add), reads=[bwg], writes=[bwg])
                            if b == 3:
                                P.op("dve", lambda e: e.tensor_scalar(out=mergedT[:, j, :], in0=macc[:], scalar1=0.5, scalar2=None, op0=ALU.mult),
                                     reads=[bwg], writes=[bmg[j]])

                    for b_i in range(4):
                        gate_b(b_i)

                for j in range(8):
                    gate_body(j)
                if lvl < 4:
                    return

                def wout_half(half):
                    Wh, bWh = wload(lambda s_: [(s_[:, :, 0:512], wout_d[:, half * 512:(half + 1) * 512].rearrange("(k p) n -> p k n", p=128))])
                    for cc in range(4):
                        c = T * 4 + cc
                        bkm, bbkm = C.bank()
                        for k in range(8):
                            P.op("pe", lambda e, k=k, cc=cc, bkm=bkm: e.matmul(bkm[:], lhsT=mergedT[:, k, cc * 128:(cc + 1) * 128], rhs=Wh[:, k, 0:512],
                                                                               start=(k == 0), stop=(k == 7)), reads=[bWh] + bmg, writes=[bbkm])
                        P.op("dve", lambda e, c=c, bkm=bkm: e.scalar_tensor_tensor(
                            out=hres[:, c, half * 512:(half + 1) * 512], in0=hres[:, c, half * 512:(half + 1) * 512], scalar=ALPHA,
                            in1=bkm[:], op0=ALU.mult, op1=ALU.add), reads=[bbkm, bhres[c]], writes=[bhres[c]])

                for half_i in range(2):
                    wout_half(half_i)

                def ln_body(c):
                    for hf in range(2):
                        P.op("dve", lambda e, hf=hf: e.bn_stats(out=lnstat[:, hf, :], in_=hres[:, c, hf * 512:(hf + 1) * 512]),
                             reads=[bhres[c]], writes=[bln])
                    P.op("dve", lambda e: e.bn_aggr(out=lnmv[:], in_=lnstat[:].rearrange("p a b -> p (a b)")), reads=[bln], writes=[bln])
                    P.op("dve", lambda e: e.tensor_scalar(out=lnr[:, 0:1], in0=lnmv[:, 1:2], scalar1=LN_EPS, scalar2=None, op0=ALU.add),
                         reads=[bln], writes=[bln])
                    P.op("act", lambda e: e.activation(out=lnr[:, 0:1], in_=lnr[:, 0:1], func=AF.Sqrt), reads=[bln], writes=[bln])
                    P.op("dve", lambda e: e.reciprocal(out=lnr[:, 0:1], in_=lnr[:, 0:1]), reads=[bln], writes=[bln])
                    P.op("dve", lambda e: e.tensor_scalar(out=lnr[:, 1:2], in0=lnmv[:, 0:1], scalar1=lnr[:, 0:1], scalar2=-1.0,
                                                          op0=ALU.mult, op1=ALU.mult), reads=[bln], writes=[bln])
                    P.op("act", lambda e: e.activation(out=h1[:], in_=hres[:, c, :], func=AF.Identity, scale=lnr[:, 0:1], bias=lnr[:, 1:2]),
                         reads=[bhres[c], bln], writes=[bh1])
                    P.op("pool", lambda e: e.tensor_tensor(out=h1[:], in0=h1[:], in1=lmg[:], op=ALU.mult), reads=[bh1, bpar], writes=[bh1])
                    P.op("pool", lambda e: e.tensor_tensor(out=h1[:], in0=h1[:], in1=lmb[:], op=ALU.add), reads=[bh1, bpar], writes=[bh1])
                    P.op("act", lambda e: e.activation(out=hres[:, c, :], in_=h1[:], func=AF.Copy, scale=ALPHA), reads=[bh1], writes=[bhres[c]])
                    for half in range(2):
                        bk_, bbk_ = C.bank()
                        for k4 in range(4):
                            k = half * 4 + k4
                            P.op("pe", lambda e, k=k, k4=k4, bk_=bk_: e.transpose(out=bk_[:, k4 * 128:(k4 + 1) * 128], in_=h1[:, k * 128:(k + 1) * 128],
                                                                                  identity=identf[:]), reads=[bh1, bconst], writes=[bbk_])
                        P.op("act" if half == 0 else "dve",
                             (lambda e, bk_=bk_, half=half: e.activation(out=h1T[:, half * 4:half * 4 + 4, :],
                                                                         in_=bk_[:].rearrange("p (k n) -> p k n", k=4), func=AF.Copy)) if half == 0 else
                             (lambda e, bk_=bk_, half=half: e.tensor_copy(out=h1T[:, half * 4:half * 4 + 4, :],
                                                                          in_=bk_[:].rearrange("p (k n) -> p k n", k=4))),
                             reads=[bbk_], writes=[bh1T])
                    P.op("pool", lambda e: e.tensor_copy(out=hT[:, :, c * 128:(c + 1) * 128], in_=h1T[:]), reads=[bh1T], writes=[bhT[c]])
                    bkr, bbkr = C.bank()
                    for k in range(8):
                        P.op("pe", lambda e, k=k: e.matmul(bkr[:, 0:36], lhsT=h1T[:, k, :], rhs=wr[:, k, :], start=(k == 0), stop=(k == 7)),
                             reads=[bh1T, bpar], writes=[bbkr])
                    R_ = lambda a, b=None: rs[:, a:(a + 1 if b is None else b)]
                    P.op("dve", lambda e: e.tensor_tensor(out=lg[:], in0=bkr[:, 0:36], in1=rbias[:], op=ALU.add), reads=[bbkr, bpar], writes=[brt])
                    P.op("dve", lambda e: e.reduce_max(out=R_(0), in_=lg[:, 0:4], axis=AX.X), reads=[brt], writes=[brt])
                    P.op("dve", lambda e: e.tensor_scalar(out=R_(4, 8), in0=lg[:, 0:4], scalar1=R_(0), scalar2=None, op0=ALU.is_ge),
                         reads=[brt], writes=[brt])
                    P.op("dve", lambda e: e.tensor_scalar(out=R_(4, 8), in0=R_(4, 8), scalar1=-1.0, scalar2=1e30, op0=ALU.add, op1=ALU.mult),
                         reads=[brt], writes=[brt])
                    for g in range(4):
                        P.op("dve", lambda e, g=g: e.tensor_scalar(out=elm[:, g * 8:(g + 1) * 8], in0=lg[:, 4 + g * 8:12 + g * 8],
                                                                   scalar1=R_(4 + g), scalar2=None, op0=ALU.add), reads=[brt], writes=[brt])
                    P.op("dve", lambda e: e.max(out=top8[:], in_=elm[:]), reads=[brt], writes=[brt])
                    P.op("dve", lambda e: e.tensor_tensor(out=R_(1), in0=top8[:, 1:2], in1=top8[:, 0:1], op=ALU.subtract), reads=[brt], writes=[brt])
                    P.op("act", lambda e: e.activation(out=R_(1), in_=R_(1), func=AF.Exp), reads=[brt], writes=[brt])
                    P.op("dve", lambda e: e.tensor_scalar(out=R_(2), in0=R_(1), scalar1=1.0, scalar2=None, op0=ALU.add), reads=[brt], writes=[brt])
                    P.op("dve", lambda e: e.reciprocal(out=R_(2), in_=R_(2)), reads=[brt], writes=[brt])
                    P.op("dve", lambda e: e.tensor_tensor(out=R_(3), in0=R_(1), in1=R_(2), op=ALU.mult), reads=[brt], writes=[brt])
                    P.op("dve", lambda e: e.tensor_scalar(out=R_(8), in0=R_(0), scalar1=-1.0, scalar2=None, op0=ALU.mult), reads=[brt], writes=[brt])
                    P.op("act", lambda e: e.activation(out=R_(9, 13), in_=lg[:, 0:4], func=AF.Exp, bias=R_(8)), reads=[brt], writes=[brt])
                    P.op("dve", lambda e: e.reduce_sum(out=R_(13), in_=R_(9, 13), axis=AX.X), reads=[brt], writes=[brt])
                    P.op("dve", lambda e: e.reciprocal(out=R_(13), in_=R_(13)), reads=[brt], writes=[brt])
                    P.op("dve", lambda e: e.tensor_tensor(out=R_(2), in0=R_(2), in1=R_(13), op=ALU.mult), reads=[brt], writes=[brt])
                    P.op("dve", lambda e: e.tensor_tensor(out=R_(3), in0=R_(3), in1=R_(13), op=ALU.mult), reads=[brt], writes=[brt])
                    P.op("dve", lambda e: e.tensor_scalar(out=c1[:], in0=elm[:], scalar1=top8[:, 0:1], scalar2=R_(2), op0=ALU.is_equal, op1=ALU.mult),
                         reads=[brt], writes=[brt])
                    P.op("dve", lambda e: e.tensor_scalar(out=comb[:, c, :], in0=elm[:], scalar1=top8[:, 1:2], scalar2=R_(3), op0=ALU.is_equal,
                                                          op1=ALU.mult), reads=[brt], writes=[bcomb[c]])
                    P.op("dve", lambda e: e.tensor_tensor(out=comb[:, c, :], in0=comb[:, c, :], in1=c1[:], op=ALU.add), reads=[brt, bcomb[c]],
                         writes=[bcomb[c]])
                    if stage != "full":
                        P.dma("sp", cdbg, o_h[c * 128:(c + 1) * 128, :], h1[:], reads=[bh1], writes=[bdbg])
                        P.dma("sp", cdbg, o_dbg[c * 128:(c + 1) * 128, :], comb[:, c, :], reads=[bcomb[c]], writes=[bdbg])

                if lvl < 5:
                    return
                for cc in range(4):
                    ln_body(T * 4 + cc)

            for T_ in range(NT):
                tile_body(T_)
            if stage != "full":
                P.wait_all("sp", [bdbg])
            P.barrier(allchans)
            with nc.Block() as block:
                P.emit(block)
        if stage != "full":
            return nc
        build_p2_moe(nc, st, C, hres, bhres, hT, bhT, comb, bcomb, identb, bconst, ewg_d, ewu_d, ewd_d, lfg_d, lfb_d, o_h, allchans)
    return nc


def build_p3(nexp=32):
    nc = bass.Bass("TRN2", target_bir_lowering=False)
    with ExitStack() as st:
        C = Ctx(nc, st)
        P = C.P
        h1_d = C.din("h1", [TOK, D], F32)
        comb_d = C.din("comb", [TOK, 32], F32)
        ewg_d = C.din("exp_w_gate", [32, D, 256], F32)
        ewu_d = C.din("exp_w_up", [32, D, 256], F32)
        ewd_d = C.din("exp_w_down", [32, 256, D], F32)
        lfg_d = C.din("ln_ffn_g", [1, D], F32)
        lfb_d = C.din("ln_ffn_b", [1, D], F32)
        identb_d = C.din("ident_b", [128, 128], BF16)
        o_h = C.dout("o_h", [TOK, D], F32)

        hres = C.sb("hres", [128, NCH, D], F32)
        bhres = [Buf("hres%d" % c) for c in range(NCH)]
        hT = C.sb("hT", [128, 8, TOK], BF16)
        bhT = [Buf("hT%d" % c) for c in range(NCH)]
        comb = C.sb("comb", [128, NCH, 32], F32)
        bcomb = Buf("comb")
        identb = C.sb("identb", [128, 128], BF16)
        lfg = C.sb("lfg", [128, D], F32)
        lfb = C.sb("lfb", [128, D], F32)
        bconst = Buf("const")
        cst = P.chan()
        P.dma("sp", cst, identb[:], identb_d, writes=[bconst])
        P.dma("sp", cst, lfg[:], lfg_d.partition_broadcast(128), writes=[bconst])
        P.dma("sp", cst, lfb[:], lfb_d.partition_broadcast(128), writes=[bconst])
        P.dma("sp", cst, comb[:], comb_d.rearrange("(c p) n -> p c n", p=128), writes=[bcomb])

        hb16 = [C.sb("hb16_%d" % i, [128, D], BF16) for i in range(2)]
        bhb16 = [Buf("hb16_%d" % i) for i in range(2)]
        chh = [P.chan() for _ in range(2)]

        def load_chunk(c):
            s_ = c % 2
            P.dma("sp" if c % 2 == 0 else "act", chh[s_], hres[:, c, :], h1_d[c * 128:(c + 1) * 128, :], writes=[bhres[c]])
            P.op("pool", lambda e: e.tensor_copy(out=hb16[s_][:], in_=hres[:, c, :]), reads=[bhres[c]], writes=[bhb16[s_]])
            bk, bbk = C.bank(2, 4)
            bv = bk[:].bitcast(BF16)
            for k in range(8):
                P.op("pe", lambda e, k=k: e.transpose(out=bv[:, k * 128:(k + 1) * 128], in_=hb16[s_][:, k * 128:(k + 1) * 128],
                                                      identity=identb[:]), reads=[bhb16[s_], bconst], writes=[bbk])
            P.op("act", lambda e: e.activation(out=hT[:, 0:4, c * 128:(c + 1) * 128],
                                               in_=bv[:, 0:512].rearrange("p (k n) -> p k n", k=4), func=AF.Copy), reads=[bbk], writes=[bhT[c]])
            P.op("dve", lambda e: e.tensor_copy(out=hT[:, 4:8, c * 128:(c + 1) * 128],
                                                in_=bv[:, 512:1024].rearrange("p (k n) -> p k n", k=4)), reads=[bbk], writes=[bhT[c]])
            P.op("act", lambda e: e.activation(out=hres[:, c, :], in_=hres[:, c, :], func=AF.Copy, scale=ALPHA),
                 reads=[bhb16[s_]], writes=[bhres[c]])

        for c_ in range(NCH):
            load_chunk(c_)

        GS = 2
        NSET = 2
        wgu = [[C.sb("wgu%d_%d" % (s_, i), [128, 8, 512], BF16) for i in range(GS)] for s_ in range(NSET)]
        wdn = [[C.sb("wdn%d_%d" % (s_, i), [128, 2, D], BF16) for i in range(GS)] for s_ in range(NSET)]
        bwset = [Buf("wset%d" % s_) for s_ in range(NSET)]
        cwset = [P.chan() for _ in range(NSET)]

        def load_group(g):
            s_ = g % NSET
            pairs = []
            for i in range(GS):
                e_ = g * GS + i
                pairs.append((wgu[s_][i][:, :, 0:256], ewg_d[e_].rearrange("(k p) n -> p k n", p=128)))
                pairs.append((wgu[s_][i][:, :, 256:512], ewu_d[e_].rearrange("(k p) n -> p k n", p=128)))
                pairs.append((wdn[s_][i][:], ewd_d[e_].rearrange("(k p) n -> p k n", p=128)))
            P.dma_group("pool", cwset[s_], pairs, writes=[bwset[s_]])

        sl = C.sb("sl", [128, 256], F32)
        act = C.sb("act", [128, 256], BF16)
        actT = C.sb("actT", [128, 2, 128], BF16)
        bsl, bact, bactT = Buf("sl"), Buf("act"), Buf("actT")
        accset = [0]

        def compute_group(g):
            s_ = g % NSET

            def chunk(c):
                a0 = 4 + (accset[0] % 2) * 2
                accset[0] += 1
                acc = [C.banks[a0], C.banks[a0 + 1]]
                bacc = [C.bbank[a0], C.bbank[a0 + 1]]

                def expert(i):
                    e_ = g * GS + i
                    bk, bbk = C.bank(0, 2)
                    for k in range(8):
                        P.op("pe", lambda e, k=k: e.matmul(bk[:], lhsT=hT[:, k, c * 128:(c + 1) * 128], rhs=wgu[s_][i][:, k, :],
                                                           start=(k == 0), stop=(k == 7)), reads=[bhT[c], bwset[s_]], writes=[bbk])
                    P.op("act", lambda e: e.activation(out=sl[:], in_=bk[:, 0:256], func=AF.Silu), reads=[bbk], writes=[bsl])
                    P.op("dve", lambda e: e.scalar_tensor_tensor(out=act[:], in0=sl[:], scalar=comb[:, c, e_:e_ + 1], in1=bk[:, 256:512],
                                                                 op0=ALU.mult, op1=ALU.mult), reads=[bsl, bcomb, bbk], writes=[bact])
                    bt, bbt = C.bank(2, 4)
                    btv = bt[:].bitcast(BF16)
                    for j in range(2):
                        P.op("pe", lambda e, j=j: e.transpose(out=btv[:, j * 128:(j + 1) * 128], in_=act[:, j * 128:(j + 1) * 128],
                                                              identity=identb[:]), reads=[bact, bconst], writes=[bbt])
                    P.op("act", lambda e: e.activation(out=actT[:], in_=btv[:, 0:256].rearrange("p (j n) -> p j n", j=2), func=AF.Copy),
                         reads=[bbt], writes=[bactT])
                    for half in range(2):
                        for kc in range(2):
                            P.op("pe", lambda e, half=half, kc=kc: e.matmul(
                                acc[half][:], lhsT=actT[:, kc, :], rhs=wdn[s_][i][:, kc, half * 512:(half + 1) * 512],
                                start=(i == 0 and kc == 0), stop=(i == GS - 1 and kc == 1)),
                                reads=[bactT, bwset[s_]], writes=[bacc[half]])

                for i_ in range(GS):
                    expert(i_)
                for half in range(2):
                    P.op("dve", lambda e, half=half: e.tensor_tensor(out=hres[:, c, half * 512:(half + 1) * 512], in0=acc[half][:],
                                                                     in1=hres[:, c, half * 512:(half + 1) * 512], op=ALU.add),
                         reads=[bacc[half], bhres[c]], writes=[bhres[c]])

            for c_ in range(NCH):
                chunk(c_)

        ngroups = nexp // GS
        load_group(0)
        for g_ in range(ngroups):
            if g_ + 1 < ngroups:
                load_group(g_ + 1)
            compute_group(g_)

        lnstat = C.sb("lnstat", [128, 2, 6], F32)
        lnmv = C.sb("lnmv", [128, 2], F32)
        lnr = C.sb("lnr", [128, 2], F32)
        ho = [C.sb("ho%d" % i, [128, D], F32) for i in range(2)]
        bho = [Buf("ho%d" % i) for i in range(2)]
        bln = Buf("ln")
        cout = [P.chan() for _ in range(2)]
        bout = Buf("out")

        def ln_chunk(c):
            s_ = c % 2
            for hf in range(2):
                P.op("dve", lambda e, hf=hf: e.bn_stats(out=lnstat[:, hf, :], in_=hres[:, c, hf * 512:(hf + 1) * 512]),
                     reads=[bhres[c]], writes=[bln])
            P.op("dve", lambda e: e.bn_aggr(out=lnmv[:], in_=lnstat[:].rearrange("p a b -> p (a b)")), reads=[bln], writes=[bln])
            P.op("dve", lambda e: e.tensor_scalar(out=lnr[:, 0:1], in0=lnmv[:, 1:2], scalar1=LN_EPS, scalar2=None, op0=ALU.add),
                 reads=[bln], writes=[bln])
            P.op("act", lambda e: e.activation(out=lnr[:, 0:1], in_=lnr[:, 0:1], func=AF.Sqrt), reads=[bln], writes=[bln])
            P.op("dve", lambda e: e.reciprocal(out=lnr[:, 0:1], in_=lnr[:, 0:1]), reads=[bln], writes=[bln])
            P.op("dve", lambda e: e.tensor_scalar(out=lnr[:, 1:2], in0=lnmv[:, 0:1], scalar1=lnr[:, 0:1], scalar2=-1.0,
                                                  op0=ALU.mult, op1=ALU.mult), reads=[bln], writes=[bln])
            P.op("act", lambda e: e.activation(out=ho[s_][:], in_=hres[:, c, :], func=AF.Identity, scale=lnr[:, 0:1], bias=lnr[:, 1:2]),
                 reads=[bhres[c], bln], writes=[bho[s_]])
            P.op("pool", lambda e: e.tensor_tensor(out=ho[s_][:], in0=ho[s_][:], in1=lfg[:], op=ALU.mult), reads=[bho[s_], bconst], writes=[bho[s_]])
            P.op("pool", lambda e: e.tensor_tensor(out=ho[s_][:], in0=ho[s_][:], in1=lfb[:], op=ALU.add), reads=[bho[s_], bconst], writes=[bho[s_]])
            P.dma("sp", cout[s_], o_h[c * 128:(c + 1) * 128, :], ho[s_][:], reads=[bho[s_]], writes=[bout])

        for c_ in range(NCH):
            ln_chunk(c_)
        P.wait_all("sp", [bout])
        with nc.Block() as block:
            P.emit(block)
    return nc


_PROGS = {}


def _prog(name):
    if name not in _PROGS:
        _PROGS[name] = {"p1": build_p1, "p2": lambda: build_p2("A"), "p3": build_p3}[name]()
    return _PROGS[name]


def _seg(c):
    b, k = c // 4, c % 4
    return b, k * TOK


def _halo(h_full, c):
    b, s0 = _seg(c)
    halo = np.zeros((4, D), np.float32)
    if s0 > 0:
        halo[1:4] = h_full[b, s0 - 3:s0]
    return halo


def kernel(**inputs):
    inp = {k: np.asarray(v) for k, v in inputs.items()}
    cs, cs2 = consts_np(), consts2_np()
    h_full = np.ascontiguousarray(inp["x"], dtype=np.float32)
    pos = np.ascontiguousarray(inp["positions"]).astype(np.int32)
    cores = list(range(NCORES))
    for L in range(2):
        w_in = np.ascontiguousarray(inp["w_in"][L])
        w_p1 = np.ascontiguousarray(np.concatenate([w_in[:, 768:1024], w_in[:, 1536:2304]], axis=1))
        row = lambda name: np.ascontiguousarray(inp[name][L][None])
        in1 = []
        for c in cores:
            b, s0 = _seg(c)
            d = dict(h=np.ascontiguousarray(h_full[b, s0:s0 + TOK]), halo=_halo(h_full, c),
                     pos=np.ascontiguousarray(pos[b:b + 1, s0:s0 + TOK]), w_p1=w_p1,
                     lru_conv_w=np.ascontiguousarray(inp["lru_conv_w"][L]), lru_conv_b=row("lru_conv_b"),
                     lru_w_r=np.ascontiguousarray(inp["lru_w_r"][L]), lru_w_i=np.ascontiguousarray(inp["lru_w_i"][L]),
                     lru_b_r=row("lru_b_r"), lru_b_i=row("lru_b_i"), lru_lambda=row("lru_lambda"))
            for n in ("ident_f", "ident_b", "rot_b", "invf", "kdec", "cd"):
                d[n] = cs[n]
            in1.append(d)
        r1 = run_bass_kernel_spmd(_prog("p1"), in1, core_ids=cores).results
        ends_all = np.ascontiguousarray(np.stack([np.asarray(r1[c]["o_end"]) for c in cores]))
        in2 = []
        for c in cores:
            b, s0 = _seg(c)
            d = dict(h=np.ascontiguousarray(h_full[b, s0:s0 + TOK]), halo=_halo(h_full, c),
                     hloc=np.asarray(r1[c]["o_hloc"]), Pc=np.asarray(r1[c]["o_P"]), qT=np.asarray(r1[c]["o_qT"]), kT=np.asarray(r1[c]["o_kT"]),
                     v=np.asarray(r1[c]["o_v"]), kv=np.asarray(r1[c]["o_kv"]), ends_all=ends_all, sel=sel_np(c), w_in=w_in,
                     sc_conv_w=np.ascontiguousarray(inp["sc_conv_w"][L]), sc_conv_b=row("sc_conv_b"), sg_norm_g=row("sg_norm_g"),
                     sg_w_s=np.ascontiguousarray(inp["sg_w_s"][L]), sg_b_s=np.ascontiguousarray(inp["sg_b_s"][L]), ret_norm_g=row("ret_norm_g"),
                     branch_proj=np.ascontiguousarray(inp["branch_proj"][L]), w_out=np.ascontiguousarray(inp["w_out"][L]),
                     ln_mix_g=row("ln_mix_g"), ln_mix_b=row("ln_mix_b"),
                     router_group_w=np.ascontiguousarray(inp["router_group_w"][L]), router_group_b=row("router_group_b"),
                     router_expert_w=np.ascontiguousarray(inp["router_expert_w"][L]), router_expert_b=row("router_expert_b"))
            d["ident_f"], d["ident_b"], d["cd"] = cs["ident_f"], cs["ident_b"], cs["cd"]
            for n in ("decayT", "qdecT", "cdT", "causal"):
                d[n] = cs2[n]
            in2.append(d)
        r2 = run_bass_kernel_spmd(_prog("p2"), in2, core_ids=cores).results
        ewg = np.ascontiguousarray(inp["exp_w_gate"][L])
        ewu = np.ascontiguousarray(inp["exp_w_up"][L])
        ewd = np.ascontiguousarray(inp["exp_w_down"][L])
        in3 = [dict(h1=np.asarray(r2[c]["o_h"]), comb=np.asarray(r2[c]["o_dbg"]), exp_w_gate=ewg, exp_w_up=ewu, exp_w_down=ewd,
                    ln_ffn_g=row("ln_ffn_g"), ln_ffn_b=row("ln_ffn_b"), ident_b=cs["ident_b"]) for c in cores]
        r3 = run_bass_kernel_spmd(_prog("p3"), in3, core_ids=cores).results
        h_next = np.empty_like(h_full)
        for c in cores:
            b, s0 = _seg(c)
            h_next[b, s0:s0 + TOK] = np.asarray(r3[c]["o_h"])
        h_full = h_next
    return h_full.astype(np.float32)
```

```python
import numpy as np
from contextlib import ExitStack
import ml_dtypes
import concourse.bass as bass
import concourse.mybir as mybir
from concourse.bass_utils import run_bass_kernel_spmd

F32 = mybir.dt.float32
BF16 = mybir.dt.bfloat16
I32 = mybir.dt.int32
AF = mybir.ActivationFunctionType
ALU = mybir.AluOpType
AX = mybir.AxisListType

NCORES = 8
D = 1024
TOK = 2048
NCH = TOK // 128
NT = TOK // 512
GAMMA = [1.0 - 2.0 ** (-5.0 - h) for h in range(4)]


class Buf:
    __slots__ = ("w", "r", "name")

    def __init__(self, name=""):
        self.w = None
        self.r = {}
        self.name = name


class Chan:
    __slots__ = ("key", "cnt")

    def __init__(self, key):
        self.key = key
        self.cnt = 0


class Prog:
    ENG = ("pe", "act", "dve", "pool", "sp")

    def __init__(self, nc, stack):
        self.nc = nc
        self.stack = stack
        self.q = {e: [] for e in self.ENG}
        self.sems = {}
        for e in self.ENG:
            self.sems[e] = stack.enter_context(nc.semaphore("s_" + e))
        self.cnt = {e: 0 for e in self.ENG}
        self.seen = {e: {} for e in self.ENG}
        self.nchan = 0

    def chan(self):
        key = "c%d" % self.nchan
        self.nchan += 1
        self.sems[key] = self.stack.enter_context(self.nc.semaphore("s_" + key))
        return Chan(key)

    def _deps(self, eng, reads, writes, extra=()):
        need = {}

        def add(sp):
            if sp is None:
                return
            k, v = sp
            if need.get(k, 0) < v:
                need[k] = v

        for b in reads:
            add(b.w)
        for b in writes:
            add(b.w)
            for k, v in b.r.items():
                add((k, v))
        for sp in extra:
            add(sp)
        waits = []
        for k, v in need.items():
            if k == eng and eng == "pe":
                continue
            if self.seen[eng].get(k, 0) < v:
                self.seen[eng][k] = v
                waits.append((k, v))
        return waits

    def op(self, eng, fn, reads=(), writes=()):
        waits = self._deps(eng, reads, writes)
        self.cnt[eng] += 1
        v = self.cnt[eng]
        self.q[eng].append((waits, fn, eng, 1))
        for b in reads:
            if b.r.get(eng, 0) < v:
                b.r[eng] = v
        for b in writes:
            b.w = (eng, v)
            b.r = {}

    def dma(self, qeng, chan, out, in_, reads=(), writes=(), **kw):
        prev = (chan.key, chan.cnt) if chan.cnt else None
        waits = self._deps(qeng, reads, writes, extra=(prev,) if prev else ())
        chan.cnt += 16
        v = chan.cnt

        def fn(e, out=out, in_=in_, kw=kw):
            return e.dma_start(out=out, in_=in_, **kw)

        self.q[qeng].append((waits, fn, chan.key, 16))
        for b in reads:
            b.r[chan.key] = v
        for b in writes:
            b.w = (chan.key, v)
            b.r = {}

    def dma_group(self, qeng, chan, pairs, reads=(), writes=(), **kw):
        prev = (chan.key, chan.cnt) if chan.cnt else None
        waits = self._deps(qeng, reads, writes, extra=(prev,) if prev else ())
        for i, (out, in_) in enumerate(pairs):
            chan.cnt += 16

            def fn(e, out=out, in_=in_, kw=kw):
                return e.dma_start(out=out, in_=in_, **kw)

            self.q[qeng].append((waits if i == 0 else [], fn, chan.key, 16))
        v = chan.cnt
        for b in reads:
            b.r[chan.key] = v
        for b in writes:
            b.w = (chan.key, v)
            b.r = {}

    def barrier(self, chans=()):
        for e in self.ENG:
            waits = []
            for k in self.ENG:
                v = self.cnt[k]
                if k != e and v and self.seen[e].get(k, 0) < v:
                    self.seen[e][k] = v
                    waits.append((k, v))
            for ch in chans:
                if ch.cnt and self.seen[e].get(ch.key, 0) < ch.cnt:
                    self.seen[e][ch.key] = ch.cnt
                    waits.append((ch.key, ch.cnt))
            self.q[e].append((waits, None, None, 0))

    def wait_all(self, eng, bufs):
        waits = self._deps(eng, bufs, bufs)
        self.q[eng].append((waits, None, None, 0))

    def emit(self, block):
        sems = self.sems

        def run(e, items):
            for waits, fn, key, inc in items:
                for k, v in waits:
                    e.wait_ge(sems[k], v)
                if fn is not None:
                    fn(e).then_inc(sems[key], inc)

        @block.tensor
        def _(e):
            run(e, self.q["pe"])

        @block.scalar
        def _(e):
            run(e, self.q["act"])

        @block.vector
        def _(e):
            run(e, self.q["dve"])

        @block.gpsimd
        def _(e):
            run(e, self.q["pool"])

        @block.sync
        def _(e):
            run(e, self.q["sp"])

        self.q = {e: [] for e in self.ENG}


class Ctx:
    def __init__(self, nc, st):
        self.nc, self.st = nc, st
        self.P = Prog(nc, st)
        self.banks = [st.enter_context(nc.psum_tensor("bank%d" % i, [128, 512], F32)) for i in range(8)]
        self.bbank = [Buf("bank%d" % i) for i in range(8)]
        self.rr = 0

    def sb(self, name, shape, dt):
        return self.st.enter_context(self.nc.sbuf_tensor("sb_" + name, shape, dt))

    def din(self, name, shape, dt):
        return self.nc.dram_tensor(name, list(shape), dt, kind="ExternalInput").ap()

    def dout(self, name, shape, dt):
        return self.nc.dram_tensor(name, list(shape), dt, kind="ExternalOutput").ap()

    def bank(self, lo=0, hi=8):
        n = hi - lo
        i = lo + (self.rr % n)
        self.rr += 1
        return self.banks[i], self.bbank[i]


def consts_np():
    c = {}
    c["ident_f"] = np.eye(128, dtype=np.float32)
    c["ident_b"] = np.eye(128, dtype=np.float32).astype(ml_dtypes.bfloat16)
    rot = np.zeros((128, 128), np.float32)
    for h in range(2):
        for d in range(32):
            rot[h * 64 + d + 32, h * 64 + d] = -1.0
            rot[h * 64 + d, h * 64 + d + 32] = 1.0
    c["rot_b"] = rot.astype(ml_dtypes.bfloat16)
    half = 32
    invf = (10000.0 ** (-np.arange(half, dtype=np.float32) / half)).astype(np.float32)
    c["invf"] = np.tile(invf, 4).reshape(128, 1).astype(np.float32)
    lg = np.log1p(-np.exp2(-5.0 - np.arange(4, dtype=np.float64)))
    pos = np.arange(128, dtype=np.float64)
    kdec = np.exp((127.0 - pos)[:, None] * lg[None, :])
    c["kdec"] = np.repeat(kdec, 64, axis=1).astype(np.float32)
    cd = np.exp(128.0 * lg)
    c["cd"] = np.stack([np.repeat(cd[0:2], 64), np.repeat(cd[2:4], 64)], 1).astype(np.float32)
    return c


def build_p1(debug=False):
    nc = bass.Bass("TRN2", target_bir_lowering=False)
    with ExitStack() as st:
        C = Ctx(nc, st)
        P = C.P
        h_d = C.din("h", [TOK, D], F32)
        halo_d = C.din("halo", [4, D], F32)
        pos_d = C.din("pos", [1, TOK], I32)
        w_d = C.din("w_p1", [D, 1024], F32)
        cw_d = C.din("lru_conv_w", [4, 256], F32)
        cb_d = C.din("lru_conv_b", [1, 256], F32)
        wr_d = C.din("lru_w_r", [4, 64, 64], F32)
        wi_d = C.din("lru_w_i", [4, 64, 64], F32)
        br_d = C.din("lru_b_r", [1, 256], F32)
        bi_d = C.din("lru_b_i", [1, 256], F32)
        lam_d = C.din("lru_lambda", [1, 256], F32)
        identf_d = C.din("ident_f", [128, 128], F32)
        identb_d = C.din("ident_b", [128, 128], BF16)
        rot_d = C.din("rot_b", [128, 128], BF16)
        invf_d = C.din("invf", [128, 1], F32)
        kdec_d = C.din("kdec", [128, 256], F32)
        cd_d = C.din("cd", [128, 2], F32)
        o_hloc = C.dout("o_hloc", [2, 128, TOK], F32)
        o_P = C.dout("o_P", [2, 128, TOK], F32)
        o_qT = C.dout("o_qT", [2, 128, TOK], BF16)
        o_kT = C.dout("o_kT", [2, 128, TOK], BF16)
        o_v = C.dout("o_v", [NCH, 128, 256], BF16)
        o_kv = C.dout("o_kv", [NCH, 128, 128], F32)
        o_end = C.dout("o_end", [128, 4 + 128], F32)
        hT = C.sb("hT", [128, 8, TOK], BF16)
        bhT = [Buf("hT%d" % c) for c in range(NCH)]
        haloT = C.sb("haloT", [128, 8, 128], BF16)
        bhaloT = Buf("haloT")
        w_sb = C.sb("w_sb", [128, 8, 1024], BF16)
        bw = Buf("w")
        identf = C.sb("identf", [128, 128], F32)
        identb = C.sb("identb", [128, 128], BF16)
        rot = C.sb("rot", [128, 128], BF16)
        invf = C.sb("invf", [128, 1], F32)
        kdec = C.sb("kdec", [128, 256], F32)
        cd = C.sb("cd", [128, 2], F32)
        bconst = Buf("const")
        hin = [C.sb("hin%d" % i, [128, D], F32) for i in range(2)]
        bhin = [Buf("hin%d" % i) for i in range(2)]
        chin = [P.chan() for _ in range(2)]
        halo_sb = hin[1]
        posb = C.sb("posb", [128, TOK], I32)
        posf = C.sb("posf", [128, TOK], F32)
        bpos = Buf("pos")
        cw = C.sb("cw", [128, 2, 4], F32)
        cb = C.sb("cb", [128, 2], F32)
        brs = C.sb("brs", [128, 2], F32)
        bis = C.sb("bis", [128, 2], F32)
        lam = C.sb("lam", [128, 2], F32)
        cexp = C.sb("cexp", [128, 2], F32)
        cexp2 = C.sb("cexp2", [128, 2], F32)
        bdf = C.sb("bdf", [128, 2, 2, 128], F32)
        bd = C.sb("bd", [128, 2, 2, 128], BF16)
        bpar = Buf("par")
        cst = P.chan()
        cst2 = P.chan()
        for dst, src in ((identf, identf_d), (identb, identb_d), (rot, rot_d), (invf, invf_d), (kdec, kdec_d), (cd, cd_d)):
            P.dma("sp", cst, dst[:], src, writes=[bconst])
        wst = [C.sb("wst%d" % i, [128, 1024], F32) for i in range(2)]
        bwst = [Buf("wst%d" % i) for i in range(2)]
        cwst = [P.chan() for _ in range(2)]
        bwk = [Buf("w%d" % k) for k in range(8)]
        for k in range(8):
            P.dma("act", cwst[k % 2], wst[k % 2][:], w_d[k * 128:(k + 1) * 128, :], writes=[bwst[k % 2]])
            P.op("pool", lambda e, k=k: e.tensor_copy(out=w_sb[:, k, :], in_=wst[k % 2][:]), reads=[bwst[k % 2]], writes=[bwk[k], bw])
        P.dma("sp", cst, posb[:], pos_d.partition_broadcast(128), writes=[bpos])
        P.op("dve", lambda e: e.tensor_copy(out=posf[:], in_=posb[:]), reads=[bpos], writes=[bpos])
        for c_ in range(2):
            P.dma("sp", cst, cw[:, c_, :], cw_d[:, c_ * 128:(c_ + 1) * 128].rearrange("j p -> p j"), writes=[bpar],
                  allow_slow_non_contiguous=True)
        for dst, src in ((cb, cb_d), (brs, br_d), (bis, bi_d), (lam, lam_d)):
            P.dma("sp", cst, dst[:], src.rearrange("o (c p) -> p (o c)", p=128), writes=[bpar], allow_slow_non_contiguous=True)
        P.op("pool", lambda e: e.memset(bdf[:], 0.0), writes=[bpar])
        for gi, src in enumerate((wr_d, wi_d)):
            for hh in range(4):
                ch, lo = hh // 2, (hh % 2) * 64
                P.dma("sp", cst, bdf[lo:lo + 64, gi, ch, lo:lo + 64], src[hh], writes=[bpar])
        P.op("dve", lambda e: e.tensor_copy(out=bd[:], in_=bdf[:]), reads=[bpar], writes=[bpar])
        P.op("act", lambda e: e.activation(out=cexp[:], in_=lam[:], func=AF.Exp, scale=-1.0), reads=[bpar], writes=[bpar])
        P.op("act", lambda e: e.activation(out=cexp[:], in_=cexp[:], func=AF.Ln, bias=1.0), reads=[bpar], writes=[bpar])
        P.op("dve", lambda e: e.tensor_scalar(out=cexp2[:], in0=cexp[:], scalar1=-16.0, scalar2=None, op0=ALU.mult), reads=[bpar], writes=[bpar])
        P.op("dve", lambda e: e.tensor_scalar(out=cexp[:], in0=cexp[:], scalar1=-8.0, scalar2=None, op0=ALU.mult), reads=[bpar], writes=[bpar])

        hb16 = [C.sb("hb16_%d" % i, [128, D], BF16) for i in range(2)]
        bhb16 = [Buf("hb16_%d" % i) for i in range(2)]
        tcount = [0]

        def transpose_rows(src_sb, bsrc, nrows, dstT, bdst, col0):
            s_ = tcount[0] % 2
            tcount[0] += 1
            P.op("pool", lambda e, s_=s_: e.tensor_copy(out=hb16[s_][:], in_=src_sb[:]), reads=[bsrc], writes=[bhb16[s_]])
            bk, bbk = C.bank()
            bv = bk[:].bitcast(BF16)
            for k in range(8):
                P.op("pe", lambda e, k=k, bv=bv, s_=s_: e.transpose(
                    out=bv[:, k * 128:k * 128 + nrows], in_=hb16[s_][0:nrows, k * 128:(k + 1) * 128],
                    identity=identb[0:nrows, 0:nrows]), reads=[bhb16[s_], bconst], writes=[bbk])
            for half in range(2):
                src = bv[:, half * 512:(half + 1) * 512].rearrange("p (k n) -> p k n", k=4)[:, :, 0:nrows]
                dst = dstT[:, half * 4:half * 4 + 4, col0:col0 + nrows]
                if half == 0:
                    P.op("act", lambda e, src=src, dst=dst: e.activation(out=dst, in_=src, func=AF.Copy), reads=[bbk], writes=[bdst])
                else:
                    P.op("dve", lambda e, src=src, dst=dst: e.tensor_copy(out=dst, in_=src), reads=[bbk], writes=[bdst])

        bhalo = Buf("halo")
        bhalo = bhin[1]
        P.op("pool", lambda e: e.memset(halo_sb[:], 0.0), writes=[bhalo])
        P.dma("sp", chin[1], halo_sb[0:4, :], halo_d, writes=[bhalo])
        transpose_rows(halo_sb, bhalo, 128, haloT, bhaloT, 0)
        for c in range(NCH):
            s = c % 2
            P.dma("sp" if c % 2 == 0 else "act", chin[s], hin[s][:], h_d[c * 128:(c + 1) * 128, :], writes=[bhin[s]])
            transpose_rows(hin[s], bhin[s], 128, hT, bhT[c], c * 128)

        xl = C.sb("xl", [128, 2, 3 + 512], F32)
        bxl = Buf("xl")
        xc = C.sb("xc", [128, 2, 512], F32)
        xcb = C.sb("xcb", [128, 2, 512], BF16)
        bxc = Buf("xc")
        rg = C.sb("rg", [128, 2, 512], F32)
        ig = C.sb("ig", [128, 2, 512], F32)
        av = C.sb("av", [128, 2, 512], F32)
        uv = C.sb("uv", [128, 2, 512], F32)
        blru = Buf("lruwork")
        zeros = C.sb("zeros", [128, 512], F32)
        bzero = Buf("zeros")
        P.op("pool", lambda e: e.memset(zeros[:], 0.0), writes=[bzero])
        hloc = C.sb("hloc", [128, 2, TOK], F32)
        Pc = C.sb("Pc", [128, 2, TOK], F32)
        bscan = Buf("scan")
        ang = C.sb("ang", [128, 512], F32)
        tmpa = C.sb("tmpa", [128, 512], F32)
        tmpi = C.sb("tmpi", [128, 512], I32)
        cosT = C.sb("cosT", [128, 512], F32)
        sinT = C.sb("sinT", [128, 512], F32)
        btrig = Buf("trig")
        qk_f = C.sb("qk_f", [128, 4, 512], F32)
        qk_b = C.sb("qk_b", [128, 4, 512], BF16)
        bqk = Buf("qk")
        qkr = C.sb("qkr", [128, 4, TOK], BF16)
        bqkr = [Buf("qkr%d" % t) for t in range(NT)]
        vtok = C.sb("vtok", [128, NCH, 256], BF16)
        bvt = [Buf("vt%d" % c) for c in range(NCH)]
        kdt = C.sb("kdt", [128, 256], BF16)
        bkdt = Buf("kdt")
        kvs = C.sb("kvs", [128, NCH, 128], F32)
        bkvs = [Buf("kvs%d" % c) for c in range(NCH)]
        Sst = C.sb("Sst", [128, 128], F32)
        bS = Buf("S")
        P.op("pool", lambda e: e.memset(Sst[:], 0.0), writes=[bS])
        endt = C.sb("endt", [128, 4 + 128], F32)
        bend = Buf("end")
        TWO_PI = float(2.0 * np.pi)

        def dump_debug():
            o_dbg = C.dout("o_dbg", [10, 128, 512], F32)
            cdb = P.chan()
            bdbg = Buf("dbg")
            for i_, (t_, b_) in enumerate(((xc[:, 0, :], bxc), (rg[:, 0, :], blru), (ig[:, 0, :], blru), (av[:, 0, :], blru),
                                          (uv[:, 0, :], blru), (zeros[:], bzero), (xl[:, 0, 0:512], bxl), (xl[:, 1, 0:512], bxl),
                                          (xc[:, 1, :], bxc), (xl[:, 0, 3:515], bxl))):
                P.dma("sp", cdb, o_dbg[i_], t_, reads=[b_], writes=[bdbg])
            return bdbg
        dbg_bufs = []
        def tile_body(T):
            t0 = T * 512
            bhT_t = bhT[T * 4:T * 4 + 4]
            if T == 0:
                bk, bbk = C.bank()
                for j in range(2):
                    for k in range(8):
                        P.op("pe", lambda e, j=j, k=k, bk=bk: e.matmul(
                            bk[:, j * 4:j * 4 + 4], lhsT=w_sb[:, k, j * 128:(j + 1) * 128], rhs=haloT[:, k, 0:4],
                            start=(k == 0), stop=(k == 7)), reads=[bw, bhaloT], writes=[bbk])
                P.op("act", lambda e, bk=bk: e.activation(
                    out=xl[:, :, 0:3], in_=bk[:, 0:8].rearrange("p (j n) -> p j n", j=2)[:, :, 1:4], func=AF.Copy),
                    reads=[bbk], writes=[bxl])
            else:
                P.op("pool", lambda e: e.tensor_copy(out=xl[:, :, 0:3], in_=xl[:, :, 512:515]), reads=[bxl], writes=[bxl])
            for j in range(2):
                bk, bbk = C.bank()
                for k in range(8):
                    P.op("pe", lambda e, j=j, k=k, bk=bk: e.matmul(
                        bk[:], lhsT=w_sb[:, k, j * 128:(j + 1) * 128], rhs=hT[:, k, t0:t0 + 512],
                        start=(k == 0), stop=(k == 7)), reads=[bw] + bhT_t, writes=[bbk])
                P.op("act", lambda e, j=j, bk=bk: e.activation(out=xl[:, j, 3:515], in_=bk[:], func=AF.Copy),
                     reads=[bbk], writes=[bxl])
            for j in range(2):
                P.op("dve", lambda e, j=j: e.tensor_scalar(
                    out=xc[:, j, :], in0=xl[:, j, 0:512], scalar1=cw[:, j, 0:1], scalar2=cb[:, j:j + 1],
                    op0=ALU.mult, op1=ALU.add), reads=[bxl, bpar], writes=[bxc])
                for jj in range(1, 4):
                    P.op("dve", lambda e, j=j, jj=jj: e.scalar_tensor_tensor(
                        out=xc[:, j, :], in0=xl[:, j, jj:jj + 512], scalar=cw[:, j, jj:jj + 1], in1=xc[:, j, :],
                        op0=ALU.mult, op1=ALU.add), reads=[bxl, bpar, bxc], writes=[bxc])
            P.op("pool", lambda e: e.tensor_copy(out=xcb[:], in_=xc[:]), reads=[bxc], writes=[bxc])
            for j in range(2):
                for gi, (dst, bias) in enumerate(((rg, brs), (ig, bis))):
                    bk, bbk = C.bank()
                    P.op("pe", lambda e, j=j, gi=gi, bk=bk: e.matmul(
                        bk[:], lhsT=bd[:, gi, j, :], rhs=xcb[:, j, :], start=True, stop=True),
                        reads=[bpar, bxc], writes=[bbk])
                    P.op("act", lambda e, j=j, dst=dst, bias=bias, bk=bk: e.activation(
                        out=dst[:, j, :], in_=bk[:], func=AF.Sigmoid, bias=bias[:, j:j + 1]),
                        reads=[bbk, bpar], writes=[blru])
            for j in range(2):
                P.op("act", lambda e, j=j: e.activation(out=av[:, j, :], in_=rg[:, j, :], func=AF.Exp, scale=cexp[:, j:j + 1]),
                     reads=[blru, bpar], writes=[blru])
                P.op("act", lambda e, j=j: e.activation(out=uv[:, j, :], in_=rg[:, j, :], func=AF.Exp, scale=cexp2[:, j:j + 1]),
                     reads=[blru, bpar], writes=[blru])
                P.op("dve", lambda e, j=j: e.tensor_scalar(out=uv[:, j, :], in0=uv[:, j, :], scalar1=-1.0, scalar2=1.0,
                                                           op0=ALU.mult, op1=ALU.add), reads=[blru], writes=[blru])
                P.op("dve", lambda e, j=j: e.tensor_scalar_max(out=uv[:, j, :], in0=uv[:, j, :], scalar1=0.0),
                     reads=[blru], writes=[blru])
                P.op("act", lambda e, j=j: e.activation(out=uv[:, j, :], in_=uv[:, j, :], func=AF.Sqrt),
                     reads=[blru], writes=[blru])
                P.op("dve", lambda e, j=j: e.tensor_tensor(out=ig[:, j, :], in0=ig[:, j, :], in1=xc[:, j, :], op=ALU.mult),
                     reads=[blru, bxc], writes=[blru])
                P.op("dve", lambda e, j=j: e.tensor_tensor(out=uv[:, j, :], in0=uv[:, j, :], in1=ig[:, j, :], op=ALU.mult),
                     reads=[blru], writes=[blru])
                ini_h = 0.0 if T == 0 else hloc[:, j, t0 - 1:t0]
                ini_p = 1.0 if T == 0 else Pc[:, j, t0 - 1:t0]
                P.op("dve", lambda e, j=j, ini_h=ini_h: e.tensor_tensor_scan(
                    out=hloc[:, j, t0:t0 + 512], data0=av[:, j, :], data1=uv[:, j, :], initial=ini_h,
                    op0=ALU.mult, op1=ALU.add), reads=[blru, bscan], writes=[bscan])
                P.op("dve", lambda e, j=j, ini_p=ini_p: e.tensor_tensor_scan(
                    out=Pc[:, j, t0:t0 + 512], data0=av[:, j, :], data1=zeros[:], initial=ini_p,
                    op0=ALU.mult, op1=ALU.add), reads=[blru, bscan, bzero], writes=[bscan])

            if debug and T == debug - 1:
                dbg_bufs.append(dump_debug())
            P.op("dve", lambda e: e.tensor_scalar(out=ang[:], in0=posf[:, t0:t0 + 512], scalar1=invf[:, 0:1], scalar2=None,
                                                  op0=ALU.mult), reads=[bpos, bconst], writes=[btrig])
            for dst, shift in ((sinT, 0.0), (cosT, float(np.pi / 2))):
                P.op("dve", lambda e, shift=shift: e.tensor_scalar(out=tmpa[:], in0=ang[:], scalar1=shift, scalar2=1.0 / TWO_PI,
                                                                   op0=ALU.add, op1=ALU.mult), reads=[btrig], writes=[btrig])
                P.op("dve", lambda e: e.tensor_copy(out=tmpi[:], in_=tmpa[:]), reads=[btrig], writes=[btrig])
                P.op("dve", lambda e: e.tensor_copy(out=tmpa[:], in_=tmpi[:]), reads=[btrig], writes=[btrig])
                P.op("dve", lambda e: e.tensor_scalar(out=tmpa[:], in0=tmpa[:], scalar1=-TWO_PI, scalar2=None, op0=ALU.mult),
                     reads=[btrig], writes=[btrig])
                P.op("dve", lambda e, shift=shift: e.scalar_tensor_tensor(out=tmpa[:], in0=ang[:], scalar=shift, in1=tmpa[:],
                                                                          op0=ALU.add, op1=ALU.add), reads=[btrig], writes=[btrig])
                P.op("dve", lambda e, dst=dst: e.tensor_scalar(out=dst[:], in0=tmpa[:], scalar1=float(np.pi), scalar2=-TWO_PI,
                                                               op0=ALU.is_gt, op1=ALU.mult), reads=[btrig], writes=[btrig])
                P.op("dve", lambda e, dst=dst: e.tensor_tensor(out=tmpa[:], in0=tmpa[:], in1=dst[:], op=ALU.add),
                     reads=[btrig], writes=[btrig])
                P.op("dve", lambda e, dst=dst: e.tensor_scalar(out=dst[:], in0=tmpa[:], scalar1=float(-np.pi), scalar2=TWO_PI,
                                                               op0=ALU.is_lt, op1=ALU.mult), reads=[btrig], writes=[btrig])
                P.op("dve", lambda e, dst=dst: e.tensor_tensor(out=tmpa[:], in0=tmpa[:], in1=dst[:], op=ALU.add),
                     reads=[btrig], writes=[btrig])
                P.op("act", lambda e, dst=dst: e.activation(out=dst[:], in_=tmpa[:], func=AF.Sin), reads=[btrig], writes=[btrig])

            for i in range(4):
                col = 256 + i * 128
                bk, bbk = C.bank()
                for k in range(8):
                    P.op("pe", lambda e, k=k, col=col, bk=bk: e.matmul(
                        bk[:], lhsT=w_sb[:, k, col:col + 128], rhs=hT[:, k, t0:t0 + 512],
                        start=(k == 0), stop=(k == 7)), reads=[bw] + bhT_t, writes=[bbk])
                sc = 1.0 if i < 2 else 0.125
                P.op("act", lambda e, i=i, sc=sc, bk=bk: e.activation(out=qk_f[:, i, :], in_=bk[:], func=AF.Copy, scale=sc),
                     reads=[bbk], writes=[bqk])
            P.op("pool", lambda e: e.tensor_copy(out=qk_b[:], in_=qk_f[:]), reads=[bqk], writes=[bqk])
            for i in range(4):
                bk, bbk = C.bank()
                P.op("pe", lambda e, i=i, bk=bk: e.matmul(bk[:], lhsT=rot[:], rhs=qk_b[:, i, :], start=True, stop=True),
                     reads=[bconst, bqk], writes=[bbk])
                P.op("dve", lambda e, i=i, bk=bk: e.tensor_tensor(out=tmpa[:], in0=bk[:], in1=sinT[:], op=ALU.mult),
                     reads=[bbk, btrig], writes=[btrig])
                P.op("dve", lambda e, i=i: e.tensor_tensor(out=qk_f[:, i, :], in0=qk_f[:, i, :], in1=cosT[:], op=ALU.mult),
                     reads=[bqk, btrig], writes=[bqk])
                P.op("dve", lambda e, i=i: e.tensor_tensor(out=qkr[:, i, t0:t0 + 512], in0=qk_f[:, i, :], in1=tmpa[:], op=ALU.add),
                     reads=[bqk, btrig], writes=[bqkr[T]])

            if debug and T == debug - 1:
                o_qpre = C.dout("o_qpre", [128, 512], BF16)
                o_qrop = C.dout("o_qrop", [128, 512], BF16)
                cq = P.chan()
                bq_ = Buf("qdbg")
                P.dma("sp", cq, o_qpre, qk_b[:, 0, :], reads=[bqk], writes=[bq_])
                P.dma("sp", cq, o_qrop, qkr[:, 0, t0:t0 + 512], reads=[bqkr[T]], writes=[bq_])
                dbg_bufs.append(bq_)
            def chunk_body(c):
                c0 = c * 128
                bk, bbk = C.bank()
                for k in range(8):
                    P.op("pe", lambda e, k=k, c0=c0, bk=bk: e.matmul(
                        bk[:, 0:256], lhsT=hT[:, k, c0:c0 + 128], rhs=w_sb[:, k, 768:1024],
                        start=(k == 0), stop=(k == 7)), reads=[bw, bhT[c]], writes=[bbk])
                P.op("act", lambda e, c=c, bk=bk: e.activation(out=vtok[:, c, :], in_=bk[:, 0:256], func=AF.Copy),
                     reads=[bbk], writes=[bvt[c]])
                bk2, bbk2 = C.bank()
                kview = bk2[:].bitcast(BF16)
                for pr in range(2):
                    P.op("pe", lambda e, pr=pr, c0=c0, kview=kview: e.transpose(
                        out=kview[:, pr * 128:(pr + 1) * 128], in_=qkr[:, 2 + pr, c0:c0 + 128], identity=identb[:]),
                        reads=[bqkr[T], bconst], writes=[bbk2])
                P.op("dve", lambda e, kview=kview: e.tensor_tensor(out=kdt[:], in0=kview[:, 0:256], in1=kdec[:], op=ALU.mult),
                     reads=[bbk2, bconst], writes=[bkdt])
                bk3, bbk3 = C.bank()
                for pr in range(2):
                    P.op("pe", lambda e, pr=pr, c=c, bk3=bk3: e.matmul(
                        bk3[:, pr * 128:(pr + 1) * 128], lhsT=kdt[:, pr * 128:(pr + 1) * 128],
                        rhs=vtok[:, c, pr * 128:(pr + 1) * 128], start=True, stop=True),
                        reads=[bkdt, bvt[c]], writes=[bbk3])
                for pr in range(2):
                    for hh in range(2):
                        lo = hh * 64
                        P.op("act", lambda e, pr=pr, lo=lo, c=c, bk3=bk3: e.activation(
                            out=kvs[lo:lo + 64, c, pr * 64:(pr + 1) * 64],
                            in_=bk3[lo:lo + 64, pr * 128 + lo:pr * 128 + lo + 64], func=AF.Copy),
                            reads=[bbk3], writes=[bkvs[c]])
                for pr in range(2):
                    P.op("dve", lambda e, pr=pr, c=c: e.scalar_tensor_tensor(
                        out=Sst[:, pr * 64:(pr + 1) * 64], in0=Sst[:, pr * 64:(pr + 1) * 64], scalar=cd[:, pr:pr + 1],
                        in1=kvs[:, c, pr * 64:(pr + 1) * 64], op0=ALU.mult, op1=ALU.add),
                        reads=[bS, bconst, bkvs[c]], writes=[bS])

            for cc in range(4):
                chunk_body(T * 4 + cc)

        for T_ in range(NT):
            tile_body(T_)

        for j in range(2):
            P.op("act", lambda e, j=j: e.activation(out=endt[:, j:j + 1], in_=Pc[:, j, TOK - 1:TOK], func=AF.Copy),
                 reads=[bscan], writes=[bend])
            P.op("act", lambda e, j=j: e.activation(out=endt[:, 2 + j:3 + j], in_=hloc[:, j, TOK - 1:TOK], func=AF.Copy),
                 reads=[bscan], writes=[bend])
        P.op("act", lambda e: e.activation(out=endt[:, 4:132], in_=Sst[:], func=AF.Copy), reads=[bS], writes=[bend])
        bo = Buf("outs")
        co = [P.chan() for _ in range(4)]
        P.dma("sp", co[0], o_hloc.rearrange("j p t -> p j t"), hloc[:], reads=[bscan], writes=[bo])
        P.dma("act", co[1], o_P.rearrange("j p t -> p j t"), Pc[:], reads=[bscan], writes=[bo])
        P.dma("sp", co[2], o_qT.rearrange("j p t -> p j t"), qkr[:, 0:2, :], reads=bqkr, writes=[bo])
        P.dma("act", co[3], o_kT.rearrange("j p t -> p j t"), qkr[:, 2:4, :], reads=bqkr, writes=[bo])
        P.dma("sp", co[0], o_v.rearrange("c p n -> p c n"), vtok[:], reads=bvt, writes=[bo])
        P.dma("act", co[1], o_kv.rearrange("c p n -> p c n"), kvs[:], reads=bkvs, writes=[bo])
        P.dma("sp", co[2], o_end, endt[:], reads=[bend], writes=[bo])
        if debug:
            tD = (debug - 1) * 512
            bk, bbk = C.bank()
            for k in range(8):
                P.op("pe", lambda e, k=k, bk=bk: e.matmul(bk[:], lhsT=w_sb[:, k, 0:128], rhs=hT[:, k, tD:tD + 512],
                                                          start=(k == 0), stop=(k == 7)), reads=[bw] + bhT, writes=[bbk])
            P.op("act", lambda e, bk=bk: e.activation(out=ang[:], in_=bk[:], func=AF.Copy), reads=[bbk], writes=[btrig])
            o_late = C.dout("o_late", [128, 512], F32)
            o_hT = C.dout("o_hT", [128, 8, 512], BF16)
            o_w = C.dout("o_w", [128, 8, 128], BF16)
            cl = P.chan()
            bl = Buf("late")
            P.dma("sp", cl, o_late, ang[:], reads=[btrig], writes=[bl])
            P.dma("sp", cl, o_hT, hT[:, :, tD:tD + 512], reads=bhT, writes=[bl])
            P.dma("sp", cl, o_w, w_sb[:, :, 0:128], reads=[bw], writes=[bl])
            dbg_bufs.append(bl)
        P.wait_all("sp", [bo] + dbg_bufs)
        with nc.Block() as block:
            P.emit(block)
    return nc


ALPHA = float((2.0 * 2) ** 0.25)
LN_EPS = 1e-5


def consts2_np():
    c = {}
    lg = np.log1p(-np.exp2(-5.0 - np.arange(4, dtype=np.float64)))
    m = np.arange(128)[:, None].astype(np.float64)
    n = np.arange(128)[None, :].astype(np.float64)
    dec = np.zeros((128, 4, 128), np.float64)
    for h in range(4):
        dec[:, h, :] = np.where(n >= m, np.exp(np.maximum(n - m, 0.0) * lg[h]), 0.0)
    c["decayT"] = dec.astype(np.float32)
    qd = np.zeros((128, 2, 128), np.float64)
    cdT = np.zeros((128, 2), np.float64)
    for pr in range(2):
        for hh in range(2):
            h = pr * 2 + hh
            qd[hh * 64:(hh + 1) * 64, pr, :] = np.exp((np.arange(128) + 1.0) * lg[h])[None, :]
            cdT[hh * 64:(hh + 1) * 64, pr] = np.exp(2048.0 * lg[h])
    c["qdecT"] = qd.astype(np.float32)
    c["cdT"] = cdT.astype(np.float32)
    c["causal"] = np.tril(np.ones((128, 128), np.float32))
    return c


def sel_np(core):
    s = np.zeros((128, 8), np.float32)
    for j in range(8):
        if j // 4 == core // 4 and j < core:
            s[:, j] = 1.0
    return s


def build_p2(stage="full", lvl=9):
    nc = bass.Bass("TRN2", target_bir_lowering=False)
    with ExitStack() as st:
        C = Ctx(nc, st)
        P = C.P
        h_d = C.din("h", [TOK, D], F32)
        halo_d = C.din("halo", [4, D], F32)
        hloc_d = C.din("hloc", [2, 128, TOK], F32)
        Pc_d = C.din("Pc", [2, 128, TOK], F32)
        qT_d = C.din("qT", [2, 128, TOK], BF16)
        kT_d = C.din("kT", [2, 128, TOK], BF16)
        v_d = C.din("v", [NCH, 128, 256], BF16)
        kv_d = C.din("kv", [NCH, 128, 128], F32)
        ends_d = C.din("ends_all", [8, 128, 132], F32)
        sel_d = C.din("sel", [128, 8], F32)
        win_d = C.din("w_in", [D, 6656], F32)
        scw_d = C.din("sc_conv_w", [3, 256], F32)
        scb_d = C.din("sc_conv_b", [1, 256], F32)
        sgn_d = C.din("sg_norm_g", [1, 256], F32)
        sgw_d = C.din("sg_w_s", [4, 128, 128], F32)
        sgb_d = C.din("sg_b_s", [4, 128], F32)
        rng_d = C.din("ret_norm_g", [1, 256], F32)
        bp_d = C.din("branch_proj", [4, 256, D], F32)
        wout_d = C.din("w_out", [D, D], F32)
        lmg_d = C.din("ln_mix_g", [1, D], F32)
        lmb_d = C.din("ln_mix_b", [1, D], F32)
        rgw_d = C.din("router_group_w", [D, 4], F32)
        rgb_d = C.din("router_group_b", [1, 4], F32)
        rew_d = C.din("router_expert_w", [D, 32], F32)
        reb_d = C.din("router_expert_b", [1, 32], F32)
        if stage == "full":
            ewg_d = C.din("exp_w_gate", [32, D, 256], F32)
            ewu_d = C.din("exp_w_up", [32, D, 256], F32)
            ewd_d = C.din("exp_w_down", [32, 256, D], F32)
            lfg_d = C.din("ln_ffn_g", [1, D], F32)
            lfb_d = C.din("ln_ffn_b", [1, D], F32)
        identf_d = C.din("ident_f", [128, 128], F32)
        identb_d = C.din("ident_b", [128, 128], BF16)
        decayT_d = C.din("decayT", [128, 4, 128], F32)
        qdecT_d = C.din("qdecT", [128, 2, 128], F32)
        cd_d = C.din("cd", [128, 2], F32)
        cdT_d = C.din("cdT", [128, 2], F32)
        causal_d = C.din("causal", [128, 128], F32)
        o_h = C.dout("o_h", [TOK, D], F32)
        o_dbg = C.dout("o_dbg", [TOK, 32], F32) if stage != "full" else None
        o_brT = C.dout("o_brT", [8, 128, TOK], BF16) if stage != "full" else None

        hres = C.sb("hres", [128, NCH, D], F32)
        bhres = [Buf("hres%d" % c) for c in range(NCH)]
        hT = C.sb("hT", [128, 8, TOK], BF16)
        bhT = [Buf("hT%d" % c) for c in range(NCH)]
        identf = C.sb("identf", [128, 128], F32)
        identb = C.sb("identb", [128, 128], BF16)
        comb = C.sb("comb", [128, NCH, 32], F32)
        bcomb = [Buf("comb%d" % c) for c in range(NCH)]
        bconst = Buf("const")
        cst = P.chan()
        P.dma("sp", cst, identf[:], identf_d, writes=[bconst])
        P.dma("sp", cst, identb[:], identb_d, writes=[bconst])
        allchans = [cst]

        def newchan():
            ch = P.chan()
            allchans.append(ch)
            return ch

        with ExitStack() as stA:
            def sbA(name, shape, dt):
                return stA.enter_context(nc.sbuf_tensor("sa_" + name, shape, dt))

            haloT = sbA("haloT", [128, 8, 4], BF16)
            bhaloT = Buf("haloT")
            hb16 = [sbA("hb16_%d" % i, [128, D], BF16) for i in range(2)]
            bhb16 = [Buf("hb16_%d" % i) for i in range(2)]
            tcount = [0]

            def transpose_rows(src_ap, bsrc, dstT, bdst, col0, ncols=128):
                s_ = tcount[0] % 2
                tcount[0] += 1
                P.op("pool", lambda e: e.tensor_copy(out=hb16[s_][:], in_=src_ap), reads=[bsrc], writes=[bhb16[s_]])
                bk, bbk = C.bank()
                bv = bk[:].bitcast(BF16)
                for k in range(8):
                    P.op("pe", lambda e, k=k: e.transpose(out=bv[:, k * 128:(k + 1) * 128], in_=hb16[s_][:, k * 128:(k + 1) * 128],
                                                          identity=identb[:]), reads=[bhb16[s_], bconst], writes=[bbk])
                for half in range(2):
                    src = bv[:, half * 512:(half + 1) * 512].rearrange("p (k n) -> p k n", k=4)[:, :, 0:ncols]
                    dst = dstT[:, half * 4:half * 4 + 4, col0:col0 + ncols]
                    if half == 0:
                        P.op("act", lambda e, src=src, dst=dst: e.activation(out=dst, in_=src, func=AF.Copy), reads=[bbk], writes=[bdst])
                    else:
                        P.op("dve", lambda e, src=src, dst=dst: e.tensor_copy(out=dst, in_=src), reads=[bbk], writes=[bdst])

            decayT = sbA("decayT", [128, 4, 128], F32)
            qdecT = sbA("qdecT", [128, 2, 128], F32)
            cd = sbA("cd", [128, 2], F32)
            cdT = sbA("cdT", [128, 2], F32)
            causal = sbA("causal", [128, 128], F32)
            sel = sbA("sel", [128, 8], F32)
            ends = sbA("ends", [128, 8, 132], F32)
            for dst, src in ((decayT, decayT_d), (qdecT, qdecT_d), (cd, cd_d), (cdT, cdT_d), (causal, causal_d), (sel, sel_d)):
                P.dma("sp", cst, dst[:], src, writes=[bconst])
            P.dma("sp", cst, ends[:], ends_d.rearrange("r p n -> p r n"), writes=[bconst])
            scw = sbA("scw", [128, 2, 3], F32)
            scb = sbA("scb", [128, 2], F32)
            bpar = Buf("par")
            for c_ in range(2):
                P.dma("sp", cst, scw[:, c_, :], scw_d[:, c_ * 128:(c_ + 1) * 128].rearrange("j p -> p j"), writes=[bpar],
                      allow_slow_non_contiguous=True)
            P.dma("sp", cst, scb[:], scb_d.rearrange("o (c p) -> p (o c)", p=128), writes=[bpar], allow_slow_non_contiguous=True)
            sgn = sbA("sgn", [128, 256], F32)
            rng_ = sbA("rng", [128, 256], F32)
            lmg = sbA("lmg", [128, D], F32)
            lmb = sbA("lmb", [128, D], F32)
            rbias = sbA("rbias", [128, 36], F32)
            P.dma("sp", cst, sgn[:], sgn_d.partition_broadcast(128), writes=[bpar])
            P.dma("sp", cst, rng_[:], rng_d.partition_broadcast(128), writes=[bpar])
            P.dma("sp", cst, lmg[:], lmg_d.partition_broadcast(128), writes=[bpar])
            P.dma("sp", cst, lmb[:], lmb_d.partition_broadcast(128), writes=[bpar])
            P.dma("sp", cst, rbias[:, 0:4], rgb_d.partition_broadcast(128), writes=[bpar])
            P.dma("sp", cst, rbias[:, 4:36], reb_d.partition_broadcast(128), writes=[bpar])
            wr = sbA("wr", [128, 8, 36], F32)
            P.dma("sp", cst, wr[:, :, 0:4], rgw_d.rearrange("(k p) n -> p k n", p=128), writes=[bpar])
            P.dma("sp", cst, wr[:, :, 4:36], rew_d.rearrange("(k p) n -> p k n", p=128), writes=[bpar])
            bsb = sbA("bsb", [128, 2, 128], F32)
            for g in range(4):
                lo = (g % 2) * 64
                P.dma("sp", cst, bsb[lo:lo + 64, g // 2, :], sgb_d[g:g + 1, :].partition_broadcast(64), writes=[bpar])
            WcT = sbA("WcT", [128, 4, 128], BF16)
            wtmp = sbA("wtmp", [128, 128], F32)
            wtmpb = sbA("wtmpb", [128, 128], BF16)
            bwtmp = Buf("wtmp")
            for g in range(4):
                P.dma("sp", cst, wtmp[:], sgw_d[g], writes=[bwtmp])
                P.op("dve", lambda e: e.tensor_tensor(out=wtmpb[:], in0=wtmp[:], in1=causal[:], op=ALU.mult),
                     reads=[bwtmp, bconst], writes=[bwtmp])
                bk, bbk = C.bank()
                bv = bk[:].bitcast(BF16)
                P.op("pe", lambda e, bv=bv: e.transpose(out=bv[:, 0:128], in_=wtmpb[:], identity=identb[:]),
                     reads=[bwtmp, bconst], writes=[bbk])
                P.op("act", lambda e, bv=bv, g=g: e.activation(out=WcT[:, g, :], in_=bv[:, 0:128], func=AF.Copy),
                     reads=[bbk], writes=[bpar])

            hin_l = sbA("hin_l", [128, 2], F32)
            Sst = sbA("Sst", [128, 128], F32)
            Sb = sbA("Sb", [128, 128], BF16)
            ta = sbA("ta", [128, 2], F32)
            tb = sbA("tb", [128, 2], F32)
            tS = sbA("tS", [128, 128], F32)
            oms = sbA("oms", [128, 8], F32)
            bS = Buf("S")
            bcmb = Buf("cmb")
            P.op("pool", lambda e: e.memset(hin_l[:], 0.0), writes=[bcmb])
            P.op("pool", lambda e: e.memset(Sst[:], 0.0), writes=[bS])
            P.op("dve", lambda e: e.tensor_scalar(out=oms[:], in0=sel[:], scalar1=-1.0, scalar2=1.0, op0=ALU.mult, op1=ALU.add),
                 reads=[bconst], writes=[bcmb])
            for j in range(8):
                P.op("dve", lambda e, j=j: e.tensor_scalar(out=ta[:], in0=ends[:, j, 0:2], scalar1=sel[:, j:j + 1], scalar2=oms[:, j:j + 1],
                                                           op0=ALU.mult, op1=ALU.add), reads=[bconst, bcmb], writes=[bcmb])
                P.op("dve", lambda e, j=j: e.tensor_scalar(out=tb[:], in0=ends[:, j, 2:4], scalar1=sel[:, j:j + 1], scalar2=None,
                                                           op0=ALU.mult), reads=[bconst, bcmb], writes=[bcmb])
                P.op("dve", lambda e: e.tensor_tensor(out=hin_l[:], in0=hin_l[:], in1=ta[:], op=ALU.mult), reads=[bcmb], writes=[bcmb])
                P.op("dve", lambda e: e.tensor_tensor(out=hin_l[:], in0=hin_l[:], in1=tb[:], op=ALU.add), reads=[bcmb], writes=[bcmb])
                P.op("dve", lambda e, j=j: e.tensor_scalar(out=ta[:], in0=cdT[:], scalar1=sel[:, j:j + 1], scalar2=oms[:, j:j + 1],
                                                           op0=ALU.mult, op1=ALU.add), reads=[bconst, bcmb], writes=[bcmb])
                P.op("dve", lambda e, j=j: e.tensor_scalar(out=tS[:], in0=ends[:, j, 4:132], scalar1=sel[:, j:j + 1], scalar2=None,
                                                           op0=ALU.mult), reads=[bconst, bcmb], writes=[bcmb])
                for pr in range(2):
                    P.op("dve", lambda e, pr=pr: e.scalar_tensor_tensor(
                        out=Sst[:, pr * 64:(pr + 1) * 64], in0=Sst[:, pr * 64:(pr + 1) * 64], scalar=ta[:, pr:pr + 1],
                        in1=tS[:, pr * 64:(pr + 1) * 64], op0=ALU.mult, op1=ALU.add), reads=[bS, bcmb], writes=[bS])
            P.op("act", lambda e: e.activation(out=Sb[:], in_=Sst[:], func=AF.Copy), reads=[bS], writes=[bS])

            chh = [newchan() for _ in range(2)]
            h1 = sbA("h1", [128, D], F32)
            bh1 = Buf("h1")
            halo_sb = h1
            bhalo = bh1
            P.op("pool", lambda e: e.memset(halo_sb[:], 0.0), writes=[bhalo])
            P.dma("sp", chh[0], halo_sb[0:4, :], halo_d, writes=[bhalo])
            transpose_rows(halo_sb[:], bhalo, haloT, bhaloT, 0, ncols=4)
            for c in range(NCH):
                P.dma("sp" if c % 2 == 0 else "act", chh[c % 2], hres[:, c, :], h_d[c * 128:(c + 1) * 128, :], writes=[bhres[c]])
                transpose_rows(hres[:, c, :], bhres[c], hT, bhT[c], c * 128)

            NSLOT = 2
            wslot = [sbA("wslot%d" % i, [128, 8, 640], BF16) for i in range(NSLOT)]
            bws = [Buf("wslot%d" % i) for i in range(NSLOT)]
            cws = [newchan() for _ in range(NSLOT)]
            wsn = [0]

            def wload(pairs_fn):
                i = wsn[0] % NSLOT
                wsn[0] += 1
                P.dma_group("pool", cws[i], pairs_fn(wslot[i]), writes=[bws[i]])
                return wslot[i], bws[i]

            def win_cols(lo, n):
                return win_d[:, lo:lo + n].rearrange("(k p) n -> p k n", p=128)

            hl_t = sbA("hl_t", [128, 2, 512], F32)
            pc_t = sbA("pc_t", [128, 2, 512], F32)
            q_t = sbA("q_t", [128, 2, 512], BF16)
            k_t = sbA("k_t", [128, 2, 512], BF16)
            v_t = sbA("v_t", [128, 4, 256], BF16)
            kv_t = sbA("kv_t", [128, 4, 128], F32)
            bstore = Buf("store")
            cstore = newchan()

            cxl = sbA("cxl", [128, 2, 514], F32)
            bcxl = Buf("cxl")
            tmpx = sbA("tmpx", [128, 512], F32)
            accA = sbA("accA", [128, 512], F32)
            bwa = Buf("workA")
            brT = sbA("brT", [128, 8, 512], BF16)
            bbr = [Buf("br%d" % i) for i in range(8)]
            uT = sbA("uT", [128, 2, 512], F32)
            buT = Buf("uT")
            vn = sbA("vn", [128, 256], F32)
            vnb = sbA("vnb", [128, 256], BF16)
            stat = sbA("stat", [128, 4, 6], F32)
            mv = sbA("mv", [128, 4, 2], F32)
            rstd = sbA("rstd", [128, 4], F32)
            bwc = Buf("workC")
            tmpc = sbA("tmpc", [128, 128], F32)
            sm = sbA("sm", [128, 4, 128], BF16)
            qd = sbA("qd", [128, 2, 128], BF16)
            sgt = sbA("sgt", [128, 256], F32)
            on = sbA("on", [128, 256], F32)
            oraw = on
            sraw_t = hb16[0][:].bitcast(F32).rearrange("p (h n) -> p h n", h=4)
            onb = sbA("onb", [128, 256], BF16)
            bwd = Buf("workD")
            sig = tmpx
            mtmp = accA
            macc = sbA("macc", [128, 512], F32)
            bwg = bwa
            mergedT = sbA("mergedT", [128, 8, 512], BF16)
            bmg = [Buf("mg%d" % j) for j in range(8)]
            lnstat = sbA("lnstat", [128, 2, 6], F32)
            lnmv = sbA("lnmv", [128, 2], F32)
            lnr = sbA("lnr", [128, 2], F32)
            h1T = sbA("h1T", [128, 8, 128], F32)
            bln = Buf("ln")
            bh1T = Buf("h1T")
            lg = sbA("lg", [128, 36], F32)
            elm = sbA("elm", [128, 32], F32)
            top8 = sbA("top8", [128, 8], F32)
            rs = sbA("rs", [128, 16], F32)
            c1 = sbA("c1", [128, 32], F32)
            brt = Buf("router")
            cdbg = newchan()
            bdbg = Buf("dbg")

            def tile_body(T):
                t0 = T * 512
                bhT_t = bhT[T * 4:T * 4 + 4]
                if lvl < 1:
                    return
                P.dma_group("sp", cstore, [
                    (hl_t[:], hloc_d[:, :, t0:t0 + 512].rearrange("j p t -> p j t")),
                    (pc_t[:], Pc_d[:, :, t0:t0 + 512].rearrange("j p t -> p j t")),
                    (q_t[:], qT_d[:, :, t0:t0 + 512].rearrange("j p t -> p j t")),
                    (k_t[:], kT_d[:, :, t0:t0 + 512].rearrange("j p t -> p j t")),
                    (v_t[:], v_d[T * 4:T * 4 + 4].rearrange("c p n -> p c n")),
                    (kv_t[:], kv_d[T * 4:T * 4 + 4].rearrange("c p n -> p c n")),
                ], writes=[bstore])
                G0, bG0 = wload(lambda s_: [(s_[:, :, 0:512], win_cols(0, 512))])
                G1, bG1 = wload(lambda s_: [(s_[:, :, 0:256], win_cols(512, 256)), (s_[:, :, 256:512], win_cols(1024, 256))])

                def zfeat(G, bG, col, bk, bbk):
                    for k in range(8):
                        P.op("pe", lambda e, k=k: e.matmul(bk[:], lhsT=G[:, k, col:col + 128], rhs=hT[:, k, t0:t0 + 512],
                                                           start=(k == 0), stop=(k == 7)), reads=[bG] + bhT_t, writes=[bbk])

                if T == 0:
                    bkh, bbkh = C.bank()
                    for jc in range(2):
                        for wi, (G, bG, col) in enumerate(((G0, bG0, 256 + jc * 128), (G1, bG1, jc * 128))):
                            for k in range(8):
                                P.op("pe", lambda e, k=k, G=G, col=col, o_=(jc * 2 + wi) * 4: e.matmul(
                                    bkh[:, o_:o_ + 4], lhsT=G[:, k, col:col + 128], rhs=haloT[:, k, 0:4],
                                    start=(k == 0), stop=(k == 7)), reads=[bG, bhaloT], writes=[bbkh])
                    for jc in range(2):
                        P.op("act", lambda e, jc=jc: e.activation(out=tmpx[:, 0:2], in_=bkh[:, (jc * 2 + 1) * 4 + 2:(jc * 2 + 1) * 4 + 4],
                                                                  func=AF.Copy), reads=[bbkh], writes=[bwa])
                        P.op("dve", lambda e, jc=jc: e.tensor_tensor(out=cxl[:, jc, 0:2], in0=bkh[:, (jc * 2) * 4 + 2:(jc * 2) * 4 + 4],
                                                                     in1=tmpx[:, 0:2], op=ALU.mult), reads=[bbkh, bwa], writes=[bcxl])
                else:
                    P.op("pool", lambda e: e.tensor_copy(out=cxl[:, :, 0:2], in_=cxl[:, :, 512:514]), reads=[bcxl], writes=[bcxl])
                for jc in range(2):
                    bkb, bbkb = C.bank()
                    bkc, bbkc = C.bank()
                    bkx, bbkx = C.bank()
                    zfeat(G0, bG0, jc * 128, bkb, bbkb)
                    zfeat(G0, bG0, 256 + jc * 128, bkc, bbkc)
                    zfeat(G1, bG1, jc * 128, bkx, bbkx)
                    P.op("act", lambda e, bkx=bkx: e.activation(out=tmpx[:], in_=bkx[:], func=AF.Copy), reads=[bbkx], writes=[bwa])
                    P.op("dve", lambda e, jc=jc, bkc=bkc: e.tensor_tensor(out=cxl[:, jc, 2:514], in0=bkc[:], in1=tmpx[:], op=ALU.mult),
                         reads=[bbkc, bwa], writes=[bcxl])
                    P.op("dve", lambda e, jc=jc: e.tensor_scalar(out=accA[:], in0=cxl[:, jc, 0:512], scalar1=scw[:, jc, 0:1],
                                                                 scalar2=scb[:, jc:jc + 1], op0=ALU.mult, op1=ALU.add),
                         reads=[bcxl, bpar], writes=[bwa])
                    for jj in range(1, 3):
                        P.op("dve", lambda e, jc=jc, jj=jj: e.scalar_tensor_tensor(
                            out=accA[:], in0=cxl[:, jc, jj:jj + 512], scalar=scw[:, jc, jj:jj + 1], in1=accA[:],
                            op0=ALU.mult, op1=ALU.add), reads=[bcxl, bpar, bwa], writes=[bwa])
                    P.op("dve", lambda e, jc=jc, bkb=bkb: e.tensor_tensor(out=brT[:, jc, :], in0=bkb[:], in1=accA[:], op=ALU.mult),
                         reads=[bbkb, bwa], writes=[bbr[jc]])
                for j in range(2):
                    P.op("dve", lambda e, j=j: e.scalar_tensor_tensor(out=brT[:, 2 + j, :], in0=pc_t[:, j, :], scalar=hin_l[:, j:j + 1],
                                                                      in1=hl_t[:, j, :], op0=ALU.mult, op1=ALU.add),
                         reads=[bstore, bcmb], writes=[bbr[2 + j]])
                for jc in range(2):
                    bku, bbku = C.bank()
                    zfeat(G1, bG1, 256 + jc * 128, bku, bbku)
                    P.op("act", lambda e, jc=jc, bku=bku: e.activation(out=uT[:, jc, :], in_=bku[:], func=AF.Gelu_apprx_tanh),
                         reads=[bbku], writes=[buT])

                G2, bG2 = wload(lambda s_: [(s_[:, :, 0:256], win_cols(1280, 256)), (s_[:, :, 256:512], win_cols(2304, 256))])

                def chunk_body(cc):
                    c = T * 4 + cc
                    c0 = c * 128
                    l0 = cc * 128
                    bkv, bbkv = C.bank()
                    for k in range(8):
                        P.op("pe", lambda e, k=k: e.matmul(bkv[:, 0:256], lhsT=hT[:, k, c0:c0 + 128], rhs=G2[:, k, 0:256],
                                                           start=(k == 0), stop=(k == 7)), reads=[bG2, bhT[c]], writes=[bbkv])
                    P.op("act", lambda e: e.activation(out=vn[:], in_=bkv[:, 0:256], func=AF.Gelu_apprx_tanh), reads=[bbkv], writes=[bwc])
                    for g in range(4):
                        P.op("dve", lambda e, g=g: e.bn_stats(out=stat[:, g, :], in_=vn[:, g * 64:(g + 1) * 64]), reads=[bwc], writes=[bwc])
                    for g in range(4):
                        P.op("dve", lambda e, g=g: e.bn_aggr(out=mv[:, g, :], in_=stat[:, g, :]), reads=[bwc], writes=[bwc])
                    P.op("dve", lambda e: e.tensor_scalar(out=rstd[:], in0=mv[:, :, 1], scalar1=LN_EPS, scalar2=None, op0=ALU.add),
                         reads=[bwc], writes=[bwc])
                    P.op("act", lambda e: e.activation(out=rstd[:], in_=rstd[:], func=AF.Sqrt), reads=[bwc], writes=[bwc])
                    P.op("dve", lambda e: e.reciprocal(out=rstd[:], in_=rstd[:]), reads=[bwc], writes=[bwc])
                    for g in range(4):
                        P.op("dve", lambda e, g=g: e.tensor_scalar(out=vn[:, g * 64:(g + 1) * 64], in0=vn[:, g * 64:(g + 1) * 64],
                                                                   scalar1=mv[:, g, 0:1], scalar2=rstd[:, g:g + 1],
                                                                   op0=ALU.subtract, op1=ALU.mult), reads=[bwc], writes=[bwc])
                    P.op("dve", lambda e: e.tensor_tensor(out=vnb[:], in0=vn[:], in1=sgn[:], op=ALU.mult), reads=[bwc, bpar], writes=[bwc])
                    if lvl < 1.2:
                        return
                    bkc_, bbkc_ = C.bank()
                    for g in range(4):
                        lo = (g % 2) * 64
                        P.op("pe", lambda e, g=g, lo=lo: e.matmul(bkc_[lo:lo + 64, (g // 2) * 128:(g // 2) * 128 + 128],
                                                                  lhsT=vnb[:, g * 64:(g + 1) * 64], rhs=WcT[:, g, :], start=True, stop=True),
                             reads=[bwc, bpar], writes=[bbkc_])
                    if lvl < 1.3:
                        return
                    P.op("act", lambda e: e.activation(out=on[:], in_=bkc_[:, 0:256], func=AF.Copy), reads=[bbkc_, bwd], writes=[bwd])
                    for pr in range(2):
                        P.op("dve", lambda e, pr=pr: e.tensor_tensor(out=tmpc[:], in0=on[:, pr * 128:(pr + 1) * 128], in1=bsb[:, pr, :],
                                                                     op=ALU.add), reads=[bwd, bpar], writes=[bwc])
                        P.op("dve", lambda e, pr=pr: e.tensor_tensor(out=brT[:, 4 + pr, l0:l0 + 128], in0=tmpc[:], in1=uT[:, pr, l0:l0 + 128],
                                                                     op=ALU.mult), reads=[bwc, buT], writes=[bbr[4 + pr]])
                    if lvl < 1.4:
                        return
                    bks0, bbks0 = C.bank()
                    bks1, bbks1 = C.bank()
                    for h in range(4):
                        pr, hh = h // 2, h % 2
                        lo = hh * 64
                        bk_, bbk_ = (bks0, bbks0) if hh == 0 else (bks1, bbks1)
                        P.op("pe", lambda e, pr=pr, lo=lo, bk_=bk_: e.matmul(bk_[:, pr * 128:(pr + 1) * 128], lhsT=k_t[lo:lo + 64, pr, l0:l0 + 128],
                                                                             rhs=q_t[lo:lo + 64, pr, l0:l0 + 128], start=True, stop=True),
                             reads=[bstore], writes=[bbk_])
                    P.op("act", lambda e: e.activation(out=sraw_t.rearrange("p (pr hh) n -> p pr hh n", hh=2)[:, :, 0, :],
                                                       in_=bks0[:, 0:256].rearrange("p (pr n) -> p pr n", pr=2), func=AF.Copy),
                         reads=[bbks0, bwd], writes=[bwd, bhb16[0]])
                    P.op("act", lambda e: e.activation(out=sraw_t.rearrange("p (pr hh) n -> p pr hh n", hh=2)[:, :, 1, :],
                                                       in_=bks1[:, 0:256].rearrange("p (pr n) -> p pr n", pr=2), func=AF.Copy),
                         reads=[bbks1, bwd], writes=[bwd, bhb16[0]])
                    P.op("dve", lambda e: e.tensor_tensor(out=sm[:], in0=sraw_t, in1=decayT[:], op=ALU.mult),
                         reads=[bwd, bconst], writes=[bwd])
                    P.op("dve", lambda e: e.tensor_tensor(out=qd[:], in0=q_t[:, :, l0:l0 + 128], in1=qdecT[:], op=ALU.mult),
                         reads=[bstore, bconst], writes=[bwd])
                    bko0, bbko0 = C.bank()
                    bko1, bbko1 = C.bank()
                    for h in range(4):
                        pr, hh = h // 2, h % 2
                        lo = hh * 64
                        bk_, bbk_ = (bko0, bbko0) if hh == 0 else (bko1, bbko1)
                        P.op("pe", lambda e, h=h, pr=pr, bk_=bk_: e.matmul(bk_[:, pr * 64:(pr + 1) * 64], lhsT=sm[:, h, :], rhs=v_t[:, cc, h * 64:(h + 1) * 64],
                                                                           start=True, stop=False), reads=[bwd, bstore], writes=[bbk_])
                        P.op("pe", lambda e, pr=pr, lo=lo, bk_=bk_: e.matmul(bk_[:, pr * 64:(pr + 1) * 64], lhsT=qd[lo:lo + 64, pr, :],
                                                                             rhs=Sb[lo:lo + 64, pr * 64:(pr + 1) * 64], start=False, stop=True),
                             reads=[bwd, bS], writes=[bbk_])
                    P.op("act", lambda e: e.activation(out=on[:].rearrange("p (pr hh n) -> p pr hh n", pr=2, hh=2)[:, :, 0, :],
                                                       in_=bko0[:, 0:128].rearrange("p (pr n) -> p pr n", pr=2), func=AF.Copy),
                         reads=[bbko0, bwd], writes=[bwd])
                    P.op("act", lambda e: e.activation(out=on[:].rearrange("p (pr hh n) -> p pr hh n", pr=2, hh=2)[:, :, 1, :],
                                                       in_=bko1[:, 0:128].rearrange("p (pr n) -> p pr n", pr=2), func=AF.Copy),
                         reads=[bbko1, bwd], writes=[bwd])
                    if lvl < 1.6:
                        return
                    bkg, bbkg = C.bank()
                    for k in range(8):
                        P.op("pe", lambda e, k=k: e.matmul(bkg[:, 0:256], lhsT=hT[:, k, c0:c0 + 128], rhs=G2[:, k, 256:512],
                                                           start=(k == 0), stop=(k == 7)), reads=[bG2, bhT[c]], writes=[bbkg])
                    P.op("act", lambda e: e.activation(out=sgt[:], in_=bkg[:, 0:256], func=AF.Silu), reads=[bbkg], writes=[bwd])
                    for h in range(4):
                        P.op("dve", lambda e, h=h: e.bn_stats(out=stat[:, h, :], in_=oraw[:, h * 64:(h + 1) * 64]), reads=[bwd, bwc], writes=[bwc])
                    for h in range(4):
                        P.op("dve", lambda e, h=h: e.bn_aggr(out=mv[:, h, :], in_=stat[:, h, :]), reads=[bwc], writes=[bwc])
                    P.op("dve", lambda e: e.tensor_scalar(out=rstd[:], in0=mv[:, :, 1], scalar1=LN_EPS, scalar2=None, op0=ALU.add),
                         reads=[bwc], writes=[bwc])
                    P.op("act", lambda e: e.activation(out=rstd[:], in_=rstd[:], func=AF.Sqrt), reads=[bwc], writes=[bwc])
                    P.op("dve", lambda e: e.reciprocal(out=rstd[:], in_=rstd[:]), reads=[bwc], writes=[bwc])
                    for h in range(4):
                        P.op("dve", lambda e, h=h: e.tensor_scalar(out=on[:, h * 64:(h + 1) * 64], in0=oraw[:, h * 64:(h + 1) * 64],
                                                                   scalar1=mv[:, h, 0:1], scalar2=rstd[:, h:h + 1],
                                                                   op0=ALU.subtract, op1=ALU.mult), reads=[bwc, bwd], writes=[bwd])
                    P.op("dve", lambda e: e.tensor_tensor(out=on[:], in0=on[:], in1=rng_[:], op=ALU.mult), reads=[bwd, bpar], writes=[bwd])
                    P.op("dve", lambda e: e.tensor_tensor(out=onb[:], in0=on[:], in1=sgt[:], op=ALU.mult), reads=[bwd], writes=[bwd])
                    if lvl < 1.8:
                        return
                    bkt, bbkt = C.bank()
                    btv = bkt[:].bitcast(BF16)
                    for pr in range(2):
                        P.op("pe", lambda e, pr=pr: e.transpose(out=btv[:, pr * 128:(pr + 1) * 128], in_=onb[:, pr * 128:(pr + 1) * 128],
                                                                identity=identb[:]), reads=[bwd, bconst], writes=[bbkt])
                    P.op("act", lambda e: e.activation(out=brT[:, 6:8, l0:l0 + 128], in_=btv[:, 0:256].rearrange("p (j n) -> p j n", j=2),
                                                       func=AF.Copy), reads=[bbkt], writes=[bbr[6], bbr[7]])
                    for pr in range(2):
                        P.op("dve", lambda e, pr=pr: e.scalar_tensor_tensor(
                            out=Sst[:, pr * 64:(pr + 1) * 64], in0=Sst[:, pr * 64:(pr + 1) * 64], scalar=cd[:, pr:pr + 1],
                            in1=kv_t[:, cc, pr * 64:(pr + 1) * 64], op0=ALU.mult, op1=ALU.add), reads=[bS, bconst, bstore], writes=[bS])
                    P.op("act", lambda e: e.activation(out=Sb[:], in_=Sst[:], func=AF.Copy), reads=[bS], writes=[bS])

                if lvl < 1.1:
                    return
                for cc in range(4):
                    chunk_body(cc)
                if lvl < 3:
                    return

                if stage != "full":
                    P.dma("sp", cdbg, o_brT[:, :, t0:t0 + 512].rearrange("j p t -> p j t"), brT[:], reads=bbr, writes=[bdbg])
                def gate_body(j):
                    def pairs(s_):
                        pr_ = [(s_[:, :, b * 128:(b + 1) * 128], win_cols(2560 + b * 1024 + j * 128, 128)) for b in range(4)]
                        pr_ += [(s_[:, 0:2, 512 + b * 32:512 + b * 32 + 32].rearrange("p k n -> p k n"), None) for b in range(0)]
                        return pr_
                    Gj, bGj = wload(pairs)
                    i_ = (wsn[0] - 1) % NSLOT
                    P.dma_group("pool", cws[i_], [(wslot[i_][:, b * 2:b * 2 + 2, 512:640],
                                                   bp_d[b, :, j * 128:(j + 1) * 128].rearrange("(k p) n -> p k n", p=128)) for b in range(4)],
                                writes=[bGj])
                    if lvl == 3.1:
                        return
                    def gate_b(b):
                        bkz, bbkz = C.bank()
                        for k in range(8):
                            P.op("pe", lambda e, k=k, b=b: e.matmul(bkz[:], lhsT=Gj[:, k, b * 128:(b + 1) * 128], rhs=hT[:, k, t0:t0 + 512],
                                                                    start=(k == 0), stop=(k == 7)), reads=[bGj] + bhT_t, writes=[bbkz])
                        bkp, bbkp = C.bank()
                        for kc in range(2):
                            P.op("pe", lambda e, kc=kc, b=b: e.matmul(bkp[:], lhsT=Gj[:, b * 2 + kc, 512:640], rhs=brT[:, b * 2 + kc, :],
                                                                      start=(kc == 0), stop=(kc == 1)),
                                 reads=[bGj, bbr[b * 2], bbr[b * 2 + 1]], writes=[bbkp])
                        if lvl == 3.2:
                            return
                        P.op("act", lambda e: e.activation(out=sig[:], in_=bkz[:], func=AF.Tanh, scale=0.5), reads=[bbkz], writes=[bwg])
                        if lvl == 3.3:
                            return
                        if b == 0:
                            P.op("dve", lambda e: e.scalar_tensor_tensor(out=macc[:], in0=sig[:], scalar=1.0, in1=bkp[:], op0=ALU.add, op1=ALU.mult),
                                 reads=[bbkp, bwg], writes=[bwg])
                        else:
                            P.op("dve", lambda e: e.scalar_tensor_tensor(out=mtmp[:], in0=sig[:], scalar=1.0, in1=bkp[:], op0=ALU.add, op1=ALU.mult),
                                 reads=[bbkp, bwg], writes=[bwg])
                            P.op("dve", lambda e: e.tensor_tensor(out=macc[:], in0=macc[:], in1=mtmp[:], op=ALU.add), reads=[bwg], writes=[bwg])
                            if b == 3:
                                P.op("dve", lambda e: e.tensor_scalar(out=mergedT[:, j, :], in0=macc[:], scalar1=0.5, scalar2=None, op0=ALU.mult),
                                     reads=[bwg], writes=[bmg[j]])

                    for b_i in range(4):
                        gate_b(b_i)

                for j in range(8):
                    gate_body(j)
                if lvl < 4:
                    return

                def wout_half(half):
                    Wh, bWh = wload(lambda s_: [(s_[:, :, 0:512], wout_d[:, half * 512:(half + 1) * 512].rearrange("(k p) n -> p k n", p=128))])
                    for cc in range(4):
                        c = T * 4 + cc
                        bkm, bbkm = C.bank()
                        for k in range(8):
                            P.op("pe", lambda e, k=k, cc=cc, bkm=bkm: e.matmul(bkm[:], lhsT=mergedT[:, k, cc * 128:(cc + 1) * 128], rhs=Wh[:, k, 0:512],
                                                                               start=(k == 0), stop=(k == 7)), reads=[bWh] + bmg, writes=[bbkm])
                        P.op("dve", lambda e, c=c, bkm=bkm: e.scalar_tensor_tensor(
                            out=hres[:, c, half * 512:(half + 1) * 512], in0=hres[:, c, half * 512:(half + 1) * 512], scalar=ALPHA,
                            in1=bkm[:], op0=ALU.mult, op1=ALU.add), reads=[bbkm, bhres[c]], writes=[bhres[c]])

                for half_i in range(2):
                    wout_half(half_i)

                def ln_body(c):
                    for hf in range(2):
                        P.op("dve", lambda e, hf=hf: e.bn_stats(out=lnstat[:, hf, :], in_=hres[:, c, hf * 512:(hf + 1) * 512]),
                             reads=[bhres[c]], writes=[bln])
                    P.op("dve", lambda e: e.bn_aggr(out=lnmv[:], in_=lnstat[:].rearrange("p a b -> p (a b)")), reads=[bln], writes=[bln])
                    P.op("dve", lambda e: e.tensor_scalar(out=lnr[:, 0:1], in0=lnmv[:, 1:2], scalar1=LN_EPS, scalar2=None, op0=ALU.add),
                         reads=[bln], writes=[bln])
                    P.op("act", lambda e: e.activation(out=lnr[:, 0:1], in_=lnr[:, 0:1], func=AF.Sqrt), reads=[bln], writes=[bln])
                    P.op("dve", lambda e: e.reciprocal(out=lnr[:, 0:1], in_=lnr[:, 0:1]), reads=[bln], writes=[bln])
                    P.op("dve", lambda e: e.tensor_scalar(out=lnr[:, 1:2], in0=lnmv[:, 0:1], scalar1=lnr[:, 0:1], scalar2=-1.0,
                                                          op0=ALU.mult, op1=ALU.mult), reads=[bln], writes=[bln])
                    P.op("act", lambda e: e.activation(out=h1[:], in_=hres[:, c, :], func=AF.Identity, scale=lnr[:, 0:1], bias=lnr[:, 1:2]),
                         reads=[bhres[c], bln], writes=[bh1])
                    P.op("pool", lambda e: e.tensor_tensor(out=h1[:], in0=h1[:], in1=lmg[:], op=ALU.mult), reads=[bh1, bpar], writes=[bh1])
                    P.op("pool", lambda e: e.tensor_tensor(out=h1[:], in0=h1[:], in1=lmb[:], op=ALU.add), reads=[bh1, bpar], writes=[bh1])
                    P.op("act", lambda e: e.activation(out=hres[:, c, :], in_=h1[:], func=AF.Copy, scale=ALPHA), reads=[bh1], writes=[bhres[c]])
                    for half in range(2):
                        bk_, bbk_ = C.bank()
                        for k4 in range(4):
                            k = half * 4 + k4
                            P.op("pe", lambda e, k=k, k4=k4, bk_=bk_: e.transpose(out=bk_[:, k4 * 128:(k4 + 1) * 128], in_=h1[:, k * 128:(k + 1) * 128],
                                                                                  identity=identf[:]), reads=[bh1, bconst], writes=[bbk_])
                        P.op("act" if half == 0 else "dve",
                             (lambda e, bk_=bk_, half=half: e.activation(out=h1T[:, half * 4:half * 4 + 4, :],
                                                                         in_=bk_[:].rearrange("p (k n) -> p k n", k=4), func=AF.Copy)) if half == 0 else
                             (lambda e, bk_=bk_, half=half: e.tensor_copy(out=h1T[:, half * 4:half * 4 + 4, :],
                                                                          in_=bk_[:].rearrange("p (k n) -> p k n", k=4))),
                             reads=[bbk_], writes=[bh1T])
                    P.op("pool", lambda e: e.tensor_copy(out=hT[:, :, c * 128:(c + 1) * 128], in_=h1T[:]), reads=[bh1T], writes=[bhT[c]])
                    bkr, bbkr = C.bank()
                    for k in range(8):
                        P.op("pe", lambda e, k=k: e.matmul(bkr[:, 0:36], lhsT=h1T[:, k, :], rhs=wr[:, k, :], start=(k == 0), stop=(k == 7)),
                             reads=[bh1T, bpar], writes=[bbkr])
                    R_ = lambda a, b=None: rs[:, a:(a + 1 if b is None else b)]
                    P.op("dve", lambda e: e.tensor_tensor(out=lg[:], in0=bkr[:, 0:36], in1=rbias[:], op=ALU.add), reads=[bbkr, bpar], writes=[brt])
                    P.op("dve", lambda e: e.reduce_max(out=R_(0), in_=lg[:, 0:4], axis=AX.X), reads=[brt], writes=[brt])
                    P.op("dve", lambda e: e.tensor_scalar(out=R_(4, 8), in0=lg[:, 0:4], scalar1=R_(0), scalar2=None, op0=ALU.is_ge),
                         reads=[brt], writes=[brt])
                    P.op("dve", lambda e: e.tensor_scalar(out=R_(4, 8), in0=R_(4, 8), scalar1=-1.0, scalar2=1e30, op0=ALU.add, op1=ALU.mult),
                         reads=[brt], writes=[brt])
                    for g in range(4):
                        P.op("dve", lambda e, g=g: e.tensor_scalar(out=elm[:, g * 8:(g + 1) * 8], in0=lg[:, 4 + g * 8:12 + g * 8],
                                                                   scalar1=R_(4 + g), scalar2=None, op0=ALU.add), reads=[brt], writes=[brt])
                    P.op("dve", lambda e: e.max(out=top8[:], in_=elm[:]), reads=[brt], writes=[brt])
                    P.op("dve", lambda e: e.tensor_tensor(out=R_(1), in0=top8[:, 1:2], in1=top8[:, 0:1], op=ALU.subtract), reads=[brt], writes=[brt])
                    P.op("act", lambda e: e.activation(out=R_(1), in_=R_(1), func=AF.Exp), reads=[brt], writes=[brt])
                    P.op("dve", lambda e: e.tensor_scalar(out=R_(2), in0=R_(1), scalar1=1.0, scalar2=None, op0=ALU.add), reads=[brt], writes=[brt])
                    P.op("dve", lambda e: e.reciprocal(out=R_(2), in_=R_(2)), reads=[brt], writes=[brt])
                    P.op("dve", lambda e: e.tensor_tensor(out=R_(3), in0=R_(1), in1=R_(2), op=ALU.mult), reads=[brt], writes=[brt])
                    P.op("dve", lambda e: e.tensor_scalar(out=R_(8), in0=R_(0), scalar1=-1.0, scalar2=None, op0=ALU.mult), reads=[brt], writes=[brt])
                    P.op("act", lambda e: e.activation(out=R_(9, 13), in_=lg[:, 0:4], func=AF.Exp, bias=R_(8)), reads=[brt], writes=[brt])
                    P.op("dve", lambda e: e.reduce_sum(out=R_(13), in_=R_(9, 13), axis=AX.X), reads=[brt], writes=[brt])
                    P.op("dve", lambda e: e.reciprocal(out=R_(13), in_=R_(13)), reads=[brt], writes=[brt])
                    P.op("dve", lambda e: e.tensor_tensor(out=R_(2), in0=R_(2), in1=R_(13), op=ALU.mult), reads=[brt], writes=[brt])
                    P.op("dve", lambda e: e.tensor_tensor(out=R_(3), in0=R_(3), in1=R_(13), op=ALU.mult), reads=[brt], writes=[brt])
                    P.op("dve", lambda e: e.tensor_scalar(out=c1[:], in0=elm[:], scalar1=top8[:, 0:1], scalar2=R_(2), op0=ALU.is_equal, op1=ALU.mult),
                         reads=[brt], writes=[brt])
                    P.op("dve", lambda e: e.tensor_scalar(out=comb[:, c, :], in0=elm[:], scalar1=top8[:, 1:2], scalar2=R_(3), op0=ALU.is_equal,
                                                          op1=ALU.mult), reads=[brt], writes=[bcomb[c]])
                    P.op("dve", lambda e: e.tensor_tensor(out=comb[:, c, :], in0=comb[:, c, :], in1=c1[:], op=ALU.add), reads=[brt, bcomb[c]],
                         writes=[bcomb[c]])
                    if stage != "full":
                        P.dma("sp", cdbg, o_h[c * 128:(c + 1) * 128, :], h1[:], reads=[bh1], writes=[bdbg])
                        P.dma("sp", cdbg, o_dbg[c * 128:(c + 1) * 128, :], comb[:, c, :], reads=[bcomb[c]], writes=[bdbg])

                if lvl < 5:
                    return
                for cc in range(4):
                    ln_body(T * 4 + cc)

            for T_ in range(NT):
                tile_body(T_)
            if stage != "full":
                P.wait_all("sp", [bdbg])
            P.barrier(allchans)
            with nc.Block() as block:
                P.emit(block)
        if stage != "full":
            return nc
        build_p2_moe(nc, st, C, hres, bhres, hT, bhT, comb, bcomb, identb, bconst, ewg_d, ewu_d, ewd_d, lfg_d, lfb_d, o_h, allchans)
    return nc


def build_p3(nexp=32):
    nc = bass.Bass("TRN2", target_bir_lowering=False)
    with ExitStack() as st:
        C = Ctx(nc, st)
        P = C.P
        h1_d = C.din("h1", [TOK, D], F32)
        comb_d = C.din("comb", [TOK, 32], F32)
        ewg_d = C.din("exp_w_gate", [32, D, 256], F32)
        ewu_d = C.din("exp_w_up", [32, D, 256], F32)
        ewd_d = C.din("exp_w_down", [32, 256, D], F32)
        lfg_d = C.din("ln_ffn_g", [1, D], F32)
        lfb_d = C.din("ln_ffn_b", [1, D], F32)
        identb_d = C.din("ident_b", [128, 128], BF16)
        o_h = C.dout("o_h", [TOK, D], F32)

        hres = C.sb("hres", [128, NCH, D], F32)
        bhres = [Buf("hres%d" % c) for c in range(NCH)]
        hT = C.sb("hT", [128, 8, TOK], BF16)
        bhT = [Buf("hT%d" % c) for c in range(NCH)]
        comb = C.sb("comb", [128, NCH, 32], F32)
        bcomb = Buf("comb")
        identb = C.sb("identb", [128, 128], BF16)
        lfg = C.sb("lfg", [128, D], F32)
        lfb = C.sb("lfb", [128, D], F32)
        bconst = Buf("const")
        cst = P.chan()
        P.dma("sp", cst, identb[:], identb_d, writes=[bconst])
        P.dma("sp", cst, lfg[:], lfg_d.partition_broadcast(128), writes=[bconst])
        P.dma("sp", cst, lfb[:], lfb_d.partition_broadcast(128), writes=[bconst])
        P.dma("sp", cst, comb[:], comb_d.rearrange("(c p) n -> p c n", p=128), writes=[bcomb])

        hb16 = [C.sb("hb16_%d" % i, [128, D], BF16) for i in range(2)]
        bhb16 = [Buf("hb16_%d" % i) for i in range(2)]
        chh = [P.chan() for _ in range(2)]

        def load_chunk(c):
            s_ = c % 2
            P.dma("sp" if c % 2 == 0 else "act", chh[s_], hres[:, c, :], h1_d[c * 128:(c + 1) * 128, :], writes=[bhres[c]])
            P.op("pool", lambda e: e.tensor_copy(out=hb16[s_][:], in_=hres[:, c, :]), reads=[bhres[c]], writes=[bhb16[s_]])
            bk, bbk = C.bank(2, 4)
            bv = bk[:].bitcast(BF16)
            for k in range(8):
                P.op("pe", lambda e, k=k: e.transpose(out=bv[:, k * 128:(k + 1) * 128], in_=hb16[s_][:, k * 128:(k + 1) * 128],
                                                      identity=identb[:]), reads=[bhb16[s_], bconst], writes=[bbk])
            P.op("act", lambda e: e.activation(out=hT[:, 0:4, c * 128:(c + 1) * 128],
                                               in_=bv[:, 0:512].rearrange("p (k n) -> p k n", k=4), func=AF.Copy), reads=[bbk], writes=[bhT[c]])
            P.op("dve", lambda e: e.tensor_copy(out=hT[:, 4:8, c * 128:(c + 1) * 128],
                                                in_=bv[:, 512:1024].rearrange("p (k n) -> p k n", k=4)), reads=[bbk], writes=[bhT[c]])
            P.op("act", lambda e: e.activation(out=hres[:, c, :], in_=hres[:, c, :], func=AF.Copy, scale=ALPHA),
                 reads=[bhb16[s_]], writes=[bhres[c]])

        for c_ in range(NCH):
            load_chunk(c_)

        GS = 2
        NSET = 2
        wgu = [[C.sb("wgu%d_%d" % (s_, i), [128, 8, 512], BF16) for i in range(GS)] for s_ in range(NSET)]
        wdn = [[C.sb("wdn%d_%d" % (s_, i), [128, 2, D], BF16) for i in range(GS)] for s_ in range(NSET)]
        bwset = [Buf("wset%d" % s_) for s_ in range(NSET)]
        cwset = [P.chan() for _ in range(NSET)]

        def load_group(g):
            s_ = g % NSET
            pairs = []
            for i in range(GS):
                e_ = g * GS + i
                pairs.append((wgu[s_][i][:, :, 0:256], ewg_d[e_].rearrange("(k p) n -> p k n", p=128)))
                pairs.append((wgu[s_][i][:, :, 256:512], ewu_d[e_].rearrange("(k p) n -> p k n", p=128)))
                pairs.append((wdn[s_][i][:], ewd_d[e_].rearrange("(k p) n -> p k n", p=128)))
            P.dma_group("pool", cwset[s_], pairs, writes=[bwset[s_]])

        sl = [C.sb("sl%d" % i, [128, 256], F32) for i in range(2)]
        act = [C.sb("act%d" % i, [128, 256], BF16) for i in range(2)]
        actT = [C.sb("actT%d" % i, [128, 2, 128], BF16) for i in range(2)]
        bsl = [Buf("sl%d" % i) for i in range(2)]
        bact = [Buf("act%d" % i) for i in range(2)]
        bactT = [Buf("actT%d" % i) for i in range(2)]
        ngroups = nexp // GS
        items = [(g, c, i) for g in range(ngroups) for c in range(NCH) for i in range(GS)]

        def acc_of(g, c):
            a0 = 4 + ((g * NCH + c) % 2) * 2
            return [C.banks[a0], C.banks[a0 + 1]], [C.bbank[a0], C.bbank[a0 + 1]]

        gu_banks = {}

        def stage_a(n):
            g, c, i = items[n]
            s_ = g % NSET
            p = n % 2
            e_ = g * GS + i
            bk, bbk = C.banks[p], C.bbank[p]
            gu_banks[n] = (bk, bbk)
            for k in range(8):
                P.op("pe", lambda e, k=k: e.matmul(bk[:], lhsT=hT[:, k, c * 128:(c + 1) * 128], rhs=wgu[s_][i][:, k, :],
                                                   start=(k == 0), stop=(k == 7)), reads=[bhT[c], bwset[s_]], writes=[bbk])
            P.op("act", lambda e: e.activation(out=sl[p][:], in_=bk[:, 0:256], func=AF.Silu), reads=[bbk], writes=[bsl[p]])
            P.op("dve", lambda e: e.scalar_tensor_tensor(out=act[p][:], in0=sl[p][:], scalar=comb[:, c, e_:e_ + 1], in1=bk[:, 256:512],
                                                         op0=ALU.mult, op1=ALU.mult), reads=[bsl[p], bcomb, bbk], writes=[bact[p]])

        def stage_b(n):
            g, c, i = items[n]
            s_ = g % NSET
            p = n % 2
            acc, bacc = acc_of(g, c)
            bt, bbt = C.banks[2 + p], C.bbank[2 + p]
            btv = bt[:].bitcast(BF16)
            for j in range(2):
                P.op("pe", lambda e, j=j: e.transpose(out=btv[:, j * 128:(j + 1) * 128], in_=act[p][:, j * 128:(j + 1) * 128],
                                                      identity=identb[:]), reads=[bact[p], bconst], writes=[bbt])
            P.op("act", lambda e: e.activation(out=actT[p][:], in_=btv[:, 0:256].rearrange("p (j n) -> p j n", j=2), func=AF.Copy),
                 reads=[bbt], writes=[bactT[p]])
            for half in range(2):
                for kc in range(2):
                    P.op("pe", lambda e, half=half, kc=kc: e.matmul(
                        acc[half][:], lhsT=actT[p][:, kc, :], rhs=wdn[s_][i][:, kc, half * 512:(half + 1) * 512],
                        start=(i == 0 and kc == 0), stop=(i == GS - 1 and kc == 1)),
                        reads=[bactT[p], bwset[s_]], writes=[bacc[half]])
            if i == GS - 1:
                for half in range(2):
                    P.op("dve", lambda e, half=half: e.tensor_tensor(out=hres[:, c, half * 512:(half + 1) * 512], in0=acc[half][:],
                                                                     in1=hres[:, c, half * 512:(half + 1) * 512], op=ALU.add),
                         reads=[bacc[half], bhres[c]], writes=[bhres[c]])

        load_group(0)
        loaded = 0
        stage_a(0)
        for n_ in range(len(items)):
            g_now = items[n_][0]
            if n_ + 1 < len(items):
                g_next = items[n_ + 1][0]
                if g_next + 0 > loaded and items[n_][0] == g_next - 1 and False:
                    pass
                stage_a_n = n_ + 1
                g_a = items[stage_a_n][0]
                if g_a > loaded:
                    load_group(g_a)
                    loaded = g_a
                stage_a(stage_a_n)
            stage_b(n_)
            if items[n_][1] == 0 and items[n_][2] == 0 and g_now + 1 < ngroups and g_now + 1 > loaded:
                load_group(g_now + 1)
                loaded = g_now + 1

        lnstat = C.sb("lnstat", [128, 2, 6], F32)
        lnmv = C.sb("lnmv", [128, 2], F32)
        lnr = C.sb("lnr", [128, 2], F32)
        ho = [C.sb("ho%d" % i, [128, D], F32) for i in range(2)]
        bho = [Buf("ho%d" % i) for i in range(2)]
        bln = Buf("ln")
        cout = [P.chan() for _ in range(2)]
        bout = Buf("out")

        def ln_chunk(c):
            s_ = c % 2
            for hf in range(2):
                P.op("dve", lambda e, hf=hf: e.bn_stats(out=lnstat[:, hf, :], in_=hres[:, c, hf * 512:(hf + 1) * 512]),
                     reads=[bhres[c]], writes=[bln])
            P.op("dve", lambda e: e.bn_aggr(out=lnmv[:], in_=lnstat[:].rearrange("p a b -> p (a b)")), reads=[bln], writes=[bln])
            P.op("dve", lambda e: e.tensor_scalar(out=lnr[:, 0:1], in0=lnmv[:, 1:2], scalar1=LN_EPS, scalar2=None, op0=ALU.add),
                 reads=[bln], writes=[bln])
            P.op("act", lambda e: e.activation(out=lnr[:, 0:1], in_=lnr[:, 0:1], func=AF.Sqrt), reads=[bln], writes=[bln])
            P.op("dve", lambda e: e.reciprocal(out=lnr[:, 0:1], in_=lnr[:, 0:1]), reads=[bln], writes=[bln])
            P.op("dve", lambda e: e.tensor_scalar(out=lnr[:, 1:2], in0=lnmv[:, 0:1], scalar1=lnr[:, 0:1], scalar2=-1.0,
                                                  op0=ALU.mult, op1=ALU.mult), reads=[bln], writes=[bln])
            P.op("act", lambda e: e.activation(out=ho[s_][:], in_=hres[:, c, :], func=AF.Identity, scale=lnr[:, 0:1], bias=lnr[:, 1:2]),
                 reads=[bhres[c], bln], writes=[bho[s_]])
            P.op("pool", lambda e: e.tensor_tensor(out=ho[s_][:], in0=ho[s_][:], in1=lfg[:], op=ALU.mult), reads=[bho[s_], bconst], writes=[bho[s_]])
            P.op("pool", lambda e: e.tensor_tensor(out=ho[s_][:], in0=ho[s_][:], in1=lfb[:], op=ALU.add), reads=[bho[s_], bconst], writes=[bho[s_]])
            P.dma("sp", cout[s_], o_h[c * 128:(c + 1) * 128, :], ho[s_][:], reads=[bho[s_]], writes=[bout])

        for c_ in range(NCH):
            ln_chunk(c_)
        P.wait_all("sp", [bout])
        with nc.Block() as block:
            P.emit(block)
    return nc


_PROGS = {}


def _prog(name):
    if name not in _PROGS:
        _PROGS[name] = {"p1": build_p1, "p2": lambda: build_p2("A"), "p3": build_p3}[name]()
    return _PROGS[name]


def _seg(c):
    b, k = c // 4, c % 4
    return b, k * TOK


def _halo(h_full, c):
    b, s0 = _seg(c)
    halo = np.zeros((4, D), np.float32)
    if s0 > 0:
        halo[1:4] = h_full[b, s0 - 3:s0]
    return halo


def kernel(**inputs):
    inp = {k: np.asarray(v) for k, v in inputs.items()}
    cs, cs2 = consts_np(), consts2_np()
    h_full = np.ascontiguousarray(inp["x"], dtype=np.float32)
    pos = np.ascontiguousarray(inp["positions"]).astype(np.int32)
    cores = list(range(NCORES))
    for L in range(2):
        w_in = np.ascontiguousarray(inp["w_in"][L])
        w_p1 = np.ascontiguousarray(np.concatenate([w_in[:, 768:1024], w_in[:, 1536:2304]], axis=1))
        row = lambda name: np.ascontiguousarray(inp[name][L][None])
        in1 = []
        for c in cores:
            b, s0 = _seg(c)
            d = dict(h=np.ascontiguousarray(h_full[b, s0:s0 + TOK]), halo=_halo(h_full, c),
                     pos=np.ascontiguousarray(pos[b:b + 1, s0:s0 + TOK]), w_p1=w_p1,
                     lru_conv_w=np.ascontiguousarray(inp["lru_conv_w"][L]), lru_conv_b=row("lru_conv_b"),
                     lru_w_r=np.ascontiguousarray(inp["lru_w_r"][L]), lru_w_i=np.ascontiguousarray(inp["lru_w_i"][L]),
                     lru_b_r=row("lru_b_r"), lru_b_i=row("lru_b_i"), lru_lambda=row("lru_lambda"))
            for n in ("ident_f", "ident_b", "rot_b", "invf", "kdec", "cd"):
                d[n] = cs[n]
            in1.append(d)
        r1 = run_bass_kernel_spmd(_prog("p1"), in1, core_ids=cores).results
        ends_all = np.ascontiguousarray(np.stack([np.asarray(r1[c]["o_end"]) for c in cores]))
        in2 = []
        for c in cores:
            b, s0 = _seg(c)
            d = dict(h=np.ascontiguousarray(h_full[b, s0:s0 + TOK]), halo=_halo(h_full, c),
                     hloc=np.asarray(r1[c]["o_hloc"]), Pc=np.asarray(r1[c]["o_P"]), qT=np.asarray(r1[c]["o_qT"]), kT=np.asarray(r1[c]["o_kT"]),
                     v=np.asarray(r1[c]["o_v"]), kv=np.asarray(r1[c]["o_kv"]), ends_all=ends_all, sel=sel_np(c), w_in=w_in,
                     sc_conv_w=np.ascontiguousarray(inp["sc_conv_w"][L]), sc_conv_b=row("sc_conv_b"), sg_norm_g=row("sg_norm_g"),
                     sg_w_s=np.ascontiguousarray(inp["sg_w_s"][L]), sg_b_s=np.ascontiguousarray(inp["sg_b_s"][L]), ret_norm_g=row("ret_norm_g"),
                     branch_proj=np.ascontiguousarray(inp["branch_proj"][L]), w_out=np.ascontiguousarray(inp["w_out"][L]),
                     ln_mix_g=row("ln_mix_g"), ln_mix_b=row("ln_mix_b"),
                     router_group_w=np.ascontiguousarray(inp["router_group_w"][L]), router_group_b=row("router_group_b"),
                     router_expert_w=np.ascontiguousarray(inp["router_expert_w"][L]), router_expert_b=row("router_expert_b"))
            d["ident_f"], d["ident_b"], d["cd"] = cs["ident_f"], cs["ident_b"], cs["cd"]
            for n in ("decayT", "qdecT", "cdT", "causal"):
                d[n] = cs2[n]
            in2.append(d)
        r2 = run_bass_kernel_spmd(_prog("p2"), in2, core_ids=cores).results
        ewg = np.ascontiguousarray(inp["exp_w_gate"][L])
        ewu = np.ascontiguousarray(inp["exp_w_up"][L])
        ewd = np.ascontiguousarray(inp["exp_w_down"][L])
        in3 = [dict(h1=np.asarray(r2[c]["o_h"]), comb=np.asarray(r2[c]["o_dbg"]), exp_w_gate=ewg, exp_w_up=ewu, exp_w_down=ewd,
                    ln_ffn_g=row("ln_ffn_g"), ln_ffn_b=row("ln_ffn_b"), ident_b=cs["ident_b"]) for c in cores]
        r3 = run_bass_kernel_spmd(_prog("p3"), in3, core_ids=cores).results
        h_next = np.empty_like(h_full)
        for c in cores:
            b, s0 = _seg(c)
            h_next[b, s0:s0 + TOK] = np.asarray(r3[c]["o_h"])
        h_full = h_next
    return h_full.astype(np.float32)
```

```python
import numpy as np
from contextlib import ExitStack
import ml_dtypes
import concourse.bass as bass
import concourse.mybir as mybir
from concourse.bass_utils import run_bass_kernel_spmd

F32 = mybir.dt.float32
BF16 = mybir.dt.bfloat16
I32 = mybir.dt.int32
AF = mybir.ActivationFunctionType
ALU = mybir.AluOpType
AX = mybir.AxisListType

NCORES = 8
D = 1024
TOK = 2048
NCH = TOK // 128
NT = TOK // 512
GAMMA = [1.0 - 2.0 ** (-5.0 - h) for h in range(4)]


class Buf:
    __slots__ = ("w", "r", "name")

    def __init__(self, name=""):
        self.w = None
        self.r = {}
        self.name = name


class Chan:
    __slots__ = ("key", "cnt")

    def __init__(self, key):
        self.key = key
        self.cnt = 0


class Prog:
    ENG = ("pe", "act", "dve", "pool", "sp")

    def __init__(self, nc, stack):
        self.nc = nc
        self.stack = stack
        self.q = {e: [] for e in self.ENG}
        self.sems = {}
        for e in self.ENG:
            self.sems[e] = stack.enter_context(nc.semaphore("s_" + e))
        self.cnt = {e: 0 for e in self.ENG}
        self.seen = {e: {} for e in self.ENG}
        self.nchan = 0

    def chan(self):
        key = "c%d" % self.nchan
        self.nchan += 1
        self.sems[key] = self.stack.enter_context(self.nc.semaphore("s_" + key))
        return Chan(key)

    def _deps(self, eng, reads, writes, extra=()):
        need = {}

        def add(sp):
            if sp is None:
                return
            k, v = sp
            if need.get(k, 0) < v:
                need[k] = v

        for b in reads:
            add(b.w)
        for b in writes:
            add(b.w)
            for k, v in b.r.items():
                add((k, v))
        for sp in extra:
            add(sp)
        waits = []
        for k, v in need.items():
            if k == eng and eng == "pe":
                continue
            if self.seen[eng].get(k, 0) < v:
                self.seen[eng][k] = v
                waits.append((k, v))
        return waits

    def op(self, eng, fn, reads=(), writes=()):
        waits = self._deps(eng, reads, writes)
        self.cnt[eng] += 1
        v = self.cnt[eng]
        self.q[eng].append((waits, fn, eng, 1))
        for b in reads:
            if b.r.get(eng, 0) < v:
                b.r[eng] = v
        for b in writes:
            b.w = (eng, v)
            b.r = {}

    def dma(self, qeng, chan, out, in_, reads=(), writes=(), **kw):
        prev = (chan.key, chan.cnt) if chan.cnt else None
        waits = self._deps(qeng, reads, writes, extra=(prev,) if prev else ())
        chan.cnt += 16
        v = chan.cnt

        def fn(e, out=out, in_=in_, kw=kw):
            return e.dma_start(out=out, in_=in_, **kw)

        self.q[qeng].append((waits, fn, chan.key, 16))
        for b in reads:
            b.r[chan.key] = v
        for b in writes:
            b.w = (chan.key, v)
            b.r = {}

    def dma_group(self, qeng, chan, pairs, reads=(), writes=(), **kw):
        prev = (chan.key, chan.cnt) if chan.cnt else None
        waits = self._deps(qeng, reads, writes, extra=(prev,) if prev else ())
        for i, (out, in_) in enumerate(pairs):
            chan.cnt += 16

            def fn(e, out=out, in_=in_, kw=kw):
                return e.dma_start(out=out, in_=in_, **kw)

            self.q[qeng].append((waits if i == 0 else [], fn, chan.key, 16))
        v = chan.cnt
        for b in reads:
            b.r[chan.key] = v
        for b in writes:
            b.w = (chan.key, v)
            b.r = {}

    def barrier(self, chans=()):
        for e in self.ENG:
            waits = []
            for k in self.ENG:
                v = self.cnt[k]
                if k != e and v and self.seen[e].get(k, 0) < v:
                    self.seen[e][k] = v
                    waits.append((k, v))
            for ch in chans:
                if ch.cnt and self.seen[e].get(ch.key, 0) < ch.cnt:
                    self.seen[e][ch.key] = ch.cnt
                    waits.append((ch.key, ch.cnt))
            self.q[e].append((waits, None, None, 0))

    def wait_all(self, eng, bufs):
        waits = self._deps(eng, bufs, bufs)
        self.q[eng].append((waits, None, None, 0))

    def emit(self, block):
        sems = self.sems

        def run(e, items):
            for waits, fn, key, inc in items:
                for k, v in waits:
                    e.wait_ge(sems[k], v)
                if fn is not None:
                    fn(e).then_inc(sems[key], inc)

        @block.tensor
        def _(e):
            run(e, self.q["pe"])

        @block.scalar
        def _(e):
            run(e, self.q["act"])

        @block.vector
        def _(e):
            run(e, self.q["dve"])

        @block.gpsimd
        def _(e):
            run(e, self.q["pool"])

        @block.sync
        def _(e):
            run(e, self.q["sp"])

        self.q = {e: [] for e in self.ENG}


class Ctx:
    def __init__(self, nc, st):
        self.nc, self.st = nc, st
        self.P = Prog(nc, st)
        self.banks = [st.enter_context(nc.psum_tensor("bank%d" % i, [128, 512], F32)) for i in range(8)]
        self.bbank = [Buf("bank%d" % i) for i in range(8)]
        self.rr = 0

    def sb(self, name, shape, dt):
        return self.st.enter_context(self.nc.sbuf_tensor("sb_" + name, shape, dt))

    def din(self, name, shape, dt):
        return self.nc.dram_tensor(name, list(shape), dt, kind="ExternalInput").ap()

    def dout(self, name, shape, dt):
        return self.nc.dram_tensor(name, list(shape), dt, kind="ExternalOutput").ap()

    def bank(self, lo=0, hi=8):
        n = hi - lo
        i = lo + (self.rr % n)
        self.rr += 1
        return self.banks[i], self.bbank[i]


def consts_np():
    c = {}
    c["ident_f"] = np.eye(128, dtype=np.float32)
    c["ident_b"] = np.eye(128, dtype=np.float32).astype(ml_dtypes.bfloat16)
    rot = np.zeros((128, 128), np.float32)
    for h in range(2):
        for d in range(32):
            rot[h * 64 + d + 32, h * 64 + d] = -1.0
            rot[h * 64 + d, h * 64 + d + 32] = 1.0
    c["rot_b"] = rot.astype(ml_dtypes.bfloat16)
    half = 32
    invf = (10000.0 ** (-np.arange(half, dtype=np.float32) / half)).astype(np.float32)
    c["invf"] = np.tile(invf, 4).reshape(128, 1).astype(np.float32)
    lg = np.log1p(-np.exp2(-5.0 - np.arange(4, dtype=np.float64)))
    pos = np.arange(128, dtype=np.float64)
    kdec = np.exp((127.0 - pos)[:, None] * lg[None, :])
    c["kdec"] = np.repeat(kdec, 64, axis=1).astype(np.float32)
    cd = np.exp(128.0 * lg)
    c["cd"] = np.stack([np.repeat(cd[0:2], 64), np.repeat(cd[2:4], 64)], 1).astype(np.float32)
    return c


def build_p1(debug=False):
    nc = bass.Bass("TRN2", target_bir_lowering=False)
    with ExitStack() as st:
        C = Ctx(nc, st)
        P = C.P
        h_d = C.din("h", [TOK, D], F32)
        halo_d = C.din("halo", [4, D], F32)
        pos_d = C.din("pos", [1, TOK], I32)
        w_d = C.din("w_p1", [D, 1024], F32)
        cw_d = C.din("lru_conv_w", [4, 256], F32)
        cb_d = C.din("lru_conv_b", [1, 256], F32)
        wr_d = C.din("lru_w_r", [4, 64, 64], F32)
        wi_d = C.din("lru_w_i", [4, 64, 64], F32)
        br_d = C.din("lru_b_r", [1, 256], F32)
        bi_d = C.din("lru_b_i", [1, 256], F32)
        lam_d = C.din("lru_lambda", [1, 256], F32)
        identf_d = C.din("ident_f", [128, 128], F32)
        identb_d = C.din("ident_b", [128, 128], BF16)
        rot_d = C.din("rot_b", [128, 128], BF16)
        invf_d = C.din("invf", [128, 1], F32)
        kdec_d = C.din("kdec", [128, 256], F32)
        cd_d = C.din("cd", [128, 2], F32)
        o_hloc = C.dout("o_hloc", [2, 128, TOK], F32)
        o_P = C.dout("o_P", [2, 128, TOK], F32)
        o_qT = C.dout("o_qT", [2, 128, TOK], BF16)
        o_kT = C.dout("o_kT", [2, 128, TOK], BF16)
        o_v = C.dout("o_v", [NCH, 128, 256], BF16)
        o_kv = C.dout("o_kv", [NCH, 128, 128], F32)
        o_end = C.dout("o_end", [128, 4 + 128], F32)
        hT = C.sb("hT", [128, 8, TOK], BF16)
        bhT = [Buf("hT%d" % c) for c in range(NCH)]
        haloT = C.sb("haloT", [128, 8, 128], BF16)
        bhaloT = Buf("haloT")
        w_sb = C.sb("w_sb", [128, 8, 1024], BF16)
        bw = Buf("w")
        identf = C.sb("identf", [128, 128], F32)
        identb = C.sb("identb", [128, 128], BF16)
        rot = C.sb("rot", [128, 128], BF16)
        invf = C.sb("invf", [128, 1], F32)
        kdec = C.sb("kdec", [128, 256], F32)
        cd = C.sb("cd", [128, 2], F32)
        bconst = Buf("const")
        hin = [C.sb("hin%d" % i, [128, D], F32) for i in range(2)]
        bhin = [Buf("hin%d" % i) for i in range(2)]
        chin = [P.chan() for _ in range(2)]
        halo_sb = hin[1]
        posb = C.sb("posb", [128, TOK], I32)
        posf = C.sb("posf", [128, TOK], F32)
        bpos = Buf("pos")
        cw = C.sb("cw", [128, 2, 4], F32)
        cb = C.sb("cb", [128, 2], F32)
        brs = C.sb("brs", [128, 2], F32)
        bis = C.sb("bis", [128, 2], F32)
        lam = C.sb("lam", [128, 2], F32)
        cexp = C.sb("cexp", [128, 2], F32)
        cexp2 = C.sb("cexp2", [128, 2], F32)
        bdf = C.sb("bdf", [128, 2, 2, 128], F32)
        bd = C.sb("bd", [128, 2, 2, 128], BF16)
        bpar = Buf("par")
        cst = P.chan()
        cst2 = P.chan()
        for dst, src in ((identf, identf_d), (identb, identb_d), (rot, rot_d), (invf, invf_d), (kdec, kdec_d), (cd, cd_d)):
            P.dma("sp", cst, dst[:], src, writes=[bconst])
        wst = [C.sb("wst%d" % i, [128, 1024], F32) for i in range(2)]
        bwst = [Buf("wst%d" % i) for i in range(2)]
        cwst = [P.chan() for _ in range(2)]
        bwk = [Buf("w%d" % k) for k in range(8)]
        for k in range(8):
            P.dma("act", cwst[k % 2], wst[k % 2][:], w_d[k * 128:(k + 1) * 128, :], writes=[bwst[k % 2]])
            P.op("pool", lambda e, k=k: e.tensor_copy(out=w_sb[:, k, :], in_=wst[k % 2][:]), reads=[bwst[k % 2]], writes=[bwk[k], bw])
        P.dma("sp", cst, posb[:], pos_d.partition_broadcast(128), writes=[bpos])
        P.op("dve", lambda e: e.tensor_copy(out=posf[:], in_=posb[:]), reads=[bpos], writes=[bpos])
        for c_ in range(2):
            P.dma("sp", cst, cw[:, c_, :], cw_d[:, c_ * 128:(c_ + 1) * 128].rearrange("j p -> p j"), writes=[bpar],
                  allow_slow_non_contiguous=True)
        for dst, src in ((cb, cb_d), (brs, br_d), (bis, bi_d), (lam, lam_d)):
            P.dma("sp", cst, dst[:], src.rearrange("o (c p) -> p (o c)", p=128), writes=[bpar], allow_slow_non_contiguous=True)
        P.op("pool", lambda e: e.memset(bdf[:], 0.0), writes=[bpar])
        for gi, src in enumerate((wr_d, wi_d)):
            for hh in range(4):
                ch, lo = hh // 2, (hh % 2) * 64
                P.dma("sp", cst, bdf[lo:lo + 64, gi, ch, lo:lo + 64], src[hh], writes=[bpar])
        P.op("dve", lambda e: e.tensor_copy(out=bd[:], in_=bdf[:]), reads=[bpar], writes=[bpar])
        P.op("act", lambda e: e.activation(out=cexp[:], in_=lam[:], func=AF.Exp, scale=-1.0), reads=[bpar], writes=[bpar])
        P.op("act", lambda e: e.activation(out=cexp[:], in_=cexp[:], func=AF.Ln, bias=1.0), reads=[bpar], writes=[bpar])
        P.op("dve", lambda e: e.tensor_scalar(out=cexp2[:], in0=cexp[:], scalar1=-16.0, scalar2=None, op0=ALU.mult), reads=[bpar], writes=[bpar])
        P.op("dve", lambda e: e.tensor_scalar(out=cexp[:], in0=cexp[:], scalar1=-8.0, scalar2=None, op0=ALU.mult), reads=[bpar], writes=[bpar])

        hb16 = [C.sb("hb16_%d" % i, [128, D], BF16) for i in range(2)]
        bhb16 = [Buf("hb16_%d" % i) for i in range(2)]
        tcount = [0]

        def transpose_rows(src_sb, bsrc, nrows, dstT, bdst, col0):
            s_ = tcount[0] % 2
            tcount[0] += 1
            P.op("pool", lambda e, s_=s_: e.tensor_copy(out=hb16[s_][:], in_=src_sb[:]), reads=[bsrc], writes=[bhb16[s_]])
            bk, bbk = C.bank()
            bv = bk[:].bitcast(BF16)
            for k in range(8):
                P.op("pe", lambda e, k=k, bv=bv, s_=s_: e.transpose(
                    out=bv[:, k * 128:k * 128 + nrows], in_=hb16[s_][0:nrows, k * 128:(k + 1) * 128],
                    identity=identb[0:nrows, 0:nrows]), reads=[bhb16[s_], bconst], writes=[bbk])
            for half in range(2):
                src = bv[:, half * 512:(half + 1) * 512].rearrange("p (k n) -> p k n", k=4)[:, :, 0:nrows]
                dst = dstT[:, half * 4:half * 4 + 4, col0:col0 + nrows]
                if half == 0:
                    P.op("act", lambda e, src=src, dst=dst: e.activation(out=dst, in_=src, func=AF.Copy), reads=[bbk], writes=[bdst])
                else:
                    P.op("dve", lambda e, src=src, dst=dst: e.tensor_copy(out=dst, in_=src), reads=[bbk], writes=[bdst])

        bhalo = Buf("halo")
        bhalo = bhin[1]
        P.op("pool", lambda e: e.memset(halo_sb[:], 0.0), writes=[bhalo])
        P.dma("sp", chin[1], halo_sb[0:4, :], halo_d, writes=[bhalo])
        transpose_rows(halo_sb, bhalo, 128, haloT, bhaloT, 0)
        for c in range(NCH):
            s = c % 2
            P.dma("sp" if c % 2 == 0 else "act", chin[s], hin[s][:], h_d[c * 128:(c + 1) * 128, :], writes=[bhin[s]])
            transpose_rows(hin[s], bhin[s], 128, hT, bhT[c], c * 128)

        xl = C.sb("xl", [128, 2, 3 + 512], F32)
        bxl = Buf("xl")
        xc = C.sb("xc", [128, 2, 512], F32)
        xcb = C.sb("xcb", [128, 2, 512], BF16)
        bxc = Buf("xc")
        rg = C.sb("rg", [128, 2, 512], F32)
        ig = C.sb("ig", [128, 2, 512], F32)
        av = C.sb("av", [128, 2, 512], F32)
        uv = C.sb("uv", [128, 2, 512], F32)
        blru = Buf("lruwork")
        zeros = C.sb("zeros", [128, 512], F32)
        bzero = Buf("zeros")
        P.op("pool", lambda e: e.memset(zeros[:], 0.0), writes=[bzero])
        hloc = C.sb("hloc", [128, 2, TOK], F32)
        Pc = C.sb("Pc", [128, 2, TOK], F32)
        bscan = Buf("scan")
        ang = C.sb("ang", [128, 512], F32)
        tmpa = C.sb("tmpa", [128, 512], F32)
        tmpi = C.sb("tmpi", [128, 512], I32)
        cosT = C.sb("cosT", [128, 512], F32)
        sinT = C.sb("sinT", [128, 512], F32)
        btrig = Buf("trig")
        qk_f = C.sb("qk_f", [128, 4, 512], F32)
        qk_b = C.sb("qk_b", [128, 4, 512], BF16)
        bqk = Buf("qk")
        qkr = C.sb("qkr", [128, 4, TOK], BF16)
        bqkr = [Buf("qkr%d" % t) for t in range(NT)]
        vtok = C.sb("vtok", [128, NCH, 256], BF16)
        bvt = [Buf("vt%d" % c) for c in range(NCH)]
        kdt = C.sb("kdt", [128, 256], BF16)
        bkdt = Buf("kdt")
        kvs = C.sb("kvs", [128, NCH, 128], F32)
        bkvs = [Buf("kvs%d" % c) for c in range(NCH)]
        Sst = C.sb("Sst", [128, 128], F32)
        bS = Buf("S")
        P.op("pool", lambda e: e.memset(Sst[:], 0.0), writes=[bS])
        endt = C.sb("endt", [128, 4 + 128], F32)
        bend = Buf("end")
        TWO_PI = float(2.0 * np.pi)

        def dump_debug():
            o_dbg = C.dout("o_dbg", [10, 128, 512], F32)
            cdb = P.chan()
            bdbg = Buf("dbg")
            for i_, (t_, b_) in enumerate(((xc[:, 0, :], bxc), (rg[:, 0, :], blru), (ig[:, 0, :], blru), (av[:, 0, :], blru),
                                          (uv[:, 0, :], blru), (zeros[:], bzero), (xl[:, 0, 0:512], bxl), (xl[:, 1, 0:512], bxl),
                                          (xc[:, 1, :], bxc), (xl[:, 0, 3:515], bxl))):
                P.dma("sp", cdb, o_dbg[i_], t_, reads=[b_], writes=[bdbg])
            return bdbg
        dbg_bufs = []
        def tile_body(T):
            t0 = T * 512
            bhT_t = bhT[T * 4:T * 4 + 4]
            if T == 0:
                bk, bbk = C.bank()
                for j in range(2):
                    for k in range(8):
                        P.op("pe", lambda e, j=j, k=k, bk=bk: e.matmul(
                            bk[:, j * 4:j * 4 + 4], lhsT=w_sb[:, k, j * 128:(j + 1) * 128], rhs=haloT[:, k, 0:4],
                            start=(k == 0), stop=(k == 7)), reads=[bw, bhaloT], writes=[bbk])
                P.op("act", lambda e, bk=bk: e.activation(
                    out=xl[:, :, 0:3], in_=bk[:, 0:8].rearrange("p (j n) -> p j n", j=2)[:, :, 1:4], func=AF.Copy),
                    reads=[bbk], writes=[bxl])
            else:
                P.op("pool", lambda e: e.tensor_copy(out=xl[:, :, 0:3], in_=xl[:, :, 512:515]), reads=[bxl], writes=[bxl])
            for j in range(2):
                bk, bbk = C.bank()
                for k in range(8):
                    P.op("pe", lambda e, j=j, k=k, bk=bk: e.matmul(
                        bk[:], lhsT=w_sb[:, k, j * 128:(j + 1) * 128], rhs=hT[:, k, t0:t0 + 512],
                        start=(k == 0), stop=(k == 7)), reads=[bw] + bhT_t, writes=[bbk])
                P.op("act", lambda e, j=j, bk=bk: e.activation(out=xl[:, j, 3:515], in_=bk[:], func=AF.Copy),
                     reads=[bbk], writes=[bxl])
            for j in range(2):
                P.op("dve", lambda e, j=j: e.tensor_scalar(
                    out=xc[:, j, :], in0=xl[:, j, 0:512], scalar1=cw[:, j, 0:1], scalar2=cb[:, j:j + 1],
                    op0=ALU.mult, op1=ALU.add), reads=[bxl, bpar], writes=[bxc])
                for jj in range(1, 4):
                    P.op("dve", lambda e, j=j, jj=jj: e.scalar_tensor_tensor(
                        out=xc[:, j, :], in0=xl[:, j, jj:jj + 512], scalar=cw[:, j, jj:jj + 1], in1=xc[:, j, :],
                        op0=ALU.mult, op1=ALU.add), reads=[bxl, bpar, bxc], writes=[bxc])
            P.op("pool", lambda e: e.tensor_copy(out=xcb[:], in_=xc[:]), reads=[bxc], writes=[bxc])
            for j in range(2):
                for gi, (dst, bias) in enumerate(((rg, brs), (ig, bis))):
                    bk, bbk = C.bank()
                    P.op("pe", lambda e, j=j, gi=gi, bk=bk: e.matmul(
                        bk[:], lhsT=bd[:, gi, j, :], rhs=xcb[:, j, :], start=True, stop=True),
                        reads=[bpar, bxc], writes=[bbk])
                    P.op("act", lambda e, j=j, dst=dst, bias=bias, bk=bk: e.activation(
                        out=dst[:, j, :], in_=bk[:], func=AF.Sigmoid, bias=bias[:, j:j + 1]),
                        reads=[bbk, bpar], writes=[blru])
            for j in range(2):
                P.op("act", lambda e, j=j: e.activation(out=av[:, j, :], in_=rg[:, j, :], func=AF.Exp, scale=cexp[:, j:j + 1]),
                     reads=[blru, bpar], writes=[blru])
                P.op("act", lambda e, j=j: e.activation(out=uv[:, j, :], in_=rg[:, j, :], func=AF.Exp, scale=cexp2[:, j:j + 1]),
                     reads=[blru, bpar], writes=[blru])
                P.op("dve", lambda e, j=j: e.tensor_scalar(out=uv[:, j, :], in0=uv[:, j, :], scalar1=-1.0, scalar2=1.0,
                                                           op0=ALU.mult, op1=ALU.add), reads=[blru], writes=[blru])
                P.op("dve", lambda e, j=j: e.tensor_scalar_max(out=uv[:, j, :], in0=uv[:, j, :], scalar1=0.0),
                     reads=[blru], writes=[blru])
                P.op("act", lambda e, j=j: e.activation(out=uv[:, j, :], in_=uv[:, j, :], func=AF.Sqrt),
                     reads=[blru], writes=[blru])
                P.op("dve", lambda e, j=j: e.tensor_tensor(out=ig[:, j, :], in0=ig[:, j, :], in1=xc[:, j, :], op=ALU.mult),
                     reads=[blru, bxc], writes=[blru])
                P.op("dve", lambda e, j=j: e.tensor_tensor(out=uv[:, j, :], in0=uv[:, j, :], in1=ig[:, j, :], op=ALU.mult),
                     reads=[blru], writes=[blru])
                ini_h = 0.0 if T == 0 else hloc[:, j, t0 - 1:t0]
                ini_p = 1.0 if T == 0 else Pc[:, j, t0 - 1:t0]
                P.op("dve", lambda e, j=j, ini_h=ini_h: e.tensor_tensor_scan(
                    out=hloc[:, j, t0:t0 + 512], data0=av[:, j, :], data1=uv[:, j, :], initial=ini_h,
                    op0=ALU.mult, op1=ALU.add), reads=[blru, bscan], writes=[bscan])
                P.op("dve", lambda e, j=j, ini_p=ini_p: e.tensor_tensor_scan(
                    out=Pc[:, j, t0:t0 + 512], data0=av[:, j, :], data1=zeros[:], initial=ini_p,
                    op0=ALU.mult, op1=ALU.add), reads=[blru, bscan, bzero], writes=[bscan])

            if debug and T == debug - 1:
                dbg_bufs.append(dump_debug())
            P.op("dve", lambda e: e.tensor_scalar(out=ang[:], in0=posf[:, t0:t0 + 512], scalar1=invf[:, 0:1], scalar2=None,
                                                  op0=ALU.mult), reads=[bpos, bconst], writes=[btrig])
            for dst, shift in ((sinT, 0.0), (cosT, float(np.pi / 2))):
                P.op("dve", lambda e, shift=shift: e.tensor_scalar(out=tmpa[:], in0=ang[:], scalar1=shift, scalar2=1.0 / TWO_PI,
                                                                   op0=ALU.add, op1=ALU.mult), reads=[btrig], writes=[btrig])
                P.op("dve", lambda e: e.tensor_copy(out=tmpi[:], in_=tmpa[:]), reads=[btrig], writes=[btrig])
                P.op("dve", lambda e: e.tensor_copy(out=tmpa[:], in_=tmpi[:]), reads=[btrig], writes=[btrig])
                P.op("dve", lambda e: e.tensor_scalar(out=tmpa[:], in0=tmpa[:], scalar1=-TWO_PI, scalar2=None, op0=ALU.mult),
                     reads=[btrig], writes=[btrig])
                P.op("dve", lambda e, shift=shift: e.scalar_tensor_tensor(out=tmpa[:], in0=ang[:], scalar=shift, in1=tmpa[:],
                                                                          op0=ALU.add, op1=ALU.add), reads=[btrig], writes=[btrig])
                P.op("dve", lambda e, dst=dst: e.tensor_scalar(out=dst[:], in0=tmpa[:], scalar1=float(np.pi), scalar2=-TWO_PI,
                                                               op0=ALU.is_gt, op1=ALU.mult), reads=[btrig], writes=[btrig])
                P.op("dve", lambda e, dst=dst: e.tensor_tensor(out=tmpa[:], in0=tmpa[:], in1=dst[:], op=ALU.add),
                     reads=[btrig], writes=[btrig])
                P.op("dve", lambda e, dst=dst: e.tensor_scalar(out=dst[:], in0=tmpa[:], scalar1=float(-np.pi), scalar2=TWO_PI,
                                                               op0=ALU.is_lt, op1=ALU.mult), reads=[btrig], writes=[btrig])
                P.op("dve", lambda e, dst=dst: e.tensor_tensor(out=tmpa[:], in0=tmpa[:], in1=dst[:], op=ALU.add),
                     reads=[btrig], writes=[btrig])
                P.op("act", lambda e, dst=dst: e.activation(out=dst[:], in_=tmpa[:], func=AF.Sin), reads=[btrig], writes=[btrig])

            for i in range(4):
                col = 256 + i * 128
                bk, bbk = C.bank()
                for k in range(8):
                    P.op("pe", lambda e, k=k, col=col, bk=bk: e.matmul(
                        bk[:], lhsT=w_sb[:, k, col:col + 128], rhs=hT[:, k, t0:t0 + 512],
                        start=(k == 0), stop=(k == 7)), reads=[bw] + bhT_t, writes=[bbk])
                sc = 1.0 if i < 2 else 0.125
                P.op("act", lambda e, i=i, sc=sc, bk=bk: e.activation(out=qk_f[:, i, :], in_=bk[:], func=AF.Copy, scale=sc),
                     reads=[bbk], writes=[bqk])
            P.op("pool", lambda e: e.tensor_copy(out=qk_b[:], in_=qk_f[:]), reads=[bqk], writes=[bqk])
            for i in range(4):
                bk, bbk = C.bank()
                P.op("pe", lambda e, i=i, bk=bk: e.matmul(bk[:], lhsT=rot[:], rhs=qk_b[:, i, :], start=True, stop=True),
                     reads=[bconst, bqk], writes=[bbk])
                P.op("dve", lambda e, i=i, bk=bk: e.tensor_tensor(out=tmpa[:], in0=bk[:], in1=sinT[:], op=ALU.mult),
                     reads=[bbk, btrig], writes=[btrig])
                P.op("dve", lambda e, i=i: e.tensor_tensor(out=qk_f[:, i, :], in0=qk_f[:, i, :], in1=cosT[:], op=ALU.mult),
                     reads=[bqk, btrig], writes=[bqk])
                P.op("dve", lambda e, i=i: e.tensor_tensor(out=qkr[:, i, t0:t0 + 512], in0=qk_f[:, i, :], in1=tmpa[:], op=ALU.add),
                     reads=[bqk, btrig], writes=[bqkr[T]])

            if debug and T == debug - 1:
                o_qpre = C.dout("o_qpre", [128, 512], BF16)
                o_qrop = C.dout("o_qrop", [128, 512], BF16)
                cq = P.chan()
                bq_ = Buf("qdbg")
                P.dma("sp", cq, o_qpre, qk_b[:, 0, :], reads=[bqk], writes=[bq_])
                P.dma("sp", cq, o_qrop, qkr[:, 0, t0:t0 + 512], reads=[bqkr[T]], writes=[bq_])
                dbg_bufs.append(bq_)
            def chunk_body(c):
                c0 = c * 128
                bk, bbk = C.bank()
                for k in range(8):
                    P.op("pe", lambda e, k=k, c0=c0, bk=bk: e.matmul(
                        bk[:, 0:256], lhsT=hT[:, k, c0:c0 + 128], rhs=w_sb[:, k, 768:1024],
                        start=(k == 0), stop=(k == 7)), reads=[bw, bhT[c]], writes=[bbk])
                P.op("act", lambda e, c=c, bk=bk: e.activation(out=vtok[:, c, :], in_=bk[:, 0:256], func=AF.Copy),
                     reads=[bbk], writes=[bvt[c]])
                bk2, bbk2 = C.bank()
                kview = bk2[:].bitcast(BF16)
                for pr in range(2):
                    P.op("pe", lambda e, pr=pr, c0=c0, kview=kview: e.transpose(
                        out=kview[:, pr * 128:(pr + 1) * 128], in_=qkr[:, 2 + pr, c0:c0 + 128], identity=identb[:]),
                        reads=[bqkr[T], bconst], writes=[bbk2])
                P.op("dve", lambda e, kview=kview: e.tensor_tensor(out=kdt[:], in0=kview[:, 0:256], in1=kdec[:], op=ALU.mult),
                     reads=[bbk2, bconst], writes=[bkdt])
                bk3, bbk3 = C.bank()
                for pr in range(2):
                    P.op("pe", lambda e, pr=pr, c=c, bk3=bk3: e.matmul(
                        bk3[:, pr * 128:(pr + 1) * 128], lhsT=kdt[:, pr * 128:(pr + 1) * 128],
                        rhs=vtok[:, c, pr * 128:(pr + 1) * 128], start=True, stop=True),
                        reads=[bkdt, bvt[c]], writes=[bbk3])
                for pr in range(2):
                    for hh in range(2):
                        lo = hh * 64
                        P.op("act", lambda e, pr=pr, lo=lo, c=c, bk3=bk3: e.activation(
                            out=kvs[lo:lo + 64, c, pr * 64:(pr + 1) * 64],
                            in_=bk3[lo:lo + 64, pr * 128 + lo:pr * 128 + lo + 64], func=AF.Copy),
                            reads=[bbk3], writes=[bkvs[c]])
                for pr in range(2):
                    P.op("dve", lambda e, pr=pr, c=c: e.scalar_tensor_tensor(
                        out=Sst[:, pr * 64:(pr + 1) * 64], in0=Sst[:, pr * 64:(pr + 1) * 64], scalar=cd[:, pr:pr + 1],
                        in1=kvs[:, c, pr * 64:(pr + 1) * 64], op0=ALU.mult, op1=ALU.add),
                        reads=[bS, bconst, bkvs[c]], writes=[bS])

            for cc in range(4):
                chunk_body(T * 4 + cc)

        for T_ in range(NT):
            tile_body(T_)

        for j in range(2):
            P.op("act", lambda e, j=j: e.activation(out=endt[:, j:j + 1], in_=Pc[:, j, TOK - 1:TOK], func=AF.Copy),
                 reads=[bscan], writes=[bend])
            P.op("act", lambda e, j=j: e.activation(out=endt[:, 2 + j:3 + j], in_=hloc[:, j, TOK - 1:TOK], func=AF.Copy),
                 reads=[bscan], writes=[bend])
        P.op("act", lambda e: e.activation(out=endt[:, 4:132], in_=Sst[:], func=AF.Copy), reads=[bS], writes=[bend])
        bo = Buf("outs")
        co = [P.chan() for _ in range(4)]
        P.dma("sp", co[0], o_hloc.rearrange("j p t -> p j t"), hloc[:], reads=[bscan], writes=[bo])
        P.dma("act", co[1], o_P.rearrange("j p t -> p j t"), Pc[:], reads=[bscan], writes=[bo])
        P.dma("sp", co[2], o_qT.rearrange("j p t -> p j t"), qkr[:, 0:2, :], reads=bqkr, writes=[bo])
        P.dma("act", co[3], o_kT.rearrange("j p t -> p j t"), qkr[:, 2:4, :], reads=bqkr, writes=[bo])
        P.dma("sp", co[0], o_v.rearrange("c p n -> p c n"), vtok[:], reads=bvt, writes=[bo])
        P.dma("act", co[1], o_kv.rearrange("c p n -> p c n"), kvs[:], reads=bkvs, writes=[bo])
        P.dma("sp", co[2], o_end, endt[:], reads=[bend], writes=[bo])
        if debug:
            tD = (debug - 1) * 512
            bk, bbk = C.bank()
            for k in range(8):
                P.op("pe", lambda e, k=k, bk=bk: e.matmul(bk[:], lhsT=w_sb[:, k, 0:128], rhs=hT[:, k, tD:tD + 512],
                                                          start=(k == 0), stop=(k == 7)), reads=[bw] + bhT, writes=[bbk])
            P.op("act", lambda e, bk=bk: e.activation(out=ang[:], in_=bk[:], func=AF.Copy), reads=[bbk], writes=[btrig])
            o_late = C.dout("o_late", [128, 512], F32)
            o_hT = C.dout("o_hT", [128, 8, 512], BF16)
            o_w = C.dout("o_w", [128, 8, 128], BF16)
            cl = P.chan()
            bl = Buf("late")
            P.dma("sp", cl, o_late, ang[:], reads=[btrig], writes=[bl])
            P.dma("sp", cl, o_hT, hT[:, :, tD:tD + 512], reads=bhT, writes=[bl])
            P.dma("sp", cl, o_w, w_sb[:, :, 0:128], reads=[bw], writes=[bl])
            dbg_bufs.append(bl)
        P.wait_all("sp", [bo] + dbg_bufs)
        with nc.Block() as block:
            P.emit(block)
    return nc


ALPHA = float((2.0 * 2) ** 0.25)
LN_EPS = 1e-5


def consts2_np():
    c = {}
    lg = np.log1p(-np.exp2(-5.0 - np.arange(4, dtype=np.float64)))
    m = np.arange(128)[:, None].astype(np.float64)
    n = np.arange(128)[None, :].astype(np.float64)
    dec = np.zeros((128, 4, 128), np.float64)
    for h in range(4):
        dec[:, h, :] = np.where(n >= m, np.exp(np.maximum(n - m, 0.0) * lg[h]), 0.0)
    c["decayT"] = dec.astype(np.float32)
    qd = np.zeros((128, 2, 128), np.float64)
    cdT = np.zeros((128, 2), np.float64)
    for pr in range(2):
        for hh in range(2):
            h = pr * 2 + hh
            qd[hh * 64:(hh + 1) * 64, pr, :] = np.exp((np.arange(128) + 1.0) * lg[h])[None, :]
            cdT[hh * 64:(hh + 1) * 64, pr] = np.exp(2048.0 * lg[h])
    c["qdecT"] = qd.astype(np.float32)
    c["cdT"] = cdT.astype(np.float32)
    c["causal"] = np.tril(np.ones((128, 128), np.float32))
    return c


def sel_np(core):
    s = np.zeros((128, 8), np.float32)
    for j in range(8):
        if j // 4 == core // 4 and j < core:
            s[:, j] = 1.0
    return s


def build_p2(stage="full", lvl=9):
    nc = bass.Bass("TRN2", target_bir_lowering=False)
    with ExitStack() as st:
        C = Ctx(nc, st)
        P = C.P
        h_d = C.din("h", [TOK, D], F32)
        halo_d = C.din("halo", [4, D], F32)
        hloc_d = C.din("hloc", [2, 128, TOK], F32)
        Pc_d = C.din("Pc", [2, 128, TOK], F32)
        qT_d = C.din("qT", [2, 128, TOK], BF16)
        kT_d = C.din("kT", [2, 128, TOK], BF16)
        v_d = C.din("v", [NCH, 128, 256], BF16)
        kv_d = C.din("kv", [NCH, 128, 128], F32)
        ends_d = C.din("ends_all", [8, 128, 132], F32)
        sel_d = C.din("sel", [128, 8], F32)
        win_d = C.din("w_in", [D, 6656], F32)
        scw_d = C.din("sc_conv_w", [3, 256], F32)
        scb_d = C.din("sc_conv_b", [1, 256], F32)
        sgn_d = C.din("sg_norm_g", [1, 256], F32)
        sgw_d = C.din("sg_w_s", [4, 128, 128], F32)
        sgb_d = C.din("sg_b_s", [4, 128], F32)
        rng_d = C.din("ret_norm_g", [1, 256], F32)
        bp_d = C.din("branch_proj", [4, 256, D], F32)
        wout_d = C.din("w_out", [D, D], F32)
        lmg_d = C.din("ln_mix_g", [1, D], F32)
        lmb_d = C.din("ln_mix_b", [1, D], F32)
        rgw_d = C.din("router_group_w", [D, 4], F32)
        rgb_d = C.din("router_group_b", [1, 4], F32)
        rew_d = C.din("router_expert_w", [D, 32], F32)
        reb_d = C.din("router_expert_b", [1, 32], F32)
        if stage == "full":
            ewg_d = C.din("exp_w_gate", [32, D, 256], F32)
            ewu_d = C.din("exp_w_up", [32, D, 256], F32)
            ewd_d = C.din("exp_w_down", [32, 256, D], F32)
            lfg_d = C.din("ln_ffn_g", [1, D], F32)
            lfb_d = C.din("ln_ffn_b", [1, D], F32)
        identf_d = C.din("ident_f", [128, 128], F32)
        identb_d = C.din("ident_b", [128, 128], BF16)
        decayT_d = C.din("decayT", [128, 4, 128], F32)
        qdecT_d = C.din("qdecT", [128, 2, 128], F32)
        cd_d = C.din("cd", [128, 2], F32)
        cdT_d = C.din("cdT", [128, 2], F32)
        causal_d = C.din("causal", [128, 128], F32)
        o_h = C.dout("o_h", [TOK, D], F32)
        o_dbg = C.dout("o_dbg", [TOK, 32], F32) if stage != "full" else None
        o_brT = C.dout("o_brT", [8, 128, TOK], BF16) if stage != "full" else None

        hres = C.sb("hres", [128, NCH, D], F32)
        bhres = [Buf("hres%d" % c) for c in range(NCH)]
        hT = C.sb("hT", [128, 8, TOK], BF16)
        bhT = [Buf("hT%d" % c) for c in range(NCH)]
        identf = C.sb("identf", [128, 128], F32)
        identb = C.sb("identb", [128, 128], BF16)
        comb = C.sb("comb", [128, NCH, 32], F32)
        bcomb = [Buf("comb%d" % c) for c in range(NCH)]
        bconst = Buf("const")
        cst = P.chan()
        P.dma("sp", cst, identf[:], identf_d, writes=[bconst])
        P.dma("sp", cst, identb[:], identb_d, writes=[bconst])
        allchans = [cst]

        def newchan():
            ch = P.chan()
            allchans.append(ch)
            return ch

        with ExitStack() as stA:
            def sbA(name, shape, dt):
                return stA.enter_context(nc.sbuf_tensor("sa_" + name, shape, dt))

            haloT = sbA("haloT", [128, 8, 4], BF16)
            bhaloT = Buf("haloT")
            hb16 = [sbA("hb16_%d" % i, [128, D], BF16) for i in range(2)]
            bhb16 = [Buf("hb16_%d" % i) for i in range(2)]
            tcount = [0]

            def transpose_rows(src_ap, bsrc, dstT, bdst, col0, ncols=128):
                s_ = tcount[0] % 2
                tcount[0] += 1
                P.op("pool", lambda e: e.tensor_copy(out=hb16[s_][:], in_=src_ap), reads=[bsrc], writes=[bhb16[s_]])
                bk, bbk = C.bank()
                bv = bk[:].bitcast(BF16)
                for k in range(8):
                    P.op("pe", lambda e, k=k: e.transpose(out=bv[:, k * 128:(k + 1) * 128], in_=hb16[s_][:, k * 128:(k + 1) * 128],
                                                          identity=identb[:]), reads=[bhb16[s_], bconst], writes=[bbk])
                for half in range(2):
                    src = bv[:, half * 512:(half + 1) * 512].rearrange("p (k n) -> p k n", k=4)[:, :, 0:ncols]
                    dst = dstT[:, half * 4:half * 4 + 4, col0:col0 + ncols]
                    if half == 0:
                        P.op("act", lambda e, src=src, dst=dst: e.activation(out=dst, in_=src, func=AF.Copy), reads=[bbk], writes=[bdst])
                    else:
                        P.op("dve", lambda e, src=src, dst=dst: e.tensor_copy(out=dst, in_=src), reads=[bbk], writes=[bdst])

            decayT = sbA("decayT", [128, 4, 128], F32)
            qdecT = sbA("qdecT", [128, 2, 128], F32)
            cd = sbA("cd", [128, 2], F32)
            cdT = sbA("cdT", [128, 2], F32)
            causal = sbA("causal", [128, 128], F32)
            sel = sbA("sel", [128, 8], F32)
            ends = sbA("ends", [128, 8, 132], F32)
            for dst, src in ((decayT, decayT_d), (qdecT, qdecT_d), (cd, cd_d), (cdT, cdT_d), (causal, causal_d), (sel, sel_d)):
                P.dma("sp", cst, dst[:], src, writes=[bconst])
            P.dma("sp", cst, ends[:], ends_d.rearrange("r p n -> p r n"), writes=[bconst])
            scw = sbA("scw", [128, 2, 3], F32)
            scb = sbA("scb", [128, 2], F32)
            bpar = Buf("par")
            for c_ in range(2):
                P.dma("sp", cst, scw[:, c_, :], scw_d[:, c_ * 128:(c_ + 1) * 128].rearrange("j p -> p j"), writes=[bpar],
                      allow_slow_non_contiguous=True)
            P.dma("sp", cst, scb[:], scb_d.rearrange("o (c p) -> p (o c)", p=128), writes=[bpar], allow_slow_non_contiguous=True)
            sgn = sbA("sgn", [128, 256], F32)
            rng_ = sbA("rng", [128, 256], F32)
            lmg = sbA("lmg", [128, D], F32)
            lmb = sbA("lmb", [128, D], F32)
            rbias = sbA("rbias", [128, 36], F32)
            P.dma("sp", cst, sgn[:], sgn_d.partition_broadcast(128), writes=[bpar])
            P.dma("sp", cst, rng_[:], rng_d.partition_broadcast(128), writes=[bpar])
            P.dma("sp", cst, lmg[:], lmg_d.partition_broadcast(128), writes=[bpar])
            P.dma("sp", cst, lmb[:], lmb_d.partition_broadcast(128), writes=[bpar])
            P.dma("sp", cst, rbias[:, 0:4], rgb_d.partition_broadcast(128), writes=[bpar])
            P.dma("sp", cst, rbias[:, 4:36], reb_d.partition_broadcast(128), writes=[bpar])
            wr = sbA("wr", [128, 8, 36], F32)
            P.dma("sp", cst, wr[:, :, 0:4], rgw_d.rearrange("(k p) n -> p k n", p=128), writes=[bpar])
            P.dma("sp", cst, wr[:, :, 4:36], rew_d.rearrange("(k p) n -> p k n", p=128), writes=[bpar])
            bsb = sbA("bsb", [128, 2, 128], F32)
            for g in range(4):
                lo = (g % 2) * 64
                P.dma("sp", cst, bsb[lo:lo + 64, g // 2, :], sgb_d[g:g + 1, :].partition_broadcast(64), writes=[bpar])
            WcT = sbA("WcT", [128, 4, 128], BF16)
            wtmp = sbA("wtmp", [128, 128], F32)
            wtmpb = sbA("wtmpb", [128, 128], BF16)
            bwtmp = Buf("wtmp")
            for g in range(4):
                P.dma("sp", cst, wtmp[:], sgw_d[g], writes=[bwtmp])
                P.op("dve", lambda e: e.tensor_tensor(out=wtmpb[:], in0=wtmp[:], in1=causal[:], op=ALU.mult),
                     reads=[bwtmp, bconst], writes=[bwtmp])
                bk, bbk = C.bank()
                bv = bk[:].bitcast(BF16)
                P.op("pe", lambda e, bv=bv: e.transpose(out=bv[:, 0:128], in_=wtmpb[:], identity=identb[:]),
                     reads=[bwtmp, bconst], writes=[bbk])
                P.op("act", lambda e, bv=bv, g=g: e.activation(out=WcT[:, g, :], in_=bv[:, 0:128], func=AF.Copy),
                     reads=[bbk], writes=[bpar])

            hin_l = sbA("hin_l", [128, 2], F32)
            Sst = sbA("Sst", [128, 128], F32)
            Sb = sbA("Sb", [128, 128], BF16)
            ta = sbA("ta", [128, 2], F32)
            tb = sbA("tb", [128, 2], F32)
            tS = sbA("tS", [128, 128], F32)
            oms = sbA("oms", [128, 8], F32)
            bS = Buf("S")
            bcmb = Buf("cmb")
            P.op("pool", lambda e: e.memset(hin_l[:], 0.0), writes=[bcmb])
            P.op("pool", lambda e: e.memset(Sst[:], 0.0), writes=[bS])
            P.op("dve", lambda e: e.tensor_scalar(out=oms[:], in0=sel[:], scalar1=-1.0, scalar2=1.0, op0=ALU.mult, op1=ALU.add),
                 reads=[bconst], writes=[bcmb])
            for j in range(8):
                P.op("dve", lambda e, j=j: e.tensor_scalar(out=ta[:], in0=ends[:, j, 0:2], scalar1=sel[:, j:j + 1], scalar2=oms[:, j:j + 1],
                                                           op0=ALU.mult, op1=ALU.add), reads=[bconst, bcmb], writes=[bcmb])
                P.op("dve", lambda e, j=j: e.tensor_scalar(out=tb[:], in0=ends[:, j, 2:4], scalar1=sel[:, j:j + 1], scalar2=None,
                                                           op0=ALU.mult), reads=[bconst, bcmb], writes=[bcmb])
                P.op("dve", lambda e: e.tensor_tensor(out=hin_l[:], in0=hin_l[:], in1=ta[:], op=ALU.mult), reads=[bcmb], writes=[bcmb])
                P.op("dve", lambda e: e.tensor_tensor(out=hin_l[:], in0=hin_l[:], in1=tb[:], op=ALU.add), reads=[bcmb], writes=[bcmb])
                P.op("dve", lambda e, j=j: e.tensor_scalar(out=ta[:], in0=cdT[:], scalar1=sel[:, j:j + 1], scalar2=oms[:, j:j + 1],
                                                           op0=ALU.mult, op1=ALU.add), reads=[bconst, bcmb], writes=[bcmb])
                P.op("dve", lambda e, j=j: e.tensor_scalar(out=tS[:], in0=ends[:, j, 4:132], scalar1=sel[:, j:j + 1], scalar2=None,
                                                           op0=ALU.mult), reads=[bconst, bcmb], writes=[bcmb])
                for pr in range(2):
                    P.op("dve", lambda e, pr=pr: e.scalar_tensor_tensor(
                        out=Sst[:, pr * 64:(pr + 1) * 64], in0=Sst[:, pr * 64:(pr + 1) * 64], scalar=ta[:, pr:pr + 1],
                        in1=tS[:, pr * 64:(pr + 1) * 64], op0=ALU.mult, op1=ALU.add), reads=[bS, bcmb], writes=[bS])
            P.op("act", lambda e: e.activation(out=Sb[:], in_=Sst[:], func=AF.Copy), reads=[bS], writes=[bS])

            chh = [newchan() for _ in range(2)]
            h1 = sbA("h1", [128, D], F32)
            bh1 = Buf("h1")
            halo_sb = h1
            bhalo = bh1
            P.op("pool", lambda e: e.memset(halo_sb[:], 0.0), writes=[bhalo])
            P.dma("sp", chh[0], halo_sb[0:4, :], halo_d, writes=[bhalo])
            transpose_rows(halo_sb[:], bhalo, haloT, bhaloT, 0, ncols=4)
            for c in range(NCH):
                P.dma("sp" if c % 2 == 0 else "act", chh[c % 2], hres[:, c, :], h_d[c * 128:(c + 1) * 128, :], writes=[bhres[c]])
                transpose_rows(hres[:, c, :], bhres[c], hT, bhT[c], c * 128)

            NSLOT = 2
            wslot = [sbA("wslot%d" % i, [128, 8, 640], BF16) for i in range(NSLOT)]
            bws = [Buf("wslot%d" % i) for i in range(NSLOT)]
            cws = [newchan() for _ in range(NSLOT)]
            wsn = [0]

            def wload(pairs_fn):
                i = wsn[0] % NSLOT
                wsn[0] += 1
                P.dma_group("pool", cws[i], pairs_fn(wslot[i]), writes=[bws[i]])
                return wslot[i], bws[i]

            def win_cols(lo, n):
                return win_d[:, lo:lo + n].rearrange("(k p) n -> p k n", p=128)

            hl_t = sbA("hl_t", [128, 2, 512], F32)
            pc_t = sbA("pc_t", [128, 2, 512], F32)
            q_t = sbA("q_t", [128, 2, 512], BF16)
            k_t = sbA("k_t", [128, 2, 512], BF16)
            v_t = sbA("v_t", [128, 4, 256], BF16)
            kv_t = sbA("kv_t", [128, 4, 128], F32)
            bstore = Buf("store")
            cstore = newchan()

            cxl = sbA("cxl", [128, 2, 514], F32)
            bcxl = Buf("cxl")
            tmpx = sbA("tmpx", [128, 512], F32)
            accA = sbA("accA", [128, 512], F32)
            bwa = Buf("workA")
            brT = sbA("brT", [128, 8, 512], BF16)
            bbr = [Buf("br%d" % i) for i in range(8)]
            uT = sbA("uT", [128, 2, 512], F32)
            buT = Buf("uT")
            vn = sbA("vn", [128, 256], F32)
            vnb = sbA("vnb", [128, 256], BF16)
            stat = sbA("stat", [128, 4, 6], F32)
            mv = sbA("mv", [128, 4, 2], F32)
            rstd = sbA("rstd", [128, 4], F32)
            bwc = Buf("workC")
            tmpc = sbA("tmpc", [128, 128], F32)
            sm = sbA("sm", [128, 4, 128], BF16)
            qd = sbA("qd", [128, 2, 128], BF16)
            sgt = sbA("sgt", [128, 256], F32)
            on = sbA("on", [128, 256], F32)
            oraw = on
            sraw_t = hb16[0][:].bitcast(F32).rearrange("p (h n) -> p h n", h=4)
            onb = sbA("onb", [128, 256], BF16)
            bwd = Buf("workD")
            sig = tmpx
            mtmp = accA
            macc = sbA("macc", [128, 512], F32)
            bwg = bwa
            mergedT = sbA("mergedT", [128, 8, 512], BF16)
            bmg = [Buf("mg%d" % j) for j in range(8)]
            lnstat = sbA("lnstat", [128, 2, 6], F32)
            lnmv = sbA("lnmv", [128, 2], F32)
            lnr = sbA("lnr", [128, 2], F32)
            h1T = sbA("h1T", [128, 8, 128], F32)
            bln = Buf("ln")
            bh1T = Buf("h1T")
            lg = sbA("lg", [128, 36], F32)
            elm = sbA("elm", [128, 32], F32)
            top8 = sbA("top8", [128, 8], F32)
            rs = sbA("rs", [128, 16], F32)
            c1 = sbA("c1", [128, 32], F32)
            brt = Buf("router")
            cdbg = newchan()
            bdbg = Buf("dbg")

            def tile_body(T):
                t0 = T * 512
                bhT_t = bhT[T * 4:T * 4 + 4]
                if lvl < 1:
                    return
                P.dma_group("sp", cstore, [
                    (hl_t[:], hloc_d[:, :, t0:t0 + 512].rearrange("j p t -> p j t")),
                    (pc_t[:], Pc_d[:, :, t0:t0 + 512].rearrange("j p t -> p j t")),
                    (q_t[:], qT_d[:, :, t0:t0 + 512].rearrange("j p t -> p j t")),
                    (k_t[:], kT_d[:, :, t0:t0 + 512].rearrange("j p t -> p j t")),
                    (v_t[:], v_d[T * 4:T * 4 + 4].rearrange("c p n -> p c n")),
                    (kv_t[:], kv_d[T * 4:T * 4 + 4].rearrange("c p n -> p c n")),
                ], writes=[bstore])
                G0, bG0 = wload(lambda s_: [(s_[:, :, 0:512], win_cols(0, 512))])
                G1, bG1 = wload(lambda s_: [(s_[:, :, 0:256], win_cols(512, 256)), (s_[:, :, 256:512], win_cols(1024, 256))])

                def zfeat(G, bG, col, bk, bbk):
                    for k in range(8):
                        P.op("pe", lambda e, k=k: e.matmul(bk[:], lhsT=G[:, k, col:col + 128], rhs=hT[:, k, t0:t0 + 512],
                                                           start=(k == 0), stop=(k == 7)), reads=[bG] + bhT_t, writes=[bbk])

                if T == 0:
                    bkh, bbkh = C.bank()
                    for jc in range(2):
                        for wi, (G, bG, col) in enumerate(((G0, bG0, 256 + jc * 128), (G1, bG1, jc * 128))):
                            for k in range(8):
                                P.op("pe", lambda e, k=k, G=G, col=col, o_=(jc * 2 + wi) * 4: e.matmul(
                                    bkh[:, o_:o_ + 4], lhsT=G[:, k, col:col + 128], rhs=haloT[:, k, 0:4],
                                    start=(k == 0), stop=(k == 7)), reads=[bG, bhaloT], writes=[bbkh])
                    for jc in range(2):
                        P.op("act", lambda e, jc=jc: e.activation(out=tmpx[:, 0:2], in_=bkh[:, (jc * 2 + 1) * 4 + 2:(jc * 2 + 1) * 4 + 4],
                                                                  func=AF.Copy), reads=[bbkh], writes=[bwa])
                        P.op("dve", lambda e, jc=jc: e.tensor_tensor(out=cxl[:, jc, 0:2], in0=bkh[:, (jc * 2) * 4 + 2:(jc * 2) * 4 + 4],
                                                                     in1=tmpx[:, 0:2], op=ALU.mult), reads=[bbkh, bwa], writes=[bcxl])
                else:
                    P.op("pool", lambda e: e.tensor_copy(out=cxl[:, :, 0:2], in_=cxl[:, :, 512:514]), reads=[bcxl], writes=[bcxl])
                for jc in range(2):
                    bkb, bbkb = C.bank()
                    bkc, bbkc = C.bank()
                    bkx, bbkx = C.bank()
                    zfeat(G0, bG0, jc * 128, bkb, bbkb)
                    zfeat(G0, bG0, 256 + jc * 128, bkc, bbkc)
                    zfeat(G1, bG1, jc * 128, bkx, bbkx)
                    P.op("act", lambda e, bkx=bkx: e.activation(out=tmpx[:], in_=bkx[:], func=AF.Copy), reads=[bbkx], writes=[bwa])
                    P.op("dve", lambda e, jc=jc, bkc=bkc: e.tensor_tensor(out=cxl[:, jc, 2:514], in0=bkc[:], in1=tmpx[:], op=ALU.mult),
                         reads=[bbkc, bwa], writes=[bcxl])
                    P.op("dve", lambda e, jc=jc: e.tensor_scalar(out=accA[:], in0=cxl[:, jc, 0:512], scalar1=scw[:, jc, 0:1],
                                                                 scalar2=scb[:, jc:jc + 1], op0=ALU.mult, op1=ALU.add),
                         reads=[bcxl, bpar], writes=[bwa])
                    for jj in range(1, 3):
                        P.op("dve", lambda e, jc=jc, jj=jj: e.scalar_tensor_tensor(
                            out=accA[:], in0=cxl[:, jc, jj:jj + 512], scalar=scw[:, jc, jj:jj + 1], in1=accA[:],
                            op0=ALU.mult, op1=ALU.add), reads=[bcxl, bpar, bwa], writes=[bwa])
                    P.op("dve", lambda e, jc=jc, bkb=bkb: e.tensor_tensor(out=brT[:, jc, :], in0=bkb[:], in1=accA[:], op=ALU.mult),
                         reads=[bbkb, bwa], writes=[bbr[jc]])
                for j in range(2):
                    P.op("dve", lambda e, j=j: e.scalar_tensor_tensor(out=brT[:, 2 + j, :], in0=pc_t[:, j, :], scalar=hin_l[:, j:j + 1],
                                                                      in1=hl_t[:, j, :], op0=ALU.mult, op1=ALU.add),
                         reads=[bstore, bcmb], writes=[bbr[2 + j]])
                for jc in range(2):
                    bku, bbku = C.bank()
                    zfeat(G1, bG1, 256 + jc * 128, bku, bbku)
                    P.op("act", lambda e, jc=jc, bku=bku: e.activation(out=uT[:, jc, :], in_=bku[:], func=AF.Gelu_apprx_tanh),
                         reads=[bbku], writes=[buT])

                G2, bG2 = wload(lambda s_: [(s_[:, :, 0:256], win_cols(1280, 256)), (s_[:, :, 256:512], win_cols(2304, 256))])

                def chunk_body(cc):
                    c = T * 4 + cc
                    c0 = c * 128
                    l0 = cc * 128
                    bkv, bbkv = C.bank()
                    for k in range(8):
                        P.op("pe", lambda e, k=k: e.matmul(bkv[:, 0:256], lhsT=hT[:, k, c0:c0 + 128], rhs=G2[:, k, 0:256],
                                                           start=(k == 0), stop=(k == 7)), reads=[bG2, bhT[c]], writes=[bbkv])
                    P.op("act", lambda e: e.activation(out=vn[:], in_=bkv[:, 0:256], func=AF.Gelu_apprx_tanh), reads=[bbkv], writes=[bwc])
                    for g in range(4):
                        P.op("dve", lambda e, g=g: e.bn_stats(out=stat[:, g, :], in_=vn[:, g * 64:(g + 1) * 64]), reads=[bwc], writes=[bwc])
                    for g in range(4):
                        P.op("dve", lambda e, g=g: e.bn_aggr(out=mv[:, g, :], in_=stat[:, g, :]), reads=[bwc], writes=[bwc])
                    P.op("dve", lambda e: e.tensor_scalar(out=rstd[:], in0=mv[:, :, 1], scalar1=LN_EPS, scalar2=None, op0=ALU.add),
                         reads=[bwc], writes=[bwc])
                    P.op("act", lambda e: e.activation(out=rstd[:], in_=rstd[:], func=AF.Sqrt), reads=[bwc], writes=[bwc])
                    P.op("dve", lambda e: e.reciprocal(out=rstd[:], in_=rstd[:]), reads=[bwc], writes=[bwc])
                    for g in range(4):
                        P.op("dve", lambda e, g=g: e.tensor_scalar(out=vn[:, g * 64:(g + 1) * 64], in0=vn[:, g * 64:(g + 1) * 64],
                                                                   scalar1=mv[:, g, 0:1], scalar2=rstd[:, g:g + 1],
                                                                   op0=ALU.subtract, op1=ALU.mult), reads=[bwc], writes=[bwc])
                    P.op("dve", lambda e: e.tensor_tensor(out=vnb[:], in0=vn[:], in1=sgn[:], op=ALU.mult), reads=[bwc, bpar], writes=[bwc])
                    if lvl < 1.2:
                        return
                    bkc_, bbkc_ = C.bank()
                    for g in range(4):
                        lo = (g % 2) * 64
                        P.op("pe", lambda e, g=g, lo=lo: e.matmul(bkc_[lo:lo + 64, (g // 2) * 128:(g // 2) * 128 + 128],
                                                                  lhsT=vnb[:, g * 64:(g + 1) * 64], rhs=WcT[:, g, :], start=True, stop=True),
                             reads=[bwc, bpar], writes=[bbkc_])
                    if lvl < 1.3:
                        return
                    P.op("act", lambda e: e.activation(out=on[:], in_=bkc_[:, 0:256], func=AF.Copy), reads=[bbkc_, bwd], writes=[bwd])
                    for pr in range(2):
                        P.op("dve", lambda e, pr=pr: e.tensor_tensor(out=tmpc[:], in0=on[:, pr * 128:(pr + 1) * 128], in1=bsb[:, pr, :],
                                                                     op=ALU.add), reads=[bwd, bpar], writes=[bwc])
                        P.op("dve", lambda e, pr=pr: e.tensor_tensor(out=brT[:, 4 + pr, l0:l0 + 128], in0=tmpc[:], in1=uT[:, pr, l0:l0 + 128],
                                                                     op=ALU.mult), reads=[bwc, buT], writes=[bbr[4 + pr]])
                    if lvl < 1.4:
                        return
                    bks0, bbks0 = C.bank()
                    bks1, bbks1 = C.bank()
                    for h in range(4):
                        pr, hh = h // 2, h % 2
                        lo = hh * 64
                        bk_, bbk_ = (bks0, bbks0) if hh == 0 else (bks1, bbks1)
                        P.op("pe", lambda e, pr=pr, lo=lo, bk_=bk_: e.matmul(bk_[:, pr * 128:(pr + 1) * 128], lhsT=k_t[lo:lo + 64, pr, l0:l0 + 128],
                                                                             rhs=q_t[lo:lo + 64, pr, l0:l0 + 128], start=True, stop=True),
                             reads=[bstore], writes=[bbk_])
                    P.op("act", lambda e: e.activation(out=sraw_t.rearrange("p (pr hh) n -> p pr hh n", hh=2)[:, :, 0, :],
                                                       in_=bks0[:, 0:256].rearrange("p (pr n) -> p pr n", pr=2), func=AF.Copy),
                         reads=[bbks0, bwd], writes=[bwd, bhb16[0]])
                    P.op("act", lambda e: e.activation(out=sraw_t.rearrange("p (pr hh) n -> p pr hh n", hh=2)[:, :, 1, :],
                                                       in_=bks1[:, 0:256].rearrange("p (pr n) -> p pr n", pr=2), func=AF.Copy),
                         reads=[bbks1, bwd], writes=[bwd, bhb16[0]])
                    P.op("dve", lambda e: e.tensor_tensor(out=sm[:], in0=sraw_t, in1=decayT[:], op=ALU.mult),
                         reads=[bwd, bconst], writes=[bwd])
                    P.op("dve", lambda e: e.tensor_tensor(out=qd[:], in0=q_t[:, :, l0:l0 + 128], in1=qdecT[:], op=ALU.mult),
                         reads=[bstore, bconst], writes=[bwd])
                    bko0, bbko0 = C.bank()
                    bko1, bbko1 = C.bank()
                    for h in range(4):
                        pr, hh = h // 2, h % 2
                        lo = hh * 64
                        bk_, bbk_ = (bko0, bbko0) if hh == 0 else (bko1, bbko1)
                        P.op("pe", lambda e, h=h, pr=pr, bk_=bk_: e.matmul(bk_[:, pr * 64:(pr + 1) * 64], lhsT=sm[:, h, :], rhs=v_t[:, cc, h * 64:(h + 1) * 64],
                                                                           start=True, stop=False), reads=[bwd, bstore], writes=[bbk_])
                        P.op("pe", lambda e, pr=pr, lo=lo, bk_=bk_: e.matmul(bk_[:, pr * 64:(pr + 1) * 64], lhsT=qd[lo:lo + 64, pr, :],
                                                                             rhs=Sb[lo:lo + 64, pr * 64:(pr + 1) * 64], start=False, stop=True),
                             reads=[bwd, bS], writes=[bbk_])
                    P.op("act", lambda e: e.activation(out=on[:].rearrange("p (pr hh n) -> p pr hh n", pr=2, hh=2)[:, :, 0, :],
                                                       in_=bko0[:, 0:128].rearrange("p (pr n) -> p pr n", pr=2), func=AF.Copy),
                         reads=[bbko0, bwd], writes=[bwd])
                    P.op("act", lambda e: e.activation(out=on[:].rearrange("p (pr hh n) -> p pr hh n", pr=2, hh=2)[:, :, 1, :],
                                                       in_=bko1[:, 0:128].rearrange("p (pr n) -> p pr n", pr=2), func=AF.Copy),
                         reads=[bbko1, bwd], writes=[bwd])
                    if lvl < 1.6:
                        return
                    bkg, bbkg = C.bank()
                    for k in range(8):
                        P.op("pe", lambda e, k=k: e.matmul(bkg[:, 0:256], lhsT=hT[:, k, c0:c0 + 128], rhs=G2[:, k, 256:512],
                                                           start=(k == 0), stop=(k == 7)), reads=[bG2, bhT[c]], writes=[bbkg])
                    P.op("act", lambda e: e.activation(out=sgt[:], in_=bkg[:, 0:256], func=AF.Silu), reads=[bbkg], writes=[bwd])
                    for h in range(4):
                        P.op("dve", lambda e, h=h: e.bn_stats(out=stat[:, h, :], in_=oraw[:, h * 64:(h + 1) * 64]), reads=[bwd, bwc], writes=[bwc])
                    for h in range(4):
                        P.op("dve", lambda e, h=h: e.bn_aggr(out=mv[:, h, :], in_=stat[:, h, :]), reads=[bwc], writes=[bwc])
                    P.op("dve", lambda e: e.tensor_scalar(out=rstd[:], in0=mv[:, :, 1], scalar1=LN_EPS, scalar2=None, op0=ALU.add),
                         reads=[bwc], writes=[bwc])
                    P.op("act", lambda e: e.activation(out=rstd[:], in_=rstd[:], func=AF.Sqrt), reads=[bwc], writes=[bwc])
                    P.op("dve", lambda e: e.reciprocal(out=rstd[:], in_=rstd[:]), reads=[bwc], writes=[bwc])
                    for h in range(4):
                        P.op("dve", lambda e, h=h: e.tensor_scalar(out=on[:, h * 64:(h + 1) * 64], in0=oraw[:, h * 64:(h + 1) * 64],
                                                                   scalar1=mv[:, h, 0:1], scalar2=rstd[:, h:h + 1],
                                                                   op0=ALU.subtract, op1=ALU.mult), reads=[bwc, bwd], writes=[bwd])
                    P.op("dve", lambda e: e.tensor_tensor(out=on[:], in0=on[:], in1=rng_[:], op=ALU.mult), reads=[bwd, bpar], writes=[bwd])
                    P.op("dve", lambda e: e.tensor_tensor(out=onb[:], in0=on[:], in1=sgt[:], op=ALU.mult), reads=[bwd], writes=[bwd])
                    if lvl < 1.8:
                        return
                    bkt, bbkt = C.bank()
                    btv = bkt[:].bitcast(BF16)
                    for pr in range(2):
                        P.op("pe", lambda e, pr=pr: e.transpose(out=btv[:, pr * 128:(pr + 1) * 128], in_=onb[:, pr * 128:(pr + 1) * 128],
                                                                identity=identb[:]), reads=[bwd, bconst], writes=[bbkt])
                    P.op("act", lambda e: e.activation(out=brT[:, 6:8, l0:l0 + 128], in_=btv[:, 0:256].rearrange("p (j n) -> p j n", j=2),
                                                       func=AF.Copy), reads=[bbkt], writes=[bbr[6], bbr[7]])
                    for pr in range(2):
                        P.op("dve", lambda e, pr=pr: e.scalar_tensor_tensor(
                            out=Sst[:, pr * 64:(pr + 1) * 64], in0=Sst[:, pr * 64:(pr + 1) * 64], scalar=cd[:, pr:pr + 1],
                            in1=kv_t[:, cc, pr * 64:(pr + 1) * 64], op0=ALU.mult, op1=ALU.add), reads=[bS, bconst, bstore], writes=[bS])
                    P.op("act", lambda e: e.activation(out=Sb[:], in_=Sst[:], func=AF.Copy), reads=[bS], writes=[bS])

                if lvl < 1.1:
                    return
                for cc in range(4):
                    chunk_body(cc)
                if lvl < 3:
                    return

                if stage != "full":
                    P.dma("sp", cdbg, o_brT[:, :, t0:t0 + 512].rearrange("j p t -> p j t"), brT[:], reads=bbr, writes=[bdbg])
                def gate_body(j):
                    def pairs(s_):
                        pr_ = [(s_[:, :, b * 128:(b + 1) * 128], win_cols(2560 + b * 1024 + j * 128, 128)) for b in range(4)]
                        pr_ += [(s_[:, 0:2, 512 + b * 32:512 + b * 32 + 32].rearrange("p k n -> p k n"), None) for b in range(0)]
                        return pr_
                    Gj, bGj = wload(pairs)
                    i_ = (wsn[0] - 1) % NSLOT
                    P.dma_group("pool", cws[i_], [(wslot[i_][:, b * 2:b * 2 + 2, 512:640],
                                                   bp_d[b, :, j * 128:(j + 1) * 128].rearrange("(k p) n -> p k n", p=128)) for b in range(4)],
                                writes=[bGj])
                    if lvl == 3.1:
                        return
                    def gate_b(b):
                        bkz, bbkz = C.bank()
                        for k in range(8):
                            P.op("pe", lambda e, k=k, b=b: e.matmul(bkz[:], lhsT=Gj[:, k, b * 128:(b + 1) * 128], rhs=hT[:, k, t0:t0 + 512],
                                                                    start=(k == 0), stop=(k == 7)), reads=[bGj] + bhT_t, writes=[bbkz])
                        bkp, bbkp = C.bank()
                        for kc in range(2):
                            P.op("pe", lambda e, kc=kc, b=b: e.matmul(bkp[:], lhsT=Gj[:, b * 2 + kc, 512:640], rhs=brT[:, b * 2 + kc, :],
                                                                      start=(kc == 0), stop=(kc == 1)),
                                 reads=[bGj, bbr[b * 2], bbr[b * 2 + 1]], writes=[bbkp])
                        if lvl == 3.2:
                            return
                        P.op("act", lambda e: e.activation(out=sig[:], in_=bkz[:], func=AF.Tanh, scale=0.5), reads=[bbkz], writes=[bwg])
                        if lvl == 3.3:
                            return
                        if b == 0:
                            P.op("dve", lambda e: e.scalar_tensor_tensor(out=macc[:], in0=sig[:], scalar=1.0, in1=bkp[:], op0=ALU.add, op1=ALU.mult),
                                 reads=[bbkp, bwg], writes=[bwg])
                        else:
                            P.op("dve", lambda e: e.scalar_tensor_tensor(out=mtmp[:], in0=sig[:], scalar=1.0, in1=bkp[:], op0=ALU.add, op1=ALU.mult),
                                 reads=[bbkp, bwg], writes=[bwg])
                            P.op("dve", lambda e: e.tensor_tensor(out=macc[:], in0=macc[:], in1=mtmp[:], op=ALU.add), reads=[bwg], writes=[bwg])
                            if b == 3:
                                P.op("dve", lambda e: e.tensor_scalar(out=mergedT[:, j, :], in0=macc[:], scalar1=0.5, scalar2=None, op0=ALU.mult),
                                     reads=[bwg], writes=[bmg[j]])

                    for b_i in range(4):
                        gate_b(b_i)

                for j in range(8):
                    gate_body(j)
                if lvl < 4:
                    return

                def wout_half(half):
                    Wh, bWh = wload(lambda s_: [(s_[:, :, 0:512], wout_d[:, half * 512:(half + 1) * 512].rearrange("(k p) n -> p k n", p=128))])
                    for cc in range(4):
                        c = T * 4 + cc
                        bkm, bbkm = C.bank()
                        for k in range(8):
                            P.op("pe", lambda e, k=k, cc=cc, bkm=bkm: e.matmul(bkm[:], lhsT=mergedT[:, k, cc * 128:(cc + 1) * 128], rhs=Wh[:, k, 0:512],
                                                                               start=(k == 0), stop=(k == 7)), reads=[bWh] + bmg, writes=[bbkm])
                        P.op("dve", lambda e, c=c, bkm=bkm: e.scalar_tensor_tensor(
                            out=hres[:, c, half * 512:(half + 1) * 512], in0=hres[:, c, half * 512:(half + 1) * 512], scalar=ALPHA,
                            in1=bkm[:], op0=ALU.mult, op1=ALU.add), reads=[bbkm, bhres[c]], writes=[bhres[c]])

                for half_i in range(2):
                    wout_half(half_i)

                def ln_body(c):
                    for hf in range(2):
                        P.op("dve", lambda e, hf=hf: e.bn_stats(out=lnstat[:, hf, :], in_=hres[:, c, hf * 512:(hf + 1) * 512]),
                             reads=[bhres[c]], writes=[bln])
                    P.op("dve", lambda e: e.bn_aggr(out=lnmv[:], in_=lnstat[:].rearrange("p a b -> p (a b)")), reads=[bln], writes=[bln])
                    P.op("dve", lambda e: e.tensor_scalar(out=lnr[:, 0:1], in0=lnmv[:, 1:2], scalar1=LN_EPS, scalar2=None, op0=ALU.add),
                         reads=[bln], writes=[bln])
                    P.op("act", lambda e: e.activation(out=lnr[:, 0:1], in_=lnr[:, 0:1], func=AF.Sqrt), reads=[bln], writes=[bln])
                    P.op("dve", lambda e: e.reciprocal(out=lnr[:, 0:1], in_=lnr[:, 0:1]), reads=[bln], writes=[bln])
                    P.op("dve", lambda e: e.tensor_scalar(out=lnr[:, 1:2], in0=lnmv[:, 0:1], scalar1=lnr[:, 0:1], scalar2=-1.0,
                                                          op0=ALU.mult, op1=ALU.mult), reads=[bln], writes=[bln])
                    P.op("act", lambda e: e.activation(out=h1[:], in_=hres[:, c, :], func=AF.Identity, scale=lnr[:, 0:1], bias=lnr[:, 1:2]),
                         reads=[bhres[c], bln], writes=[bh1])
                    P.op("pool", lambda e: e.tensor_tensor(out=h1[:], in0=h1[:], in1=lmg[:], op=ALU.mult), reads=[bh1, bpar], writes=[bh1])
                    P.op("pool", lambda e: e.tensor_tensor(out=h1[:], in0=h1[:], in1=lmb[:], op=ALU.add), reads=[bh1, bpar], writes=[bh1])
                    P.op("act", lambda e: e.activation(out=hres[:, c, :], in_=h1[:], func=AF.Copy, scale=ALPHA), reads=[bh1], writes=[bhres[c]])
                    for half in range(2):
                        bk_, bbk_ = C.bank()
                        for k4 in range(4):
                            k = half * 4 + k4
                            P.op("pe", lambda e, k=k, k4=k4, bk_=bk_: e.transpose(out=bk_[:, k4 * 128:(k4 + 1) * 128], in_=h1[:, k * 128:(k + 1) * 128],
                                                                                  identity=identf[:]), reads=[bh1, bconst], writes=[bbk_])
                        P.op("act" if half == 0 else "dve",
                             (lambda e, bk_=bk_, half=half: e.activation(out=h1T[:, half * 4:half * 4 + 4, :],
                                                                         in_=bk_[:].rearrange("p (k n) -> p k n", k=4), func=AF.Copy)) if half == 0 else
                             (lambda e, bk_=bk_, half=half: e.tensor_copy(out=h1T[:, half * 4:half * 4 + 4, :],
                                                                          in_=bk_[:].rearrange("p (k n) -> p k n", k=4))),
                             reads=[bbk_], writes=[bh1T])
                    P.op("pool", lambda e: e.tensor_copy(out=hT[:, :, c * 128:(c + 1) * 128], in_=h1T[:]), reads=[bh1T], writes=[bhT[c]])
                    bkr, bbkr = C.bank()
                    for k in range(8):
                        P.op("pe", lambda e, k=k: e.matmul(bkr[:, 0:36], lhsT=h1T[:, k, :], rhs=wr[:, k, :], start=(k == 0), stop=(k == 7)),
                             reads=[bh1T, bpar], writes=[bbkr])
                    R_ = lambda a, b=None: rs[:, a:(a + 1 if b is None else b)]
                    P.op("dve", lambda e: e.tensor_tensor(out=lg[:], in0=bkr[:, 0:36], in1=rbias[:], op=ALU.add), reads=[bbkr, bpar], writes=[brt])
                    P.op("dve", lambda e: e.reduce_max(out=R_(0), in_=lg[:, 0:4], axis=AX.X), reads=[brt], writes=[brt])
                    P.op("dve", lambda e: e.tensor_scalar(out=R_(4, 8), in0=lg[:, 0:4], scalar1=R_(0), scalar2=None, op0=ALU.is_ge),
                         reads=[brt], writes=[brt])
                    P.op("dve", lambda e: e.tensor_scalar(out=R_(4, 8), in0=R_(4, 8), scalar1=-1.0, scalar2=1e30, op0=ALU.add, op1=ALU.mult),
                         reads=[brt], writes=[brt])
                    for g in range(4):
                        P.op("dve", lambda e, g=g: e.tensor_scalar(out=elm[:, g * 8:(g + 1) * 8], in0=lg[:, 4 + g * 8:12 + g * 8],
                                                                   scalar1=R_(4 + g), scalar2=None, op0=ALU.add), reads=[brt], writes=[brt])
                    P.op("dve", lambda e: e.max(out=top8[:], in_=elm[:]), reads=[brt], writes=[brt])
                    P.op("dve", lambda e: e.tensor_tensor(out=R_(1), in0=top8[:, 1:2], in1=top8[:, 0:1], op=ALU.subtract), reads=[brt], writes=[brt])
                    P.op("act", lambda e: e.activation(out=R_(1), in_=R_(1), func=AF.Exp), reads=[brt], writes=[brt])
                    P.op("dve", lambda e: e.tensor_scalar(out=R_(2), in0=R_(1), scalar1=1.0, scalar2=None, op0=ALU.add), reads=[brt], writes=[brt])
                    P.op("dve", lambda e: e.reciprocal(out=R_(2), in_=R_(2)), reads=[brt], writes=[brt])
                    P.op("dve", lambda e: e.tensor_tensor(out=R_(3), in0=R_(1), in1=R_(2), op=ALU.mult), reads=[brt], writes=[brt])
                    P.op("dve", lambda e: e.tensor_scalar(out=R_(8), in0=R_(0), scalar1=-1.0, scalar2=None, op0=ALU.mult), reads=[brt], writes=[brt])
                    P.op("act", lambda e: e.activation(out=R_(9, 13), in_=lg[:, 0:4], func=AF.Exp, bias=R_(8)), reads=[brt], writes=[brt])
                    P.op("dve", lambda e: e.reduce_sum(out=R_(13), in_=R_(9, 13), axis=AX.X), reads=[brt], writes=[brt])
                    P.op("dve", lambda e: e.reciprocal(out=R_(13), in_=R_(13)), reads=[brt], writes=[brt])
                    P.op("dve", lambda e: e.tensor_tensor(out=R_(2), in0=R_(2), in1=R_(13), op=ALU.mult), reads=[brt], writes=[brt])
                    P.op("dve", lambda e: e.tensor_tensor(out=R_(3), in0=R_(3), in1=R_(13), op=ALU.mult), reads=[brt], writes=[brt])
                    P.op("dve", lambda e: e.tensor_scalar(out=c1[:], in0=elm[:], scalar1=top8[:, 0:1], scalar2=R_(2), op0=ALU.is_equal, op1=ALU.mult),
                         reads=[brt], writes=[brt])
                    P.op("dve", lambda e: e.tensor_scalar(out=comb[:, c, :], in0=elm[:], scalar1=top8[:, 1:2], scalar2=R_(3), op0=ALU.is_equal,
                                                          op1=ALU.mult), reads=[brt], writes=[bcomb[c]])
                    P.op("dve", lambda e: e.tensor_tensor(out=comb[:, c, :], in0=comb[:, c, :], in1=c1[:], op=ALU.add), reads=[brt, bcomb[c]],
                         writes=[bcomb[c]])
                    if stage != "full":
                        P.dma("sp", cdbg, o_h[c * 128:(c + 1) * 128, :], h1[:], reads=[bh1], writes=[bdbg])
                        P.dma("sp", cdbg, o_dbg[c * 128:(c + 1) * 128, :], comb[:, c, :], reads=[bcomb[c]], writes=[bdbg])

                if lvl < 5:
                    return
                for cc in range(4):
                    ln_body(T * 4 + cc)

            for T_ in range(NT):
                tile_body(T_)
            if stage != "full":
                P.wait_all("sp", [bdbg])
            P.barrier(allchans)
            with nc.Block() as block:
                P.emit(block)
        if stage != "full":
            return nc
        build_p2_moe(nc, st, C, hres, bhres, hT, bhT, comb, bcomb, identb, bconst, ewg_d, ewu_d, ewd_d, lfg_d, lfb_d, o_h, allchans)
    return nc


def build_p3(nexp=32):
    nc = bass.Bass("TRN2", target_bir_lowering=False)
    with ExitStack() as st:
        C = Ctx(nc, st)
        P = C.P
        h1_d = C.din("h1", [TOK, D], F32)
        comb_d = C.din("comb", [TOK, 32], F32)
        ewg_d = C.din("exp_w_gate", [32, D, 256], F32)
        ewu_d = C.din("exp_w_up", [32, D, 256], F32)
        ewd_d = C.din("exp_w_down", [32, 256, D], F32)
        lfg_d = C.din("ln_ffn_g", [1, D], F32)
        lfb_d = C.din("ln_ffn_b", [1, D], F32)
        identb_d = C.din("ident_b", [128, 128], BF16)
        o_h = C.dout("o_h", [TOK, D], F32)

        hres = C.sb("hres", [128, NCH, D], F32)
        bhres = [Buf("hres%d" % c) for c in range(NCH)]
        hT = C.sb("hT", [128, 8, TOK], BF16)
        bhT = [Buf("hT%d" % c) for c in range(NCH)]
        comb = C.sb("comb", [128, NCH, 32], F32)
        bcomb = Buf("comb")
        identb = C.sb("identb", [128, 128], BF16)
        lfg = C.sb("lfg", [128, D], F32)
        lfb = C.sb("lfb", [128, D], F32)
        bconst = Buf("const")
        cst = P.chan()
        P.dma("sp", cst, identb[:], identb_d, writes=[bconst])
        P.dma("sp", cst, lfg[:], lfg_d.partition_broadcast(128), writes=[bconst])
        P.dma("sp", cst, lfb[:], lfb_d.partition_broadcast(128), writes=[bconst])
        P.dma("sp", cst, comb[:], comb_d.rearrange("(c p) n -> p c n", p=128), writes=[bcomb])

        hb16 = [C.sb("hb16_%d" % i, [128, D], BF16) for i in range(2)]
        bhb16 = [Buf("hb16_%d" % i) for i in range(2)]
        chh = [P.chan() for _ in range(2)]

        def load_chunk(c):
            s_ = c % 2
            P.dma("sp" if c % 2 == 0 else "act", chh[s_], hres[:, c, :], h1_d[c * 128:(c + 1) * 128, :], writes=[bhres[c]])
            P.op("pool", lambda e: e.tensor_copy(out=hb16[s_][:], in_=hres[:, c, :]), reads=[bhres[c]], writes=[bhb16[s_]])
            bk, bbk = C.bank(2, 4)
            bv = bk[:].bitcast(BF16)
            for k in range(8):
                P.op("pe", lambda e, k=k: e.transpose(out=bv[:, k * 128:(k + 1) * 128], in_=hb16[s_][:, k * 128:(k + 1) * 128],
                                                      identity=identb[:]), reads=[bhb16[s_], bconst], writes=[bbk])
            P.op("act", lambda e: e.activation(out=hT[:, 0:4, c * 128:(c + 1) * 128],
                                               in_=bv[:, 0:512].rearrange("p (k n) -> p k n", k=4), func=AF.Copy), reads=[bbk], writes=[bhT[c]])
            P.op("dve", lambda e: e.tensor_copy(out=hT[:, 4:8, c * 128:(c + 1) * 128],
                                                in_=bv[:, 512:1024].rearrange("p (k n) -> p k n", k=4)), reads=[bbk], writes=[bhT[c]])
            P.op("act", lambda e: e.activation(out=hres[:, c, :], in_=hres[:, c, :], func=AF.Copy, scale=ALPHA),
                 reads=[bhb16[s_]], writes=[bhres[c]])

        for c_ in range(NCH):
            load_chunk(c_)

        GS = 2
        NSET = 2
        wgu = [[C.sb("wgu%d_%d" % (s_, i), [128, 8, 512], BF16) for i in range(GS)] for s_ in range(NSET)]
        wdn = [[C.sb("wdn%d_%d" % (s_, i), [128, 2, D], BF16) for i in range(GS)] for s_ in range(NSET)]
        bwset = [Buf("wset%d" % s_) for s_ in range(NSET)]
        cwset = [P.chan() for _ in range(NSET)]

        def load_group(g):
            s_ = g % NSET
            pairs = []
            for i in range(GS):
                e_ = g * GS + i
                pairs.append((wgu[s_][i][:, :, 0:256], ewg_d[e_].rearrange("(k p) n -> p k n", p=128)))
                pairs.append((wgu[s_][i][:, :, 256:512], ewu_d[e_].rearrange("(k p) n -> p k n", p=128)))
                pairs.append((wdn[s_][i][:], ewd_d[e_].rearrange("(k p) n -> p k n", p=128)))
            P.dma_group("pool", cwset[s_], pairs, writes=[bwset[s_]])

        NB = 3
        sl = [C.sb("sl%d" % i, [128, 256], F32) for i in range(NB)]
        act = [C.sb("act%d" % i, [128, 256], BF16) for i in range(NB)]
        actT = [C.sb("actT%d" % i, [128, 2, 128], BF16) for i in range(2)]
        bsl = [Buf("sl%d" % i) for i in range(NB)]
        bact = [Buf("act%d" % i) for i in range(NB)]
        bactT = [Buf("actT%d" % i) for i in range(2)]
        ngroups = nexp // GS
        items = [(g, c, i) for g in range(ngroups) for c in range(NCH) for i in range(GS)]

        def acc_of(g, c):
            a0 = 4 + ((g * NCH + c) % 2) * 2
            return [C.banks[a0], C.banks[a0 + 1]], [C.bbank[a0], C.bbank[a0 + 1]]

        def stage_a(n):
            g, c, i = items[n]
            s_ = g % NSET
            p = n % NB
            e_ = g * GS + i
            bk, bbk = C.banks[p], C.bbank[p]
            for k in range(8):
                P.op("pe", lambda e, k=k: e.matmul(bk[:], lhsT=hT[:, k, c * 128:(c + 1) * 128], rhs=wgu[s_][i][:, k, :],
                                                   start=(k == 0), stop=(k == 7)), reads=[bhT[c], bwset[s_]], writes=[bbk])
            P.op("act", lambda e: e.activation(out=sl[p][:], in_=bk[:, 0:256], func=AF.Silu), reads=[bbk], writes=[bsl[p]])
            P.op("dve", lambda e: e.scalar_tensor_tensor(out=act[p][:], in0=sl[p][:], scalar=comb[:, c, e_:e_ + 1], in1=bk[:, 256:512],
                                                         op0=ALU.mult, op1=ALU.mult), reads=[bsl[p], bcomb, bbk], writes=[bact[p]])

        def stage_t(n):
            p, q = n % NB, n % 2
            bt, bbt = C.banks[3], C.bbank[3]
            btv = bt[:].bitcast(BF16)
            for j in range(2):
                P.op("pe", lambda e, j=j: e.transpose(out=btv[:, j * 128:(j + 1) * 128], in_=act[p][:, j * 128:(j + 1) * 128],
                                                      identity=identb[:]), reads=[bact[p], bconst], writes=[bbt])
            P.op("act", lambda e: e.activation(out=actT[q][:], in_=btv[:, 0:256].rearrange("p (j n) -> p j n", j=2), func=AF.Copy),
                 reads=[bbt], writes=[bactT[q]])

        def stage_d(n):
            g, c, i = items[n]
            s_ = g % NSET
            q = n % 2
            acc, bacc = acc_of(g, c)
            for half in range(2):
                for kc in range(2):
                    P.op("pe", lambda e, half=half, kc=kc: e.matmul(
                        acc[half][:], lhsT=actT[q][:, kc, :], rhs=wdn[s_][i][:, kc, half * 512:(half + 1) * 512],
                        start=(i == 0 and kc == 0), stop=(i == GS - 1 and kc == 1)),
                        reads=[bactT[q], bwset[s_]], writes=[bacc[half]])
            if i == GS - 1:
                for half in range(2):
                    P.op("dve", lambda e, half=half: e.tensor_tensor(out=hres[:, c, half * 512:(half + 1) * 512], in0=acc[half][:],
                                                                     in1=hres[:, c, half * 512:(half + 1) * 512], op=ALU.add),
                         reads=[bacc[half], bhres[c]], writes=[bhres[c]])

        loaded = [0]

        def ensure_loaded(g):
            while loaded[0] < g:
                loaded[0] += 1
                load_group(loaded[0])

        load_group(0)
        nit = len(items)
        for n0 in range(min(2, nit)):
            ensure_loaded(items[n0][0])
            stage_a(n0)
        for n_ in range(nit):
            stage_t(n_)
            if n_ + 2 < nit:
                g_need = items[n_ + 2][0]
                if g_need > loaded[0]:
                    assert items[n_][0] >= g_need - 1
                ensure_loaded(g_need)
                stage_a(n_ + 2)
            stage_d(n_)
            if items[n_][1] == 0 and items[n_][2] == 0 and items[n_][0] + 1 < ngroups:
                ensure_loaded(items[n_][0] + 1)

        lnstat = C.sb("lnstat", [128, 2, 6], F32)
        lnmv = C.sb("lnmv", [128, 2], F32)
        lnr = C.sb("lnr", [128, 2], F32)
        ho = [C.sb("ho%d" % i, [128, D], F32) for i in range(2)]
        bho = [Buf("ho%d" % i) for i in range(2)]
        bln = Buf("ln")
        cout = [P.chan() for _ in range(2)]
        bout = Buf("out")

        def ln_chunk(c):
            s_ = c % 2
            for hf in range(2):
                P.op("dve", lambda e, hf=hf: e.bn_stats(out=lnstat[:, hf, :], in_=hres[:, c, hf * 512:(hf + 1) * 512]),
                     reads=[bhres[c]], writes=[bln])
            P.op("dve", lambda e: e.bn_aggr(out=lnmv[:], in_=lnstat[:].rearrange("p a b -> p (a b)")), reads=[bln], writes=[bln])
            P.op("dve", lambda e: e.tensor_scalar(out=lnr[:, 0:1], in0=lnmv[:, 1:2], scalar1=LN_EPS, scalar2=None, op0=ALU.add),
                 reads=[bln], writes=[bln])
            P.op("act", lambda e: e.activation(out=lnr[:, 0:1], in_=lnr[:, 0:1], func=AF.Sqrt), reads=[bln], writes=[bln])
            P.op("dve", lambda e: e.reciprocal(out=lnr[:, 0:1], in_=lnr[:, 0:1]), reads=[bln], writes=[bln])
            P.op("dve", lambda e: e.tensor_scalar(out=lnr[:, 1:2], in0=lnmv[:, 0:1], scalar1=lnr[:, 0:1], scalar2=-1.0,
                                                  op0=ALU.mult, op1=ALU.mult), reads=[bln], writes=[bln])
            P.op("act", lambda e: e.activation(out=ho[s_][:], in_=hres[:, c, :], func=AF.Identity, scale=lnr[:, 0:1], bias=lnr[:, 1:2]),
                 reads=[bhres[c], bln], writes=[bho[s_]])
            P.op("pool", lambda e: e.tensor_tensor(out=ho[s_][:], in0=ho[s_][:], in1=lfg[:], op=ALU.mult), reads=[bho[s_], bconst], writes=[bho[s_]])
            P.op("pool", lambda e: e.tensor_tensor(out=ho[s_][:], in0=ho[s_][:], in1=lfb[:], op=ALU.add), reads=[bho[s_], bconst], writes=[bho[s_]])
            P.dma("sp", cout[s_], o_h[c * 128:(c + 1) * 128, :], ho[s_][:], reads=[bho[s_]], writes=[bout])

        for c_ in range(NCH):
            ln_chunk(c_)
        P.wait_all("sp", [bout])
        with nc.Block() as block:
            P.emit(block)
    return nc


_PROGS = {}


def _prog(name):
    if name not in _PROGS:
        _PROGS[name] = {"p1": build_p1, "p2": lambda: build_p2("A"), "p3": build_p3}[name]()
    return _PROGS[name]


def _seg(c):
    b, k = c // 4, c % 4
    return b, k * TOK


def _halo(h_full, c):
    b, s0 = _seg(c)
    halo = np.zeros((4, D), np.float32)
    if s0 > 0:
        halo[1:4] = h_full[b, s0 - 3:s0]
    return halo


def kernel(**inputs):
    inp = {k: np.asarray(v) for k, v in inputs.items()}
    cs, cs2 = consts_np(), consts2_np()
    h_full = np.ascontiguousarray(inp["x"], dtype=np.float32)
    pos = np.ascontiguousarray(inp["positions"]).astype(np.int32)
    cores = list(range(NCORES))
    for L in range(2):
        w_in = np.ascontiguousarray(inp["w_in"][L])
        w_p1 = np.ascontiguousarray(np.concatenate([w_in[:, 768:1024], w_in[:, 1536:2304]], axis=1))
        row = lambda name: np.ascontiguousarray(inp[name][L][None])
        in1 = []
        for c in cores:
            b, s0 = _seg(c)
            d = dict(h=np.ascontiguousarray(h_full[b, s0:s0 + TOK]), halo=_halo(h_full, c),
                     pos=np.ascontiguousarray(pos[b:b + 1, s0:s0 + TOK]), w_p1=w_p1,
                     lru_conv_w=np.ascontiguousarray(inp["lru_conv_w"][L]), lru_conv_b=row("lru_conv_b"),
                     lru_w_r=np.ascontiguousarray(inp["lru_w_r"][L]), lru_w_i=np.ascontiguousarray(inp["lru_w_i"][L]),
                     lru_b_r=row("lru_b_r"), lru_b_i=row("lru_b_i"), lru_lambda=row("lru_lambda"))
            for n in ("ident_f", "ident_b", "rot_b", "invf", "kdec", "cd"):
                d[n] = cs[n]
            in1.append(d)
        r1 = run_bass_kernel_spmd(_prog("p1"), in1, core_ids=cores).results
        ends_all = np.ascontiguousarray(np.stack([np.asarray(r1[c]["o_end"]) for c in cores]))
        in2 = []
        for c in cores:
            b, s0 = _seg(c)
            d = dict(h=np.ascontiguousarray(h_full[b, s0:s0 + TOK]), halo=_halo(h_full, c),
                     hloc=np.asarray(r1[c]["o_hloc"]), Pc=np.asarray(r1[c]["o_P"]), qT=np.asarray(r1[c]["o_qT"]), kT=np.asarray(r1[c]["o_kT"]),
                     v=np.asarray(r1[c]["o_v"]), kv=np.asarray(r1[c]["o_kv"]), ends_all=ends_all, sel=sel_np(c), w_in=w_in,
                     sc_conv_w=np.ascontiguousarray(inp["sc_conv_w"][L]), sc_conv_b=row("sc_conv_b"), sg_norm_g=row("sg_norm_g"),
                     sg_w_s=np.ascontiguousarray(inp["sg_w_s"][L]), sg_b_s=np.ascontiguousarray(inp["sg_b_s"][L]), ret_norm_g=row("ret_norm_g"),
                     branch_proj=np.ascontiguousarray(inp["branch_proj"][L]), w_out=np.ascontiguousarray(inp["w_out"][L]),
                     ln_mix_g=row("ln_mix_g"), ln_mix_b=row("ln_mix_b"),
                     router_group_w=np.ascontiguousarray(inp["router_group_w"][L]), router_group_b=row("router_group_b"),
                     router_expert_w=np.ascontiguousarray(inp["router_expert_w"][L]), router_expert_b=row("router_expert_b"))
            d["ident_f"], d["ident_b"], d["cd"] = cs["ident_f"], cs["ident_b"], cs["cd"]
            for n in ("decayT", "qdecT", "cdT", "causal"):
                d[n] = cs2[n]
            in2.append(d)
        r2 = run_bass_kernel_spmd(_prog("p2"), in2, core_ids=cores).results
        ewg = np.ascontiguousarray(inp["exp_w_gate"][L])
        ewu = np.ascontiguousarray(inp["exp_w_up"][L])
        ewd = np.ascontiguousarray(inp["exp_w_down"][L])
        in3 = [dict(h1=np.asarray(r2[c]["o_h"]), comb=np.asarray(r2[c]["o_dbg"]), exp_w_gate=ewg, exp_w_up=ewu, exp_w_down=ewd,
                    ln_ffn_g=row("ln_ffn_g"), ln_ffn_b=row("ln_ffn_b"), ident_b=cs["ident_b"]) for c in cores]
        r3 = run_bass_kernel_spmd(_prog("p3"), in3, core_ids=cores).results
        h_next = np.empty_like(h_full)
        for c in cores:
            b, s0 = _seg(c)
            h_next[b, s0:s0 + TOK] = np.asarray(r3[c]["o_h"])
        h_full = h_next
    return h_full.astype(np.float32)
```
